# Optimizing a Trainium2 kernel written in Bass

```python
import math, functools
import jax, jax.numpy as jnp
from jax import lax
import numpy as np

D_MODEL = 1024
BATCH = 2
SEQ = 16384
DEPTH = 2

GRID_W = 64
CTX_LEN = 256
EPS = 1e-6

GLA_HEADS = 4
GLA_DK = 32
GLA_DV = 64
GLA_K = GLA_HEADS * GLA_DK
GLA_V = GLA_HEADS * GLA_DV
GATE_RANK = 16
GATE_TEMP = 16.0
GLA_CHUNK = 64

DIFF_HEADS = 4
DIFF_DH = 64
DIFF_DV = 2 * DIFF_DH
DIFF_QK = DIFF_HEADS * 2 * DIFF_DH
DIFF_V = DIFF_HEADS * DIFF_DV
Q_BLOCK = 128
ROPE_BASE = 10000.0
AX_DIM = DIFF_DH // 2

POOL_WINDOWS = (2, 4, 8, 16)
POOL_CH = 64
POOL_W = len(POOL_WINDOWS) * POOL_CH

MIX_WIDTH = GLA_V + DIFF_V + POOL_W
IN_SIZES = (GLA_K, GLA_K, GLA_V, GLA_V, GATE_RANK, GATE_RANK, DIFF_QK, DIFF_QK, DIFF_V, POOL_W)
IN_COLS = GLA_K + GLA_K + GLA_V + GLA_V + GATE_RANK + GATE_RANK + DIFF_QK + DIFF_QK + DIFF_V + POOL_W

N_GROUPS = 4
EXPERTS_PER_GROUP = 4
N_EXPERTS = N_GROUPS * EXPERTS_PER_GROUP
TOP_K = 2
D_EXPERT = 512
MOE_BLOCK = 256

kernel_name = "hybrid_gla_diffattn_pool_hmoe_dit"


def rmsnorm(x, g):
    xf = x.astype(jnp.float32)
    xf = xf * lax.rsqrt(jnp.mean(xf * xf, axis=-1, keepdims=True) + EPS)
    return (xf * g.astype(jnp.float32)).astype(x.dtype)


def head_rmsnorm(o, g):
    h, d = o.shape[-2], o.shape[-1]
    of = o.astype(jnp.float32)
    of = of * lax.rsqrt(jnp.mean(of * of, axis=-1, keepdims=True) + EPS)
    return (of * g.astype(jnp.float32).reshape(h, d)).astype(o.dtype)


def adaln(cond, w_mod, b_mod):
    return jnp.split(jax.nn.silu(cond) @ w_mod + b_mod, 6, axis=-1)


def modulate(x, g, shift, scale):
    return rmsnorm(x, g) * (1.0 + scale) + shift


def axial_rope_tables(n_lat):
    rows = n_lat // GRID_W
    row = jnp.repeat(jnp.arange(rows, dtype=jnp.float32), GRID_W)
    col = jnp.tile(jnp.arange(GRID_W, dtype=jnp.float32), rows)
    inv = 1.0 / (ROPE_BASE ** (jnp.arange(0, AX_DIM, 2, dtype=jnp.float32) / AX_DIM))
    ang_r = row[:, None] * inv
    ang_c = col[:, None] * inv
    shp = (1, n_lat, 1, 1, AX_DIM // 2)
    return (jnp.cos(ang_r).reshape(shp), jnp.sin(ang_r).reshape(shp),
            jnp.cos(ang_c).reshape(shp), jnp.sin(ang_c).reshape(shp))


def rope_half(x, cos, sin):
    x1, x2 = jnp.split(x, 2, axis=-1)
    return jnp.concatenate([x1 * cos - x2 * sin, x2 * cos + x1 * sin], axis=-1)


def apply_axial_rope(x, tabs):
    cos_r, sin_r, cos_c, sin_c = tabs
    xf = x.astype(jnp.float32)
    out = jnp.concatenate([rope_half(xf[..., :AX_DIM], cos_r, sin_r),
                           rope_half(xf[..., AX_DIM:], cos_c, sin_c)], axis=-1)
    return out.astype(x.dtype)


def split_projection(z):
    parts, start = [], 0
    for size in IN_SIZES:
        parts.append(z[..., start:start + size])
        start += size
    return parts


def mixer_inputs(h, w_in, wa2_f, ba_f, wa2_b, ba_b):
    bsz, seq, _ = h.shape
    qg, kg, vg, og, af, ab, qd, kd, vd, pl = split_projection(h @ w_in)
    gla_shape = (bsz, seq, GLA_HEADS, GLA_DK)
    q_g = qg.reshape(gla_shape) * (GLA_DK ** -0.5)
    k_g = kg.reshape(gla_shape)
    v_g = vg.reshape(bsz, seq, GLA_HEADS, GLA_DV)
    la_f = jax.nn.log_sigmoid((af @ wa2_f + ba_f).astype(jnp.float32)).reshape(gla_shape) / GATE_TEMP
    la_b = jax.nn.log_sigmoid((ab @ wa2_b + ba_b).astype(jnp.float32)).reshape(gla_shape) / GATE_TEMP
    q_d = qd.reshape(bsz, seq, DIFF_HEADS, 2, DIFF_DH)
    k_d = kd.reshape(bsz, seq, DIFF_HEADS, 2, DIFF_DH)
    v_d = vd.reshape(bsz, seq, DIFF_HEADS, DIFF_DV)
    return q_g, k_g, v_g, og, la_f, la_b, q_d, k_d, v_d, pl


def gla_chunked(q, k, v, log_a, s0):
    bsz, seq, heads, _ = q.shape
    n_chunks = seq // GLA_CHUNK

    def to_chunks(t):
        return t.reshape(bsz, n_chunks, GLA_CHUNK, heads, t.shape[-1]).transpose(1, 0, 3, 2, 4)

    lower = jnp.tril(jnp.ones((GLA_CHUNK, GLA_CHUNK), dtype=bool))[:, :, None]

    def step(state, inp):
        qc, kc, vc, gc = inp
        b = jnp.cumsum(gc, axis=2)
        rel = jnp.where(lower, b[:, :, :, None, :] - b[:, :, None, :, :], -jnp.inf)
        att = jnp.einsum('bhtd,bhsd,bhtsd->bhts', qc, kc, jnp.exp(rel))
        out = (jnp.einsum('bhtd,bhde->bhte', qc * jnp.exp(b), state)
               + jnp.einsum('bhts,bhse->bhte', att, vc))
        b_end = b[:, :, -1, :]
        new_state = (jnp.exp(b_end)[..., None] * state
                     + jnp.einsum('bhsd,bhse->bhde', kc * jnp.exp(b_end[:, :, None, :] - b), vc))
        return new_state, out

    s_fin, o = lax.scan(step, s0, (to_chunks(q), to_chunks(k), to_chunks(v), to_chunks(log_a)))
    o = o.transpose(1, 0, 3, 2, 4).reshape(bsz, seq, heads, v.shape[-1])
    return o.astype(v.dtype), s_fin


def gla_output(o, gate, g):
    bsz, seq = o.shape[0], o.shape[1]
    return head_rmsnorm(o, g).reshape(bsz, seq, GLA_V) * jax.nn.silu(gate)


def diff_attend(q, k, v, lam):
    bsz, n_q, heads = q.shape[0], q.shape[1], q.shape[2]
    scale = DIFF_DH ** -0.5

    def one_block(qb):
        s = jnp.einsum('bqhmd,bkhmd->bhmqk', qb, k).astype(jnp.float32) * scale
        p = jax.nn.softmax(s, axis=-1)
        a = (p[:, :, 0] - lam * p[:, :, 1]).astype(v.dtype)
        return jnp.einsum('bhqk,bkhe->bqhe', a, v)

    n_blocks = n_q // Q_BLOCK
    qs = q.reshape(bsz, n_blocks, Q_BLOCK, heads, 2, DIFF_DH).transpose(1, 0, 2, 3, 4, 5)
    o = lax.map(one_block, qs)
    return o.transpose(1, 0, 2, 3, 4).reshape(bsz, n_q, heads, v.shape[-1])


def diff_output(o, g, lam_init):
    bsz, seq = o.shape[0], o.shape[1]
    return head_rmsnorm(o, g).reshape(bsz, seq, DIFF_V) * (1.0 - lam_init)


def multiscale_pool(p, pool_w, pool_scale):
    bsz, seq, _ = p.shape
    pf = p.astype(jnp.float32)
    cs = jnp.concatenate([jnp.zeros((bsz, 1, POOL_W), jnp.float32), jnp.cumsum(pf, axis=1)], axis=1)
    t = jnp.arange(seq)
    outs = []
    for gi, w in enumerate(POOL_WINDOWS):
        lo = jnp.clip(t - w // 2, 0, seq)
        hi = jnp.clip(t + w - w // 2, 0, seq)
        sl = slice(gi * POOL_CH, (gi + 1) * POOL_CH)
        csg = cs[..., sl]
        mean = (csg[:, hi] - csg[:, lo]) / (hi - lo).astype(jnp.float32)[None, :, None]
        outs.append(jnp.einsum('blc,cd->bld', (mean - pf[..., sl]).astype(p.dtype), pool_w[gi]))
    return jnp.concatenate(outs, axis=-1) * pool_scale


def token_mixer(h_lat, h_ctx, w_in, w_out, wa2_f, ba_f, wa2_b, ba_b, gla_norm,
                lam_q1, lam_k1, lam_q2, lam_k2, diff_norm, pool_w, pool_scale,
                lam_init, rope_tabs, with_ctx_out):
    lq_g, lk_g, lv_g, lo_g, lla_f, lla_b, lq_d, lk_d, lv_d, lpl = mixer_inputs(h_lat, w_in, wa2_f, ba_f, wa2_b, ba_b)
    cq_g, ck_g, cv_g, co_g, cla_f, cla_b, cq_d, ck_d, cv_d, cpl = mixer_inputs(h_ctx, w_in, wa2_f, ba_f, wa2_b, ba_b)
    flip = functools.partial(jnp.flip, axis=1)

    s0 = jnp.zeros((h_lat.shape[0], GLA_HEADS, GLA_DK, GLA_DV), jnp.float32)
    oc_f, s_f = gla_chunked(cq_g, ck_g, cv_g, cla_f, s0)
    oc_b, s_b = gla_chunked(flip(cq_g), flip(ck_g), flip(cv_g), flip(cla_b), s0)
    ol_f, _ = gla_chunked(lq_g, lk_g, lv_g, lla_f, s_f)
    ol_b, _ = gla_chunked(flip(lq_g), flip(lk_g), flip(lv_g), flip(lla_b), s_b)

    f32 = jnp.float32
    lam = (jnp.exp(jnp.sum(lam_q1.astype(f32) * lam_k1.astype(f32)))
           - jnp.exp(jnp.sum(lam_q2.astype(f32) * lam_k2.astype(f32))) + lam_init)
    q_rot = apply_axial_rope(lq_d, rope_tabs)
    k_rot = apply_axial_rope(lk_d, rope_tabs)
    k_all = jnp.concatenate([ck_d, k_rot], axis=1)
    v_all = jnp.concatenate([cv_d, lv_d], axis=1)

    y_lat = jnp.concatenate([
        gla_output(ol_f + flip(ol_b), lo_g, gla_norm),
        diff_output(diff_attend(q_rot, k_all, v_all, lam), diff_norm, lam_init),
        multiscale_pool(lpl, pool_w, pool_scale),
    ], axis=-1) @ w_out
    if not with_ctx_out:
        return y_lat, None
    y_ctx = jnp.concatenate([
        gla_output(oc_f + flip(oc_b), co_g, gla_norm),
        diff_output(diff_attend(cq_d, ck_d, cv_d, lam), diff_norm, lam_init),
        multiscale_pool(cpl, pool_w, pool_scale),
    ], axis=-1) @ w_out
    return y_lat, y_ctx


def hier_moe(h, wg, bg, we, be, w1, w3, w2):
    n_tok, d = h.shape
    g_prob = jax.nn.softmax((h @ wg + bg).astype(jnp.float32), axis=-1)
    g_top, g_idx = lax.top_k(g_prob, 1)
    e_logit = (h @ we + be).astype(jnp.float32).reshape(n_tok, N_GROUPS, EXPERTS_PER_GROUP)
    e_prob = jax.nn.softmax(e_logit[jnp.arange(n_tok), g_idx[:, 0]], axis=-1)
    e_top, e_loc = lax.top_k(e_prob, TOP_K)
    weights = (g_top * e_top / jnp.sum(e_top, axis=-1, keepdims=True)).reshape(-1)
    expert = (g_idx * EXPERTS_PER_GROUP + e_loc).reshape(-1)
    token = jnp.repeat(jnp.arange(n_tok, dtype=jnp.int32), TOP_K)
    n_assign = n_tok * TOP_K

    order = jnp.argsort(expert)
    e_sorted = expert[order]
    counts = jnp.zeros((N_EXPERTS,), jnp.int32).at[expert].add(1)
    padded = (counts + MOE_BLOCK - 1) // MOE_BLOCK * MOE_BLOCK
    padded_end = jnp.cumsum(padded)
    rank = jnp.arange(n_assign, dtype=jnp.int32) - (jnp.cumsum(counts) - counts)[e_sorted]
    dest = (padded_end - padded)[e_sorted] + rank
    n_slots = -(-n_assign // MOE_BLOCK) * MOE_BLOCK + N_EXPERTS * MOE_BLOCK
    n_blocks = n_slots // MOE_BLOCK
    slot_tok = jnp.full((n_slots,), n_tok, jnp.int32).at[dest].set(token[order])
    slot_w = jnp.zeros((n_slots,), h.dtype).at[dest].set(weights[order].astype(h.dtype))
    block_expert = jnp.minimum(
        jnp.searchsorted(padded_end, jnp.arange(n_blocks, dtype=jnp.int32) * MOE_BLOCK, side='right'),
        N_EXPERTS - 1)
    h_pad = jnp.concatenate([h, jnp.zeros((1, d), h.dtype)], axis=0)
    xb = h_pad[slot_tok].reshape(n_blocks, MOE_BLOCK, d)

    def expert_block(args):
        xblk, e = args
        return (jax.nn.silu(xblk @ w1[e]) * (xblk @ w3[e])) @ w2[e]

    yb = lax.map(expert_block, (xb, block_expert)).reshape(n_slots, d)
    out = jnp.zeros((n_tok + 1, d), h.dtype).at[slot_tok].add(yb * slot_w[:, None])
    return out[:n_tok]


def setup_inputs(seed: int = 0) -> dict:
    key = jax.random.key(seed)
    ks = jax.random.split(key, 32)
    f32 = jnp.float32

    def nrm(k, shape, scale):
        return jax.random.normal(k, shape, f32) * scale

    def gain(k, shape):
        return 1.0 + 0.05 * jax.random.normal(k, shape, f32)

    D = D_MODEL
    return {
        "x": nrm(ks[0], (BATCH, SEQ, D), 1.0),
        "c": nrm(ks[1], (BATCH, D), 1.0),
        "ctx": nrm(ks[2], (BATCH, CTX_LEN, D), 1.0),
        "c_ctx": nrm(ks[3], (D,), 1.0),
        "w_mod": nrm(ks[4], (DEPTH, D, 6 * D), 0.5 * D ** -0.5),
        "b_mod": nrm(ks[5], (DEPTH, 6 * D), 0.02),
        "norm1": gain(ks[6], (DEPTH, D)),
        "norm2": gain(ks[7], (DEPTH, D)),
        "w_in": nrm(ks[8], (DEPTH, D, IN_COLS), D ** -0.5),
        "w_out": nrm(ks[9], (DEPTH, MIX_WIDTH, D), MIX_WIDTH ** -0.5),
        "gla_wa2_f": nrm(ks[10], (DEPTH, GATE_RANK, GLA_K), GATE_RANK ** -0.5),
        "gla_ba_f": nrm(ks[11], (DEPTH, GLA_K), 0.1),
        "gla_wa2_b": nrm(ks[12], (DEPTH, GATE_RANK, GLA_K), GATE_RANK ** -0.5),
        "gla_ba_b": nrm(ks[13], (DEPTH, GLA_K), 0.1),
        "gla_norm": gain(ks[14], (DEPTH, GLA_V)),
        "lam_q1": nrm(ks[15], (DEPTH, DIFF_DH), 0.1),
        "lam_k1": nrm(ks[16], (DEPTH, DIFF_DH), 0.1),
        "lam_q2": nrm(ks[17], (DEPTH, DIFF_DH), 0.1),
        "lam_k2": nrm(ks[18], (DEPTH, DIFF_DH), 0.1),
        "diff_norm": gain(ks[19], (DEPTH, DIFF_V)),
        "pool_w": nrm(ks[20], (DEPTH, len(POOL_WINDOWS), POOL_CH, POOL_CH), POOL_CH ** -0.5),
        "pool_scale": gain(ks[21], (DEPTH, POOL_W)),
        "router_wg": nrm(ks[22], (DEPTH, D, N_GROUPS), D ** -0.5),
        "router_bg": nrm(ks[23], (DEPTH, N_GROUPS), 0.01),
        "router_we": nrm(ks[24], (DEPTH, D, N_EXPERTS), D ** -0.5),
        "router_be": nrm(ks[25], (DEPTH, N_EXPERTS), 0.01),
        "exp_w1": nrm(ks[26], (DEPTH, N_EXPERTS, D, D_EXPERT), D ** -0.5),
        "exp_w3": nrm(ks[27], (DEPTH, N_EXPERTS, D, D_EXPERT), D ** -0.5),
        "exp_w2": nrm(ks[28], (DEPTH, N_EXPERTS, D_EXPERT, D), D_EXPERT ** -0.5),
        "final_norm": gain(ks[29], (D,)),
    }


def reference(x, c, ctx, c_ctx, w_mod, b_mod, norm1, norm2, w_in, w_out,
              gla_wa2_f, gla_ba_f, gla_wa2_b, gla_ba_b, gla_norm,
              lam_q1, lam_k1, lam_q2, lam_k2, diff_norm, pool_w, pool_scale,
              router_wg, router_bg, router_we, router_be, exp_w1, exp_w3, exp_w2, final_norm):
    bsz, n_lat, d = x.shape
    n_ctx = ctx.shape[1]
    rope_tabs = axial_rope_tables(n_lat)
    for layer in range(DEPTH):
        last = layer == DEPTH - 1
        lam_init = 0.8 - 0.6 * math.exp(-0.3 * layer)
        sh1, sc1, g1, sh2, sc2, g2 = adaln(c[:, None, :], w_mod[layer], b_mod[layer])
        csh1, csc1, cg1, csh2, csc2, cg2 = adaln(c_ctx, w_mod[layer], b_mod[layer])

        h_lat = modulate(x, norm1[layer], sh1, sc1)
        h_ctx = modulate(ctx, norm1[layer], csh1, csc1)
        y_lat, y_ctx = token_mixer(
            h_lat, h_ctx, w_in[layer], w_out[layer],
            gla_wa2_f[layer], gla_ba_f[layer], gla_wa2_b[layer], gla_ba_b[layer], gla_norm[layer],
            lam_q1[layer], lam_k1[layer], lam_q2[layer], lam_k2[layer], diff_norm[layer],
            pool_w[layer], pool_scale[layer], lam_init, rope_tabs, not last)
        x = x + g1 * y_lat

        moe_args = (router_wg[layer], router_bg[layer], router_we[layer], router_be[layer],
                    exp_w1[layer], exp_w3[layer], exp_w2[layer])
        h2 = modulate(x, norm2[layer], sh2, sc2).reshape(-1, d)
        if last:
            x = x + g2 * hier_moe(h2, *moe_args).reshape(bsz, n_lat, d)
        else:
            ctx = ctx + cg1 * y_ctx
            h2c = modulate(ctx, norm2[layer], csh2, csc2).reshape(-1, d)
            f = hier_moe(jnp.concatenate([h2c, h2], axis=0), *moe_args)
            ctx = ctx + cg2 * f[:bsz * n_ctx].reshape(bsz, n_ctx, d)
            x = x + g2 * f[bsz * n_ctx:].reshape(bsz, n_lat, d)
    return rmsnorm(x, final_norm)
```

```python
import contextlib
import math
import numpy as np
import concourse.bass as bass
import concourse.mybir as mybir
from concourse.bass_utils import run_bass_kernel_spmd

F32 = mybir.dt.float32
BF16 = mybir.dt.bfloat16
ALU = mybir.AluOpType
AF = mybir.ActivationFunctionType
AX = mybir.AxisListType

D = 1024
NB = 2
CTX = 256
GRID_W = 64
EPS = 1e-6
POOL_WINDOWS = (2, 4, 8, 16)
NEXP = 16
DEXP = 512


class TK:
    def __init__(self, nc, es, sync_same=True):
        self.nc = nc
        self.sync_same = sync_same
        self.E = {'pe': nc.tensor, 'dve': nc.vector, 'act': nc.scalar, 'pool': nc.gpsimd, 'sp': nc.sync}
        self.sem = {k: es.enter_context(nc.semaphore("s_" + k)) for k in ('pe', 'dve', 'act', 'pool')}
        self.cnt = {k: 0 for k in self.sem}
        self.waited = {}
        self.reg = {}
        self.NDS = 8
        self.dsem = {q: [es.enter_context(nc.semaphore("d_%s%d" % (q, i))) for i in range(self.NDS)]
                     for q in ('sp', 'pool')}
        self.dcnt = {q: [0] * self.NDS for q in self.dsem}
        self.dnext = {q: 0 for q in self.dsem}
        self.nops = 0

    def _semof(self, src):
        if isinstance(src, tuple):
            return self.dsem[src[1]][src[2]]
        return self.sem[src]

    def _wait(self, eng, src, val, raw):
        if src == eng:
            if eng == 'pe' or not raw or not self.sync_same:
                return
        key = (eng, src)
        if self.waited.get(key, 0) >= val:
            return
        self.waited[key] = val
        self.E[eng].wait_ge(self._semof(src), val)

    def _deps(self, eng, reads, writes):
        for k in reads:
            r = self.reg.get(k)
            if r is not None and r[0] is not None:
                self._wait(eng, r[0][0], r[0][1], True)
        for k in writes:
            r = self.reg.get(k)
            if r is not None:
                if r[0] is not None:
                    self._wait(eng, r[0][0], r[0][1], False)
                for s, v in r[1].items():
                    self._wait(eng, s, v, False)

    def _commit(self, src, val, reads, writes):
        for k in reads:
            r = self.reg.get(k)
            if r is None:
                r = [None, {}]
                self.reg[k] = r
            if r[1].get(src, 0) < val:
                r[1][src] = val
        for k in writes:
            self.reg[k] = [(src, val), {}]

    def op(self, eng, fn, reads=(), writes=()):
        pr = tuple(k for k in reads if isinstance(k, tuple) and k[0] == 'ps')
        if pr:
            writes = tuple(writes) + pr
        self._deps(eng, reads, writes)
        ins = fn(self.E[eng])
        self.cnt[eng] += 1
        ins.then_inc(self.sem[eng], 1)
        self._commit(eng, self.cnt[eng], reads, writes)
        self.nops += 1

    def dma(self, q, out, in_, reads=(), writes=()):
        i = self.dnext[q]
        self.dnext[q] = (i + 1) % self.NDS
        src = ('d', q, i)
        if self.dcnt[q][i] > 0:
            self._wait(q, src, self.dcnt[q][i], True)
        self._deps(q, reads, writes)
        ins = self.E[q].dma_start(out=out, in_=in_)
        self.dcnt[q][i] += 16
        ins.then_inc(self.dsem[q][i], 16)
        self._commit(src, self.dcnt[q][i], reads, writes)
        self.nops += 1

    def coll(self, kind, op, groups, in_ap, out_ap, reads=(), writes=()):
        q = 'pool'
        i = self.dnext[q]
        self.dnext[q] = (i + 1) % self.NDS
        src = ('d', q, i)
        if self.dcnt[q][i] > 0:
            self._wait(q, src, self.dcnt[q][i], True)
        self._deps(q, reads, writes)
        ins = self.nc.gpsimd.collective_compute(kind, op, replica_groups=groups, ins=[in_ap], outs=[out_ap])
        self.dcnt[q][i] += 16
        ins.then_inc(self.dsem[q][i], 16)
        self._commit(src, self.dcnt[q][i], reads, writes)

    def barrier(self):
        for e in ('pe', 'dve', 'act', 'pool', 'sp'):
            for s_ in self.sem:
                if s_ != e and self.cnt[s_] > 0:
                    self._wait(e, s_, self.cnt[s_], True)
            for q in self.dsem:
                for i in range(self.NDS):
                    if self.dcnt[q][i] > 0:
                        self._wait(e, ('d', q, i), self.dcnt[q][i], True)
        self.reg = {}

    def finish(self):
        for q in self.dsem:
            for i in range(self.NDS):
                if self.dcnt[q][i] > 0:
                    self._wait('sp', ('d', q, i), self.dcnt[q][i], True)
        for e in self.sem:
            if self.cnt[e] > 0:
                self._wait('sp', e, self.cnt[e], True)

    def mm(self, out, lhsT, rhs, start, stop, reads, writes):
        self.op('pe', lambda e: e.matmul(out, lhsT, rhs, start=start, stop=stop,
                                         skip_group_check=True), reads, writes)

    def act(self, out, in_, func, reads, writes, bias=None, scale=None, eng='act'):
        kw = {}
        if bias is not None:
            kw['bias'] = bias
        if scale is not None:
            kw['scale'] = scale
        self.op('act', lambda e: e.activation(out=out, in_=in_, func=func, **kw), reads, writes)

    def tt(self, eng, out, in0, in1, op, reads, writes):
        self.op(eng, lambda e: e.tensor_tensor(out=out, in0=in0, in1=in1, op=op), reads, writes)

    def ts(self, eng, out, in0, s1, op0, reads, writes, s2=None, op1=None):
        if op1 is None:
            self.op(eng, lambda e: e.tensor_scalar(out=out, in0=in0, scalar1=s1, scalar2=None, op0=op0),
                    reads, writes)
        else:
            self.op(eng, lambda e: e.tensor_scalar(out=out, in0=in0, scalar1=s1, scalar2=s2, op0=op0, op1=op1),
                    reads, writes)

    def stt(self, out, in0, scalar, in1, op0, op1, reads, writes):
        self.op('dve', lambda e: e.scalar_tensor_tensor(out=out, in0=in0, scalar=scalar, in1=in1,
                                                        op0=op0, op1=op1), reads, writes)

    def copy(self, eng, out, in_, reads, writes):
        if eng == 'act':
            self.op('act', lambda e: e.copy(out=out, in_=in_), reads, writes)
        else:
            self.op(eng, lambda e: e.tensor_copy(out=out, in_=in_), reads, writes)

    def memset(self, eng, ap, val, writes):
        self.op(eng, lambda e: e.memset(ap, val), (), writes)


def _sb(nc, es, name, shape, dt):
    return es.enter_context(nc.sbuf_tensor(name, list(shape), dt))


def emit_A(nc, tk, ps, L, dr, uid=""):
    C = CTX
    T = C + L
    NKT = T // 128
    ntl = L // 512
    tiles = [(0, C)] + [(C + 512 * i, 512) for i in range(ntl)]
    stop = None
    xT, cc, wmod, bmod, nrm1, w_h, wg = (dr[k] for k in ("xT", "cc", "wmod", "bmod", "nrm1", "w_h", "wg"))
    gnorm, dnorm, poolw, pscale, bandm = (dr[k] for k in ("gnorm", "dnorm", "poolw", "pscale", "bandm"))
    cosT, sinT, lamv, lamc, trif, trib, ident, ofT = (dr[k] for k in ("cosT", "sinT", "lamv", "lamc", "trif",
                                                                      "trib", "ident", "ofT"))
    mix_gla, mix_diff, mix_pool = dr["mix_gla"], dr["mix_diff"], dr["mix_pool"]

    es = contextlib.ExitStack()
    with es:
        sb = lambda name, shape, dt=F32: _sb(nc, es, name + uid, shape, dt)
        PS = lambda i: ("ps", i)

        KT = sb("KT", [128, T], BF16)
        V = sb("V", [128, NKT, 130], BF16)
        PL = sb("PL", [128, NKT, 64], BF16)
        wfm = sb("wfm", [128, 8, 960], BF16)
        xt = [sb("xt%d" % i, [128, 8, 512]) for i in range(2)]
        hT = sb("hT", [128, 8, 512], BF16)
        sq = [sb("sq%d" % i, [128, 512], BF16) for i in range(2)]
        rstd = sb("rstd", [128, 512])
        tmpf = [sb("tmpf%d" % i, [128, 512]) for i in range(2)]
        cst = sb("cst", [128, 512])
        snt = sb("snt", [128, 512])
        QT = sb("QT", [128, 512], BF16)
        PT = [sb("PT%d" % i, [128, 512], BF16) for i in range(4)]
        onesb = sb("onesb", [128, 128], BF16)
        ones64 = sb("ones64", [64, 64], BF16)
        identS = sb("identS", [128, 128])
        trifS = sb("trifS", [128, 128])
        tribS = sb("tribS", [128, 128])
        trifN = sb("trifN", [128, 128])
        tribN = sb("tribN", [128, 128])
        band = sb("band", [128, 5, 128], BF16)
        bandf = sb("bandf", [128, 5, 128])
        wgS = sb("wgS", [33, 64])
        gnS = sb("gnS", [64, 1])
        dnS = sb("dnS", [128, 1])
        dnS2 = sb("dnS2", [128, 1])
        pwf = sb("pwf", [64, 64])
        pwS = sb("pwS", [64, 64], BF16)
        pscS = sb("pscS", [64, 1])
        lamS = sb("lamS", [128, 4, 64])
        lamcS = sb("lamcS", [128, 2])
        lamt = sb("lamt", [128, 2, 64])
        lamr = sb("lamr", [128, 4])
        nlam = sb("nlam", [128, 1])
        ccS = sb("ccS", [128, 16])
        scT = sb("scT", [128, 16])
        bmS = sb("bmS", [128, 16])
        n1S = sb("n1S", [128, 8])
        modS = sb("modS", [128, 32])
        Amod = sb("Amod", [128, 2, 8])
        Smod = sb("Smod", [128, 2, 8])
        G2 = sb("G2", [33, 512])
        qgT = sb("qgT", [32, 512])
        kgT = sb("kgT", [32, 512])
        ogT = sb("ogT", [64, 512])
        ktm = sb("ktm", [128, 128])
        vtm = sb("vtm", [128, 256], BF16)
        gz = sb("gz", [128, 256])
        gg = sb("gg", [128, 256])
        ebT = sb("ebT", [32, 512])
        enbT = sb("enbT", [32, 512])
        enb = sb("enb", [128, 128])
        qtl = sb("qtl", [32, 512], BF16)
        ktl = sb("ktl", [32, 512], BF16)
        ktlm = sb("ktlm", [128, 128], BF16)
        attm = sb("attm", [128, 512], BF16)
        Sst = sb("Sst", [32, 64])
        Sbf = sb("Sbf", [32, 64], BF16)
        Stmp = sb("Stmp", [32, 64])
        Ust = sb("Ust", [32, 256])
        oT = sb("oT", [64, 512])
        ofl = sb("ofl", [64, 512])
        osq = sb("osq", [64, 512], BF16)
        pdif = sb("pdif", [64, 128], BF16)
        o1 = sb("o1", [128, 128])
        o2 = sb("o2", [128, 128])
        rc = sb("rc", [128, 2])
        osq2 = sb("osq2", [128, 128])
        ss = sb("ss", [128, 1])
        dout = sb("dout", [128, 512])

        grs, gsg, gout, pout = rstd, cst, snt, tmpf[0]
        xT3 = xT.rearrange("(k p) t -> p k t", p=128)

        def ld(q, dst, src, key):
            tk.dma(q, dst, src, (), (key,))
        ld('sp', identS[:], ident, "identS")
        ld('sp', trifS[:], trif, "trifS")
        ld('sp', tribS[:], trib, "tribS")
        ld('sp', wgS[:], wg, "wgS")
        ld('sp', gnS[:], gnorm, "gnS")
        ld('sp', dnS[:], dnorm, "dnS")
        ld('sp', pwf[:], poolw, "pwf")
        ld('sp', pscS[:], pscale, "pscS")
        ld('sp', lamS[:], lamv, "lamS")
        ld('sp', lamcS[:], lamc, "lamcS")
        ld('sp', ccS[:], cc.rearrange("p k c -> p (k c)"), "ccS")
        ld('sp', bmS[:], bmod, "bmS")
        ld('sp', n1S[:], nrm1, "n1S")
        ld('sp', bandf[:], bandm.rearrange("b s t -> s b t"), "bandf")
        if stop == 0.1:
            tk.finish()
            return nc
        tk.memset('dve', onesb[:], 1.0 / D, ("onesb",))
        tk.memset('dve', ones64[:], 1.0 / 64, ("ones64",))
        tk.memset('dve', G2[:], 1.0, ("G2",))
        tk.memset('pool', V[:], 1.0, ("Vinit",))
        if stop == 0.2:
            tk.finish()
            return nc
        tk.ts('dve', trifN[:], trifS[:], -1.0 / 16, ALU.mult, ("trifS",), ("trifN",))
        tk.ts('dve', tribN[:], tribS[:], -1.0 / 16, ALU.mult, ("tribS",), ("tribN",))
        tk.copy('dve', band[:], bandf[:], ("bandf",), ("band",))
        tk.copy('dve', pwS[:], pwf[:], ("pwf",), ("pwS",))

        if stop == 0.3:
            tk.finish()
            return nc
        tk.tt('dve', lamt[:, 0, :], lamS[:, 0, :], lamS[:, 1, :], ALU.mult, ("lamS",), ("lamt",))
        tk.tt('dve', lamt[:, 1, :], lamS[:, 2, :], lamS[:, 3, :], ALU.mult, ("lamS", "lamt"), ("lamt",))
        if stop == 0.4:
            tk.finish()
            return nc
        tk.op('dve', lambda e: e.reduce_sum(out=lamr[:, 0:2], in_=lamt[:], axis=AX.X), ("lamt",), ("lamr",))
        if stop == 0.5:
            tk.finish()
            return nc
        tk.act(lamr[:, 2:4], lamr[:, 0:2], AF.Exp, ("lamr",), ("lamr2",))
        if stop == 0.6:
            tk.finish()
            return nc
        tk.tt('dve', nlam[:], lamr[:, 3:4], lamr[:, 2:3], ALU.subtract, ("lamr2",), ("nlam",))
        if stop == 0.7:
            tk.finish()
            return nc
        tk.tt('dve', nlam[:], nlam[:], lamcS[:, 0:1], ALU.subtract, ("nlam", "lamcS"), ("nlam",))
        if stop == 0.8:
            tk.finish()
            return nc
        tk.tt('dve', dnS2[:], dnS[:], lamcS[:, 1:2], ALU.mult, ("dnS", "lamcS"), ("dnS2",))

        if stop == 1:
            tk.finish()
            return nc
        tk.act(scT[:], ccS[:], AF.Exp, ("ccS",), ("scT",), scale=-1.0)
        tk.ts('dve', scT[:], scT[:], 1.0, ALU.add, ("scT",), ("scT",))
        tk.op('dve', lambda e: e.reciprocal(out=scT[:], in_=scT[:]), ("scT",), ("scT",))
        tk.tt('dve', scT[:], scT[:], ccS[:], ALU.mult, ("scT", "ccS"), ("scT",))
        wst = xt[0]
        wmod3 = wmod.rearrange("(k p) n -> p k n", p=128)
        for blk in range(4):
            tk.dma('sp', wst[:], wmod3[:, :, blk * 512:(blk + 1) * 512], (), ("xt0",))
            for jj in range(4):
                j = blk * 4 + jj
                for k in range(8):
                    tk.mm(ps[0][:, 2 * j:2 * j + 2], wst[:, k, jj * 128:(jj + 1) * 128],
                          scT[:, 2 * k:2 * k + 2], k == 0, k == 7, ("xt0", "scT"), (PS(0),))
        tk.copy('dve', modS[:], ps[0][:, 0:32], (PS(0),), ("modS",))
        for c in range(2):
            sh_v = modS[:, c:16:2]
            sc_v = modS[:, 16 + c:32:2]
            tk.tt('dve', Smod[:, c, :], sh_v, bmS[:, 0:8], ALU.add, ("modS", "bmS"), ("Smod",))
            tk.tt('dve', Amod[:, c, :], sc_v, bmS[:, 8:16], ALU.add, ("modS", "bmS"), ("Amod",))
            tk.ts('dve', Amod[:, c, :], Amod[:, c, :], 1.0, ALU.add, ("Amod",), ("Amod",))
            tk.tt('dve', Amod[:, c, :], Amod[:, c, :], n1S[:], ALU.mult, ("Amod", "n1S"), ("Amod",))

        if stop == 2:
            tk.finish()
            return nc
        w_h3 = w_h.rearrange("(k p) n -> p k n", p=128)
        for k in range(8):
            st = xt[1]
            tk.dma('sp', st[:, 0, :], w_h3[:, k, 0:512], (), ("xt1",))
            tk.dma('sp', st[:, 1, 0:448], w_h3[:, k, 512:960], (), ("xt1",))
            tk.copy('dve', wfm[:, k, 0:512], st[:, 0, :], ("xt1",), ("wfm",))
            tk.copy('pool', wfm[:, k, 512:960], st[:, 1, 0:448], ("xt1",), ("wfm",))

        if stop == 3:
            tk.finish()
            return nc
        def load_x(ti, buf):
            s, w = tiles[ti]
            tk.dma('sp', xt[buf][:, :, 0:w], xT3[:, :, s:s + w], (), ("xt%d" % buf,))

        def norm_tile(ti, buf):
            s, w = tiles[ti]
            c = 1 if ti == 0 else 0
            xk = "xt%d" % buf
            x_ = xt[buf]
            for k in range(8):
                tk.tt('pool', sq[k % 2][:, 0:w], x_[:, k, 0:w], x_[:, k, 0:w], ALU.mult, (xk,), ("sq%d" % (k % 2),))
                tk.mm(ps[7][:, 0:w], onesb[:], sq[k % 2][:, 0:w], k == 0, k == 7, ("onesb", "sq%d" % (k % 2)), (PS(7),))
            tk.ts('dve', rstd[:, 0:w], ps[7][:, 0:w], EPS, ALU.add, (PS(7),), ("rstd",))
            tk.act(rstd[:, 0:w], rstd[:, 0:w], AF.Ln, ("rstd",), ("rstd",))
            tk.act(rstd[:, 0:w], rstd[:, 0:w], AF.Exp, ("rstd",), ("rstd",), scale=-0.5)
            for k in range(8):
                tf = tmpf[k % 2]
                tfk = "tmpf%d" % (k % 2)
                tk.tt('dve', tf[:, 0:w], x_[:, k, 0:w], rstd[:, 0:w], ALU.mult, (xk, "rstd"), (tfk,))
                tk.act(hT[:, k, 0:w], tf[:, 0:w], AF.Identity, (tfk, "Amod", "Smod"), ("hT",),
                       bias=Smod[:, c, k:k + 1], scale=Amod[:, c, k:k + 1])

        def fm_proj(col0, M, w, bank):
            for k in range(8):
                tk.mm(ps[bank][0:M, 0:w], wfm[:, k, col0:col0 + M], hT[:, k, 0:w], k == 0, k == 7,
                      ("wfm", "hT"), (PS(bank),))

        def load_rope(ti):
            s, w = tiles[ti]
            tk.dma('sp', cst[:, 0:w], cosT[:, s:s + w], (), ("cst",))
            tk.dma('sp', snt[:, 0:w], sinT[:, s:s + w], (), ("snt",))

        def rope_from(bankA, bankB, w, out_ap, out_key):
            r1, r2 = tmpf[0], tmpf[1]
            tk.tt('dve', r1[:, 0:w], ps[bankA][:, 0:w], cst[:, 0:w], ALU.mult, (PS(bankA), "cst"), ("tmpf0",))
            tk.tt('dve', r2[:, 0:w], ps[bankB][:, 0:w], snt[:, 0:w], ALU.mult, (PS(bankB), "snt"), ("tmpf1",))
            tk.tt('pool', out_ap, r1[:, 0:w], r2[:, 0:w], ALU.add, ("tmpf0", "tmpf1"), (out_key,))

        def gla_proj(w):
            fm_proj(512, 32, w, 2)
            tk.copy('act', qgT[:, 0:w], ps[2][0:32, 0:w], (PS(2),), ("qgT",))
            fm_proj(544, 32, w, 3)
            tk.copy('act', kgT[:, 0:w], ps[3][0:32, 0:w], (PS(3),), ("kgT",))
            fm_proj(576, 64, w, 2)
            tk.copy('act', ogT[:, 0:w], ps[2][0:64, 0:w], (PS(2),), ("ogT",))
            fm_proj(640, 32, w, 3)
            tk.copy('act', G2[0:32, 0:w], ps[3][0:32, 0:w], (PS(3),), ("G2",))

        def tm_proj(j, ncol):
            for k in range(8):
                tk.mm(ps[4][:, 0:ncol], hT[:, k, j * 128:(j + 1) * 128], wfm[:, k, 672:672 + ncol],
                      k == 0, k == 7, ("hT", "wfm"), (PS(4),))

        gla_first = [True, True]

        def gla_tile(w, nsub, fwd):
            triN, triK = ("trifN", "trifS") if fwd else ("tribN", "tribS")
            triNt = trifN if fwd else tribN
            triM = trifS if fwd else tribS
            g0 = 0 if fwd else 32
            di = 0 if fwd else 1
            W2, W3 = nsub * 64, nsub * 32
            cs = [slice(j * 128, (j + 1) * 128) for j in range(nsub)]
            for j in range(nsub):
                tk.mm(ps[5][:, j * 64:(j + 1) * 64], G2[0:33, cs[j]], wgS[:], True, True, ("G2", "wgS"), (PS(5),))
            tk.act(gz[:, 0:W2], ps[5][:, 0:W2], AF.Exp, (PS(5),), ("gz",), scale=-1.0)
            tk.ts('dve', gz[:, 0:W2], gz[:, 0:W2], 1.0, ALU.add, ("gz",), ("gz",))
            tk.act(gg[:, 0:W2], gz[:, 0:W2], AF.Ln, ("gz",), ("gg",))
            for j in range(nsub):
                tk.mm(ps[5][:, 256 + j * 32:256 + (j + 1) * 32], triNt[:], gg[:, j * 64 + g0:j * 64 + g0 + 32],
                      True, True, (triN, "gg"), (PS(5),))
            for j in range(nsub):
                tk.mm(ps[6][0:32, cs[j]], gg[:, j * 64 + g0:j * 64 + g0 + 32], triNt[:], True, True,
                      (triN, "gg"), (PS(6),))
            tk.act(enb[:, 0:W3], ps[5][:, 256:256 + W3], AF.Exp, (PS(5),), ("enb",), scale=-1.0)
            tk.act(ebT[:, 0:w], ps[6][0:32, 0:w], AF.Exp, (PS(6),), ("ebT",))
            tk.act(enbT[:, 0:w], ps[6][0:32, 0:w], AF.Exp, (PS(6),), ("enbT",), scale=-1.0)
            tk.stt(qtl[:, 0:w], qgT[:, 0:w], 32 ** -0.5, ebT[:, 0:w], ALU.mult, ALU.mult, ("qgT", "ebT"), ("qtl",))
            tk.tt('dve', ktl[:, 0:w], kgT[:, 0:w], enbT[:, 0:w], ALU.mult, ("kgT", "enbT"), ("ktl",))
            tk.tt('dve', ktlm[:, 0:W3], ktm[:, 0:W3], enb[:, 0:W3], ALU.mult, ("ktm", "enb"), ("ktlm",))
            for j in range(nsub):
                tk.mm(ps[4][:, cs[j]], ktl[:, cs[j]], qtl[:, cs[j]], True, True, ("ktl", "qtl"), (PS(4),))
            for j in range(nsub):
                tk.tt('dve', attm[:, cs[j]], ps[4][:, cs[j]], triM[:], ALU.mult, (PS(4), triK), ("attm",))
            for j in range(nsub):
                tk.mm(ps[2][0:32, j * 64:(j + 1) * 64], ktlm[:, j * 32:(j + 1) * 32], vtm[:, j * 64:(j + 1) * 64],
                      True, True, ("ktlm", "vtm"), (PS(2),))
            tk.copy('act', Ust[:, 0:W2], ps[2][0:32, 0:W2], (PS(2),), ("Ust",))
            for j in (range(nsub) if fwd else range(nsub - 1, -1, -1)):
                first = gla_first[di]
                gla_first[di] = False
                tk.mm(ps[3][0:64, cs[j]], vtm[:, j * 64:(j + 1) * 64], attm[:, cs[j]], True, first,
                      ("vtm", "attm"), (PS(3),))
                if not first:
                    tk.mm(ps[3][0:64, cs[j]], Sbf[:], qtl[:, cs[j]], False, True, ("Sbf", "qtl"), (PS(3),))
                eend = ebT[:, j * 128 + 127:j * 128 + 128] if fwd else ebT[:, j * 128:j * 128 + 1]
                Uj = Ust[:, j * 64:(j + 1) * 64]
                if first:
                    tk.ts('dve', Sst[:], Uj, eend, ALU.mult, ("Ust", "ebT"), ("Sst",))
                else:
                    tk.tt('dve', Stmp[:], Uj, Sst[:], ALU.add, ("Ust", "Sst"), ("Stmp",))
                    tk.ts('dve', Sst[:], Stmp[:], eend, ALU.mult, ("Stmp", "ebT"), ("Sst",))
                tk.copy('dve', Sbf[:], Sst[:], ("Sst",), ("Sbf",))
            tk.copy('act', oT[:, 0:w], ps[3][0:64, 0:w], (PS(3),), ("oT",))

        def kt_of(ti):
            s, w = tiles[ti]
            return s // 128, w // 128

        first = True
        load_x(0, 0)
        for ti in range(len(tiles)):
            s, w = tiles[ti]
            buf = ti % 2
            if ti + 1 < len(tiles):
                load_x(ti + 1, 1 - buf)
            load_rope(ti)
            norm_tile(ti, buf)
            if stop == 4:
                tk.finish()
                return nc
            kt0, nsub = kt_of(ti)
            fm_proj(256, 128, w, 0)
            fm_proj(384, 128, w, 1)
            rope_from(0, 1, w, KT[:, s:s + w], ("KT", ti))
            if stop == 4.1:
                tk.finish()
                return nc
            gla_proj(w)
            if stop == 4.2:
                tk.finish()
                return nc
            for j in range(nsub):
                tm_proj(j, 288)
                if stop == 4.21:
                    tk.finish()
                    return nc
                tk.copy('act', ktm[:, j * 32:(j + 1) * 32], ps[4][:, 0:32], (PS(4),), ("ktm",))
                if stop == 4.22:
                    tk.finish()
                    return nc
                tk.copy('act', vtm[:, j * 64:(j + 1) * 64], ps[4][:, 32:96], (PS(4),), ("vtm",))
                if stop == 4.23:
                    tk.finish()
                    return nc
                tk.copy('dve', V[:, kt0 + j, 0:128], ps[4][:, 96:224], (PS(4), "Vinit"), (("V", ti),))
                if stop == 4.24:
                    tk.finish()
                    return nc
                tk.copy('act', PL[:, kt0 + j, :], ps[4][:, 224:288], (PS(4),), (("PL", kt0 + j),))
            if stop == 4.3:
                tk.finish()
                return nc
            gla_tile(w, nsub, True)
            if stop == 4.4:
                tk.finish()
                return nc
            tk.dma('pool', ofT[:, s:s + w], oT[:, 0:w], ("oT",), (("ofT", ti),))
            if stop == 5:
                tk.finish()
                return nc

        if stop == 6:
            tk.finish()
            return nc
        for ti in range(len(tiles)):
            s, w = tiles[ti]
            kt0, nsub = kt_of(ti)
            for j in range(nsub):
                kt = kt0 + j
                if kt < 2:
                    i, n, base = kt, 2, 0
                else:
                    i, n, base = kt - 2, NKT - 2, 2
                parts = []
                if i > 0:
                    parts.append((kt - 1, 0))
                parts.append((kt, 3 if i == 0 else (4 if i == n - 1 else 1)))
                if i < n - 1:
                    parts.append((kt + 1, 2))
                for pi, (skt, bi) in enumerate(parts):
                    tk.mm(ps[0][0:64, 0:128], PL[:, skt, :], band[:, bi, :], pi == 0, pi == len(parts) - 1,
                          (("PL", skt), "band"), (PS(0),))
                tk.copy('act', pdif[:], ps[0][0:64, 0:128], (PS(0),), ("pdif",))
                tk.mm(ps[1][0:64, 0:128], pwS[:], pdif[:], True, True, ("pwS", "pdif"), (PS(1),))
                tk.ts('dve', pout[0:64, j * 128:(j + 1) * 128], ps[1][0:64, 0:128], pscS[:], ALU.mult,
                      (PS(1), "pscS"), ("tmpf0",))
            tk.dma('pool', mix_pool[:, s:s + w], pout[0:64, 0:w], ("tmpf0",), ())

        if stop == 7:
            tk.finish()
            return nc
        order = [0] + list(range(len(tiles) - 1, 0, -1))
        first = True
        load_x(order[0], 0)
        for oi, ti in enumerate(order):
            s, w = tiles[ti]
            buf = oi % 2
            if oi + 1 < len(order):
                load_x(order[oi + 1], 1 - buf)
            load_rope(ti)
            tk.dma('sp', ofl[:, 0:w], ofT[:, s:s + w], (("ofT", ti),), ("ofl",))
            norm_tile(ti, buf)
            kt0, nsub = kt_of(ti)
            fm_proj(0, 128, w, 0)
            fm_proj(128, 128, w, 1)
            rope_from(0, 1, w, QT[:, 0:w], "QT")
            gla_proj(w)
            for j in range(nsub):
                tm_proj(j, 96)
                tk.copy('act', ktm[:, j * 32:(j + 1) * 32], ps[4][:, 0:32], (PS(4),), ("ktm",))
                tk.copy('act', vtm[:, j * 64:(j + 1) * 64], ps[4][:, 32:96], (PS(4),), ("vtm",))
            gla_tile(w, nsub, False)
            tk.tt('dve', oT[:, 0:w], oT[:, 0:w], ofl[:, 0:w], ALU.add, ("oT", "ofl"), ("oT",))
            tk.tt('pool', osq[:, 0:w], oT[:, 0:w], oT[:, 0:w], ALU.mult, ("oT",), ("osq",))
            tk.mm(ps[5][0:64, 0:w], ones64[:], osq[:, 0:w], True, True, ("ones64", "osq"), (PS(5),))
            tk.ts('dve', grs[0:64, 0:w], ps[5][0:64, 0:w], EPS, ALU.add, (PS(5),), ("rstd",))
            tk.act(grs[0:64, 0:w], grs[0:64, 0:w], AF.Ln, ("rstd",), ("rstd",))
            tk.act(grs[0:64, 0:w], grs[0:64, 0:w], AF.Exp, ("rstd",), ("rstd",), scale=-0.5)
            tk.act(gsg[0:64, 0:w], ogT[:, 0:w], AF.Exp, ("ogT",), ("cst",), scale=-1.0)
            tk.ts('dve', gsg[0:64, 0:w], gsg[0:64, 0:w], 1.0, ALU.add, ("cst",), ("cst",))
            tk.op('dve', lambda e: e.reciprocal(out=gsg[0:64, 0:w], in_=gsg[0:64, 0:w]), ("cst",), ("cst",))
            tk.tt('dve', gsg[0:64, 0:w], gsg[0:64, 0:w], ogT[:, 0:w], ALU.mult, ("cst", "ogT"), ("cst",))
            tk.stt(gout[0:64, 0:w], oT[:, 0:w], gnS[:], grs[0:64, 0:w], ALU.mult, ALU.mult,
                   ("oT", "gnS", "rstd"), ("snt",))
            tk.tt('dve', gout[0:64, 0:w], gout[0:64, 0:w], gsg[0:64, 0:w], ALU.mult, ("snt", "cst"), ("snt",))
            tk.dma('pool', mix_gla[:, s:s + w], gout[0:64, 0:w], ("snt",), ())

            if stop == 8:
                tk.finish()
                return nc
            nkt = 2 if ti == 0 else NKT
            accb = [1, 2, 3]
            sb_rot = [0, 6, 7]
            touched = set()
            sbk = [0, 5, 6, 7]
            LA = 1

            def emit_qk(kt):
                ktile = 0 if kt < 2 else 1 + (kt - 2) // 4
                for m in range(2):
                    bk = sbk[2 * (kt % 2) + m]
                    tk.mm(ps[bk][:, 0:w], KT[64 * m:64 * m + 64, kt * 128:(kt + 1) * 128],
                          QT[64 * m:64 * m + 64, 0:w], True, True, (("KT", ktile), "QT"), (PS(bk),))

            def emit_exp_pv(kt):
                ktile = 0 if kt < 2 else 1 + (kt - 2) // 4
                for m in range(2):
                    bk = sbk[2 * (kt % 2) + m]
                    pi = 2 * (kt % 2) + m
                    tk.act(PT[pi][:, 0:w], ps[bk][:, 0:w], AF.Exp, (PS(bk),), ("PT%d" % pi,), scale=0.125)
                for m in range(2):
                    pi = 2 * (kt % 2) + m
                    for j in range(nsub):
                        a = m * 4 + j
                        bank = accb[a // 3]
                        c0 = (a % 3) * 130
                        st = bank not in touched
                        touched.add(bank)
                        tk.mm(ps[bank][:, c0:c0 + 129], PT[pi][:, j * 128:(j + 1) * 128], V[:, kt, 0:129],
                              st, kt == nkt - 1, ("PT%d" % pi, ("V", ktile), "Vinit"), (PS(bank),))

            for i in range(nkt + LA):
                if i < nkt:
                    emit_qk(i)
                if i >= LA:
                    emit_exp_pv(i - LA)
            for j in range(nsub):
                a1, a2 = j, 4 + j
                b1, c1 = accb[a1 // 3], (a1 % 3) * 130
                b2, c2 = accb[a2 // 3], (a2 % 3) * 130
                tk.op('dve', lambda e: e.reciprocal(out=rc[:, 0:1], in_=ps[b1][:, c1 + 128:c1 + 129]),
                      (PS(b1),), ("rc",))
                tk.op('dve', lambda e: e.reciprocal(out=rc[:, 1:2], in_=ps[b2][:, c2 + 128:c2 + 129]),
                      (PS(b2),), ("rc",))
                tk.tt('dve', rc[:, 1:2], rc[:, 1:2], nlam[:], ALU.mult, ("rc", "nlam"), ("rc",))
                tk.ts('dve', o1[:], ps[b1][:, c1:c1 + 128], rc[:, 0:1], ALU.mult, (PS(b1), "rc"), ("o1",))
                tk.stt(o2[:], ps[b2][:, c2:c2 + 128], rc[:, 1:2], o1[:], ALU.mult, ALU.add,
                       (PS(b2), "rc", "o1"), ("o2",))
                tk.tt('pool', osq2[:], o2[:], o2[:], ALU.mult, ("o2",), ("osq2",))
                tk.op('dve', lambda e: e.reduce_sum(out=ss[:], in_=osq2[:], axis=AX.X), ("osq2",), ("ss",))
                tk.ts('dve', ss[:], ss[:], 1.0 / 128, ALU.mult, ("ss",), ("ss",), s2=EPS, op1=ALU.add)
                tk.act(ss[:], ss[:], AF.Ln, ("ss",), ("ss",))
                tk.act(ss[:], ss[:], AF.Exp, ("ss",), ("ss",), scale=-0.5)
                tk.ts('dve', o1[:], o2[:], ss[:], ALU.mult, ("o2", "ss"), ("o1",))
                tk.op('pe', lambda e: e.transpose(ps[4][:, 0:128], o1[:], identS[:]), ("o1", "identS"), (PS(4),))
                tk.ts('dve', dout[:, j * 128:(j + 1) * 128], ps[4][:, 0:128], dnS2[:], ALU.mult,
                      (PS(4), "dnS2"), ("dout",))
            tk.dma('pool', mix_diff[:, s:s + w], dout[:, 0:w], ("dout",), ())
            if stop == 9:
                tk.finish()
                return nc
        tk.barrier()


def build_A(L):
    C = CTX
    T = C + L
    nc = bass.Bass("TRN2", target_bir_lowering=False)

    def din(name, shape, dt=F32):
        return nc.dram_tensor(name, list(shape), dt, kind="ExternalInput").ap()

    dr = {"xT": din("xT", [D, T]), "cc": din("cc", [128, 8, 2]), "wmod": din("wmod", [D, 2048]),
          "bmod": din("bmod", [128, 16]), "nrm1": din("nrm1", [128, 8]), "w_h": din("w_h", [D, 960]),
          "wg": din("wg", [33, 64]), "gnorm": din("gnorm", [64, 1]), "dnorm": din("dnorm", [128, 1]),
          "poolw": din("poolw", [64, 64]), "pscale": din("pscale", [64, 1]), "bandm": din("bandm", [5, 128, 128]),
          "cosT": din("cosT", [128, T]), "sinT": din("sinT", [128, T]), "lamv": din("lamv", [128, 4, 64]),
          "lamc": din("lamc", [128, 2]), "trif": din("trif", [128, 128]), "trib": din("trib", [128, 128]),
          "ident": din("ident", [128, 128])}
    mixT = nc.dram_tensor("mixT", [256, T], F32, kind="ExternalOutput").ap()
    dr["ofT"] = nc.dram_tensor("ofT", [64, T], F32, kind="Internal").ap()
    dr["mix_gla"], dr["mix_diff"], dr["mix_pool"] = mixT[0:64, :], mixT[64:192, :], mixT[192:256, :]
    es = contextlib.ExitStack()
    with es:
        tk = TK(nc, es)
        ps = [es.enter_context(nc.psum_tensor("ps%d" % i, [128, 512], F32)) for i in range(8)]
        emit_A(nc, tk, ps, L, dr)
        tk.finish()
    return nc


def _rope_tables(L):
    ax = 32
    t = np.arange(L)
    row = (t // GRID_W).astype(np.float32)
    col = (t % GRID_W).astype(np.float32)
    inv = (1.0 / (10000.0 ** (np.arange(0, ax, 2, dtype=np.float32) / ax))).astype(np.float32)
    ang_r = row[:, None] * inv[None, :]
    ang_c = col[:, None] * inv[None, :]
    cos64 = np.zeros((64, L), np.float32)
    sin64 = np.zeros((64, L), np.float32)
    for seg, ang in enumerate((ang_r, ang_c)):
        c = np.cos(ang).astype(np.float32).T
        s = np.sin(ang).astype(np.float32).T
        cos64[seg * 32:seg * 32 + 16] = c
        cos64[seg * 32 + 16:seg * 32 + 32] = c
        sin64[seg * 32:seg * 32 + 16] = -s
        sin64[seg * 32 + 16:seg * 32 + 32] = s
    cosT = np.concatenate([np.ones((64, CTX), np.float32), cos64], axis=1)
    sinT = np.concatenate([np.zeros((64, CTX), np.float32), sin64], axis=1)
    return (np.ascontiguousarray(np.concatenate([cosT, cosT], 0)),
            np.ascontiguousarray(np.concatenate([sinT, sinT], 0)))


def _rope_perm():
    p = np.zeros(64, np.int64)
    for seg in range(2):
        for i in range(32):
            p[seg * 32 + i] = seg * 32 + (i + 16) % 32
    return p


def _band_mats(win):
    n = 128 * 4
    seq = n
    A = np.zeros((seq, seq), np.float64)
    for t in range(seq):
        lo = max(t - win // 2, 0)
        hi = min(t + win - win // 2, seq)
        A[t, lo:hi] = 1.0 / (hi - lo)
    M = (A - np.eye(seq)).T
    out = np.zeros((5, 128, 128), np.float32)
    out[0] = M[128:256, 256:384]
    out[1] = M[256:384, 256:384]
    out[2] = M[384:512, 256:384]
    out[3] = M[0:128, 0:128]
    out[4] = M[384:512, 384:512]
    return out


def _chunks(v):
    return np.ascontiguousarray(np.asarray(v, np.float32).reshape(-1, 128).T)


_IN_OFF = np.cumsum([0, 128, 128, 256, 256, 16, 16, 512, 512, 512, 256])


def prep_A(p, layer, XT):
    T = XT[0].shape[1]
    L = T - CTX
    cosT, sinT = _rope_tables(L)
    perm = _rope_perm()
    perm2 = np.concatenate([perm, 64 + perm])
    lam_init = 0.8 - 0.6 * math.exp(-0.3 * layer)
    w_in = np.asarray(p['w_in'][layer], np.float32)
    o_qg, o_kg, o_vg, o_og, o_af, o_ab, o_qd, o_kd, o_vd, o_pl = [int(v) for v in _IN_OFF[:10]]
    trif = np.triu(np.ones((128, 128), np.float32))
    trib = np.tril(np.ones((128, 128), np.float32))
    ident = np.eye(128, dtype=np.float32)
    lamv = np.stack([p['lam_q1'][layer], p['lam_k1'][layer], p['lam_q2'][layer], p['lam_k2'][layer]], 0)
    lamv = np.ascontiguousarray(np.broadcast_to(lamv[None], (128, 4, 64)).astype(np.float32))
    lamc = np.ascontiguousarray(np.broadcast_to(np.array([[lam_init, 1.0 - lam_init]], np.float32), (128, 2)))
    wmod = np.ascontiguousarray(p['w_mod'][layer][:, 0:2048])
    bmod = np.ascontiguousarray(np.asarray(p['b_mod'][layer][:2048], np.float32).reshape(16, 128).T)
    nrm1 = _chunks(p['norm1'][layer])
    maps = []
    for b in range(NB):
        cc = np.ascontiguousarray(np.stack([_chunks(p['c'][b]), _chunks(p['c_ctx'])], axis=2))
        for h in range(4):
            qd = w_in[:, o_qd + h * 128:o_qd + (h + 1) * 128]
            kd = w_in[:, o_kd + h * 128:o_kd + (h + 1) * 128]
            qg = w_in[:, o_qg + h * 32:o_qg + (h + 1) * 32]
            kg = w_in[:, o_kg + h * 32:o_kg + (h + 1) * 32]
            vg = w_in[:, o_vg + h * 64:o_vg + (h + 1) * 64]
            og = w_in[:, o_og + h * 64:o_og + (h + 1) * 64]
            af = w_in[:, o_af:o_af + 16]
            ab = w_in[:, o_ab:o_ab + 16]
            vd = w_in[:, o_vd + h * 128:o_vd + (h + 1) * 128]
            pl = w_in[:, o_pl + h * 64:o_pl + (h + 1) * 64]
            w_h = np.ascontiguousarray(np.concatenate(
                [qd, qd[:, perm2], kd, kd[:, perm2], qg, kg, og, af, ab, kg, vg, vd, pl], axis=1))
            wg = np.zeros((33, 64), np.float32)
            wg[0:16, 0:32] = p['gla_wa2_f'][layer][:, h * 32:(h + 1) * 32]
            wg[16:32, 32:64] = p['gla_wa2_b'][layer][:, h * 32:(h + 1) * 32]
            wg[32, 0:32] = p['gla_ba_f'][layer][h * 32:(h + 1) * 32]
            wg[32, 32:64] = p['gla_ba_b'][layer][h * 32:(h + 1) * 32]
            maps.append({
                "xT": XT[b], "cc": cc, "wmod": wmod, "bmod": bmod, "nrm1": nrm1, "w_h": w_h, "wg": wg,
                "gnorm": np.ascontiguousarray(p['gla_norm'][layer][h * 64:(h + 1) * 64, None]),
                "dnorm": np.ascontiguousarray(p['diff_norm'][layer][h * 128:(h + 1) * 128, None]),
                "poolw": np.ascontiguousarray(p['pool_w'][layer][h]),
                "pscale": np.ascontiguousarray(p['pool_scale'][layer][h * 64:(h + 1) * 64, None]),
                "bandm": _band_mats(POOL_WINDOWS[h]),
                "cosT": cosT, "sinT": sinT, "lamv": lamv, "lamc": lamc,
                "trif": trif, "trib": trib, "ident": ident,
            })
    return maps


def gather_A(results, T):
    out = []
    for b in range(NB):
        M = np.empty((D, T), np.float32)
        for h in range(4):
            r = results[b * 4 + h]["mixT"]
            M[h * 64:(h + 1) * 64] = r[0:64]
            M[256 + h * 128:256 + (h + 1) * 128] = r[64:192]
            M[768 + h * 64:768 + (h + 1) * 64] = r[192:256]
        out.append(M)
    return out


CB = CTX // 4


def _make_sts(TB, nst):
    n64 = TB // 64
    base = n64 // nst
    sts, s = [], 0
    for i in range(nst):
        wd = (base + (1 if i < n64 % nst else 0)) * 64
        sts.append((s, wd))
        s += wd
    assert s == TB
    return sts


def emit_B(nc, tk, ps, LB, nst, dr, gmap, uid="", final=True):
    TB = CB + LB
    sts = _make_sts(TB, nst)
    STW = max(w for _, w in sts)
    NSUB = (STW + 127) // 128
    stop = None
    xT, mixT, w_out, cc, wmod, bmod, nrm2, fnorm = (dr[k] for k in ("xT", "mixT", "w_out", "cc", "wmod", "bmod",
                                                                    "nrm2", "fnorm"))
    wr, br, w1, w3, w2, ident, xoT, xfT = (dr[k] for k in ("wr", "br", "w1", "w3", "w2", "ident", "xoT", "xfT"))

    def pieces(s, w):
        out = []
        if s < CB:
            out.append((gmap(s), 0, min(CB, s + w) - s))
        if s + w > CB:
            a = max(CB, s) - s
            out.append((gmap(s + a), a, w))
        return out

    es = contextlib.ExitStack()
    with es:
        sb = lambda name, shape, dt=F32: _sb(nc, es, name + uid, shape, dt)
        PS = lambda i: ("ps", i)
        x3 = xT.rearrange("(k p) t -> p k t", p=128)
        m3 = mixT.rearrange("(k p) t -> p k t", p=128)
        xo3 = xoT.rearrange("(k p) t -> p k t", p=128)
        xf3 = xfT.rearrange("(k p) t -> p k t", p=128) if final else None

        acc = sb("acc", [128, 8, STW])
        h2T = sb("h2T", [128, 8, STW], BF16)
        wb = [sb("wb%d" % i, [128, 12288], BF16) for i in range(2)]
        stg = [sb("stg%d" % i, [128, 1024]) for i in range(2)]
        mst = sb("mst", [128, 8, 512])
        mbf = sb("mbf", [128, 8, 512], BF16)
        aT = sb("aT", [128, 4, 512], BF16)
        sA = [sb("sA%d" % i, [128, 512]) for i in range(2)]
        uB = [sb("uB%d" % i, [128, 512]) for i in range(2)]
        tmpf = [sb("tmpf%d" % i, [128, 512]) for i in range(2)]
        rstd = sb("rstd", [128, 512])
        sq = [sb("sq%d" % i, [128, 512], BF16) for i in range(2)]
        wgt = sb("wgt", [128, NSUB, 16])
        wrep = sb("wrep", [128, 128])
        wrS = sb("wrS", [128, 8, 20])
        brS = sb("brS", [1, 20])
        ones1 = sb("ones1", [1, 128])
        onesb = sb("onesb", [128, 128], BF16)
        identS = sb("identS", [128, 128])
        ccS = sb("ccS", [128, 16])
        scT = sb("scT", [128, 16])
        bmS = sb("bmS", [128, 32])
        n2S = sb("n2S", [128, 8])
        fnS = sb("fnS", [128, 8])
        modS = sb("modS", [128, 64])
        G1 = sb("G1", [128, 2, 8])
        S2 = sb("S2", [128, 2, 8])
        A2 = sb("A2", [128, 2, 8])
        G2m = sb("G2m", [128, 2, 8])
        lg = sb("lg", [128, 20])
        rt = sb("rt", [128, 40])

        def ld(q, dst, src, key):
            tk.dma(q, dst, src, (), (key,))
        ld('sp', identS[:], ident, "identS")
        ld('sp', ccS[:], cc.rearrange("p k c -> p (k c)"), "ccS")
        ld('sp', bmS[:], bmod, "bmS")
        ld('sp', n2S[:], nrm2, "n2S")
        ld('sp', fnS[:], fnorm, "fnS")
        ld('sp', wrS[:], wr.rearrange("(k p) n -> p k n", p=128), "wrS")
        ld('sp', brS[:], br, "brS")
        tk.memset('dve', onesb[:], 1.0 / D, ("onesb",))
        tk.memset('dve', ones1[:], 1.0, ("ones1",))

        tk.act(scT[:], ccS[:], AF.Exp, ("ccS",), ("scT",), scale=-1.0)
        tk.ts('dve', scT[:], scT[:], 1.0, ALU.add, ("scT",), ("scT",))
        tk.op('dve', lambda e: e.reciprocal(out=scT[:], in_=scT[:]), ("scT",), ("scT",))
        tk.tt('dve', scT[:], scT[:], ccS[:], ALU.mult, ("scT", "ccS"), ("scT",))
        wmod3 = wmod.rearrange("(k p) n -> p k n", p=128)
        for blk in range(8):
            tk.dma('sp', mst[:], wmod3[:, :, blk * 512:(blk + 1) * 512], (), ("mst",))
            for jj in range(4):
                j = blk * 4 + jj
                for k in range(8):
                    tk.mm(ps[7][:, 2 * j:2 * j + 2], mst[:, k, jj * 128:(jj + 1) * 128],
                          scT[:, 2 * k:2 * k + 2], k == 0, k == 7, ("mst", "scT"), (PS(7),))
        tk.copy('dve', modS[:], ps[7][:, 0:64], (PS(7),), ("modS",))
        for c in range(2):
            for dst, j0, key in ((G1, 0, "G1"), (S2, 8, "S2"), (A2, 16, "A2"), (G2m, 24, "G2m")):
                tk.tt('dve', dst[:, c, :], modS[:, 2 * j0 + c:2 * j0 + 16:2], bmS[:, j0:j0 + 8], ALU.add,
                      ("modS", "bmS"), (key,))
            tk.ts('dve', A2[:, c, :], A2[:, c, :], 1.0, ALU.add, ("A2",), ("A2",))
            tk.tt('dve', A2[:, c, :], A2[:, c, :], n2S[:], ALU.mult, ("A2", "n2S"), ("A2",))
        if stop == 1:
            tk.finish()
            return nc

        def segs(s, w):
            out = []
            if s < CB:
                out.append((0, min(CB, s + w) - s, 1))
            if s + w > CB:
                a = max(CB, s) - s
                out.append((a, w, 0))
            return out

        cast_i = [0]

        def load_cast(dst_ap, src_ap, dkey, n):
            i = cast_i[0] % 2
            cast_i[0] += 1
            tk.dma('sp', stg[i][:, 0:n], src_ap, (), ("stg%d" % i,))
            eng = 'dve' if (cast_i[0] % 3 == 0) else 'pool'
            tk.copy(eng, dst_ap, stg[i][:, 0:n], ("stg%d" % i,), (dkey,))

        def mean_rstd(src_fn, w, keys):
            for k in range(8):
                tk.tt('pool', sq[k % 2][:, 0:w], src_fn(k), src_fn(k), ALU.mult, keys, ("sq%d" % (k % 2),))
                tk.mm(ps[7][:, 0:w], onesb[:], sq[k % 2][:, 0:w], k == 0, k == 7,
                      ("onesb", "sq%d" % (k % 2)), (PS(7),))
            tk.ts('dve', rstd[:, 0:w], ps[7][:, 0:w], EPS, ALU.add, (PS(7),), ("rstd",))
            tk.act(rstd[:, 0:w], rstd[:, 0:w], AF.Ln, ("rstd",), ("rstd",))
            tk.act(rstd[:, 0:w], rstd[:, 0:w], AF.Exp, ("rstd",), ("rstd",), scale=-0.5)

        for (S0, SW) in sts:
            ttiles = [(ls, min(512, SW - ls)) for ls in range(0, SW, 512)]
            for k in range(8):
                load_cast(wb[0][:, k * 1024:(k + 1) * 1024], w_out[k * 128:(k + 1) * 128, :], "wb0", 1024)
            for (ls, w) in ttiles:
                s = S0 + ls
                sg = segs(s, w)
                for (g0, a, b) in pieces(s, w):
                    tk.dma('sp', acc[:, :, ls + a:ls + b], x3[:, :, g0:g0 + b - a], (), (("acc", ls),))
                    tk.dma('sp', mst[:, :, a:b], m3[:, :, g0:g0 + b - a], (), ("mst",))
                for k in range(8):
                    tk.copy('pool' if k % 2 else 'dve', mbf[:, k, 0:w], mst[:, k, 0:w], ("mst",), ("mbf",))
                for k2 in range(8):
                    bk = 5 + k2 % 2
                    for k in range(8):
                        tk.mm(ps[bk][:, 0:w], wb[0][:, k * 1024 + k2 * 128:k * 1024 + (k2 + 1) * 128],
                              mbf[:, k, 0:w], k == 0, k == 7, ("wb0", "mbf"), (PS(bk),))
                    for (a, b, c) in sg:
                        tk.stt(acc[:, k2, ls + a:ls + b], ps[bk][:, a:b], G1[:, c, k2:k2 + 1],
                               acc[:, k2, ls + a:ls + b], ALU.mult, ALU.add,
                               (PS(bk), "G1", ("acc", ls)), (("acc", ls),))
                mean_rstd(lambda k: acc[:, k, ls:ls + w], w, (("acc", ls),))
                for k in range(8):
                    tf = tmpf[k % 2]
                    tfk = "tmpf%d" % (k % 2)
                    tk.tt('dve', tf[:, 0:w], acc[:, k, ls:ls + w], rstd[:, 0:w], ALU.mult,
                          (("acc", ls), "rstd"), (tfk,))
                    for (a, b, c) in sg:
                        tk.act(mst[:, k, a:b], tf[:, a:b], AF.Identity, (tfk, "A2", "S2"), ("mst",),
                               bias=S2[:, c, k:k + 1], scale=A2[:, c, k:k + 1])
                    tk.copy('act', h2T[:, k, ls:ls + w], mst[:, k, 0:w], ("mst",), (("h2T", ls),))
                for c0 in range(0, w, 128):
                    m = min(128, w - c0)
                    si = (ls + c0) // 128
                    for k in range(8):
                        tk.mm(ps[7][0:m, 0:20], mst[:, k, c0:c0 + m], wrS[:, k, :], k == 0, False,
                              ("mst", "wrS"), (PS(7),))
                    tk.mm(ps[7][0:m, 0:20], ones1[0:1, 0:m], brS[0:1, :], False, True, ("ones1", "brS"), (PS(7),))
                    R_ = ("rt",)
                    tk.copy('dve', lg[0:m, :], ps[7][0:m, 0:20], (PS(7),), ("lg",))
                    gmax, ngmax, gsum, gtop = rt[0:m, 0:1], rt[0:m, 1:2], rt[0:m, 2:3], rt[0:m, 3:4]
                    ge, oh, esel = rt[0:m, 4:8], rt[0:m, 8:12], rt[0:m, 12:16]
                    m1, nm1, m2, psm = rt[0:m, 16:17], rt[0:m, 17:18], rt[0:m, 18:19], rt[0:m, 19:20]
                    mk1, es2, mk2, pe_ = rt[0:m, 20:24], rt[0:m, 24:28], rt[0:m, 28:32], rt[0:m, 32:36]
                    wl = rt[0:m, 36:40]
                    tk.op('dve', lambda e: e.reduce_max(out=gmax, in_=lg[0:m, 0:4], axis=AX.X), ("lg",), R_)
                    tk.ts('dve', ngmax, gmax, -1.0, ALU.mult, R_, R_)
                    tk.act(ge, lg[0:m, 0:4], AF.Exp, ("lg", "rt"), R_, bias=ngmax)
                    tk.op('dve', lambda e: e.reduce_sum(out=gsum, in_=ge, axis=AX.X), R_, R_)
                    tk.op('dve', lambda e: e.reciprocal(out=gtop, in_=gsum), R_, R_)
                    tk.ts('dve', oh, lg[0:m, 0:4], gmax, ALU.is_equal, ("lg", "rt"), R_)
                    tk.ts('dve', esel, lg[0:m, 4:8], oh[:, 0:1], ALU.mult, ("lg", "rt"), R_)
                    for g in range(1, 4):
                        tk.stt(esel, lg[0:m, 4 + 4 * g:8 + 4 * g], oh[:, g:g + 1], esel, ALU.mult, ALU.add,
                               ("lg", "rt"), R_)
                    tk.op('dve', lambda e: e.reduce_max(out=m1, in_=esel, axis=AX.X), R_, R_)
                    tk.ts('dve', mk1, esel, m1, ALU.is_equal, R_, R_)
                    tk.stt(es2, mk1, -1.0e30, esel, ALU.mult, ALU.add, R_, R_)
                    tk.op('dve', lambda e: e.reduce_max(out=m2, in_=es2, axis=AX.X), R_, R_)
                    tk.ts('dve', mk2, es2, m2, ALU.is_equal, R_, R_)
                    tk.tt('dve', mk2, mk2, mk1, ALU.add, R_, R_)
                    tk.ts('dve', nm1, m1, -1.0, ALU.mult, R_, R_)
                    tk.act(pe_, esel, AF.Exp, R_, R_, bias=nm1)
                    tk.tt('dve', pe_, pe_, mk2, ALU.mult, R_, R_)
                    tk.op('dve', lambda e: e.reduce_sum(out=psm, in_=pe_, axis=AX.X), R_, R_)
                    tk.op('dve', lambda e: e.reciprocal(out=psm, in_=psm), R_, R_)
                    tk.tt('dve', psm, psm, gtop, ALU.mult, R_, R_)
                    tk.ts('dve', wl, pe_, psm, ALU.mult, R_, R_)
                    for g in range(4):
                        tk.ts('dve', wgt[0:m, si, 4 * g:4 * g + 4], wl, oh[:, g:g + 1], ALU.mult,
                              R_, (("wgt", si),))
            if stop == 2:
                tk.finish()
                return nc
            for e in range(NEXP):
                wbe = wb[e % 2]
                wk = "wb%d" % (e % 2)
                for k in range(0, 8, 2):
                    load_cast(wbe[:, k * 512:(k + 2) * 512],
                              w1[e].rearrange("(k p) n -> p k n", p=128)[:, k:k + 2, :], wk, 1024)
                for k in range(0, 8, 2):
                    load_cast(wbe[:, 4096 + k * 512:4096 + (k + 2) * 512],
                              w3[e].rearrange("(k p) n -> p k n", p=128)[:, k:k + 2, :], wk, 1024)
                for dc in range(4):
                    load_cast(wbe[:, 8192 + dc * 1024:8192 + (dc + 1) * 1024],
                              w2[e][dc * 128:(dc + 1) * 128, :], wk, 1024)
                for (ls, w) in ttiles:
                    s = S0 + ls
                    sg = segs(s, w)
                    for c0 in range(0, w, 128):
                        m = min(128, w - c0)
                        si = (ls + c0) // 128
                        tk.copy('pool', wrep[0:m, :], wgt[0:m, si, e:e + 1].to_broadcast([m, 128]),
                                (("wgt", si),), ("wrep",))
                        tk.mm(ps[0][:, c0:c0 + m], wrep[0:m, :], identS[0:m, 0:m], True, True,
                              ("wrep", "identS"), (PS(0),))
                    for dc in range(4):
                        bA, bB = 1 + dc % 2, 3 + dc % 2
                        for k in range(8):
                            tk.mm(ps[bA][:, 0:w], wbe[:, k * 512 + dc * 128:k * 512 + (dc + 1) * 128],
                                  h2T[:, k, ls:ls + w], k == 0, k == 7, (wk, ("h2T", ls)), (PS(bA),))
                        for k in range(8):
                            tk.mm(ps[bB][:, 0:w],
                                  wbe[:, 4096 + k * 512 + dc * 128:4096 + k * 512 + (dc + 1) * 128],
                                  h2T[:, k, ls:ls + w], k == 0, k == 7, (wk, ("h2T", ls)), (PS(bB),))
                        sa, ub = sA[dc % 2], uB[dc % 2]
                        sak, ubk = "sA%d" % (dc % 2), "uB%d" % (dc % 2)
                        tk.act(sa[:, 0:w], ps[bA][:, 0:w], AF.Silu, (PS(bA),), (sak,))
                        tk.tt('dve', ub[:, 0:w], sa[:, 0:w], ps[bB][:, 0:w], ALU.mult, (sak, PS(bB)), (ubk,))
                        tk.tt('dve', aT[:, dc, 0:w], ub[:, 0:w], ps[0][:, 0:w], ALU.mult, (ubk, PS(0)), ("aT",))
                    for k2 in range(8):
                        bk = 5 + k2 % 2
                        for dc in range(4):
                            tk.mm(ps[bk][:, 0:w],
                                  wbe[:, 8192 + dc * 1024 + k2 * 128:8192 + dc * 1024 + (k2 + 1) * 128],
                                  aT[:, dc, 0:w], dc == 0, dc == 3, (wk, "aT"), (PS(bk),))
                        for (a, b, c) in sg:
                            tk.stt(acc[:, k2, ls + a:ls + b], ps[bk][:, a:b], G2m[:, c, k2:k2 + 1],
                                   acc[:, k2, ls + a:ls + b], ALU.mult, ALU.add,
                                   (PS(bk), "G2m", ("acc", ls)), (("acc", ls),))
            for (ls, w) in ttiles:
                s = S0 + ls
                for (g0, a, b) in pieces(s, w):
                    tk.dma('pool', xo3[:, :, g0:g0 + b - a], acc[:, :, ls + a:ls + b], (("acc", ls),), ())
                if not final:
                    continue
                mean_rstd(lambda k: acc[:, k, ls:ls + w], w, (("acc", ls),))
                for k in range(8):
                    tk.stt(mst[:, k, 0:w], acc[:, k, ls:ls + w], fnS[:, k:k + 1], rstd[:, 0:w],
                           ALU.mult, ALU.mult, (("acc", ls), "fnS", "rstd"), ("mst",))
                for (g0, a, b) in pieces(s, w):
                    tk.dma('pool', xf3[:, :, g0:g0 + b - a], mst[:, :, a:b], ("mst",), ())
        tk.barrier()


def build_B(LB, nst):
    TB = CB + LB
    nc = bass.Bass("TRN2", target_bir_lowering=False)

    def din(name, shape, dt=F32):
        return nc.dram_tensor(name, list(shape), dt, kind="ExternalInput").ap()

    dr = {"xT": din("xT", [D, TB]), "mixT": din("mixT", [D, TB]), "w_out": din("w_out", [D, D]),
          "cc": din("cc", [128, 8, 2]), "wmod": din("wmod", [D, 4096]), "bmod": din("bmod", [128, 32]),
          "nrm2": din("nrm2", [128, 8]), "fnorm": din("fnorm", [128, 8]), "wr": din("wr", [D, 20]),
          "br": din("br", [1, 20]), "w1": din("w1", [NEXP, D, DEXP]), "w3": din("w3", [NEXP, D, DEXP]),
          "w2": din("w2", [NEXP, DEXP, D]), "ident": din("ident", [128, 128])}
    dr["xoT"] = nc.dram_tensor("xoT", [D, TB], F32, kind="ExternalOutput").ap()
    dr["xfT"] = nc.dram_tensor("xfT", [D, TB], F32, kind="ExternalOutput").ap()
    es = contextlib.ExitStack()
    with es:
        tk = TK(nc, es)
        ps = [es.enter_context(nc.psum_tensor("ps%d" % i, [128, 512], F32)) for i in range(8)]
        emit_B(nc, tk, ps, LB, nst, dr, lambda c: c)
        tk.finish()
    return nc


def prep_B(p, layer, XT, MIX):
    T = XT[0].shape[1]
    L = T - CTX
    LB = L // 4
    wmod = np.ascontiguousarray(p['w_mod'][layer][:, 2048:6144])
    bmod = np.ascontiguousarray(np.asarray(p['b_mod'][layer][2048:6144], np.float32).reshape(32, 128).T)
    bsel = np.concatenate([np.arange(0, 8), np.arange(8, 32)])
    wr = np.ascontiguousarray(np.concatenate([p['router_wg'][layer], p['router_we'][layer]], axis=1))
    br = np.ascontiguousarray(np.concatenate([p['router_bg'][layer], p['router_be'][layer]])[None, :])
    ident = np.eye(128, dtype=np.float32)
    maps = []
    for b in range(NB):
        cc = np.ascontiguousarray(np.stack([_chunks(p['c'][b]), _chunks(p['c_ctx'])], axis=2))
        for j in range(4):
            cols = np.concatenate([np.arange(j * CB, (j + 1) * CB), CTX + np.arange(j * LB, (j + 1) * LB)])
            maps.append({
                "xT": np.ascontiguousarray(XT[b][:, cols]), "mixT": np.ascontiguousarray(MIX[b][:, cols]),
                "w_out": np.ascontiguousarray(p['w_out'][layer]), "cc": cc, "wmod": wmod, "bmod": bmod,
                "nrm2": _chunks(p['norm2'][layer]), "fnorm": _chunks(p['final_norm']),
                "wr": wr, "br": br,
                "w1": np.ascontiguousarray(p['exp_w1'][layer]), "w3": np.ascontiguousarray(p['exp_w3'][layer]),
                "w2": np.ascontiguousarray(p['exp_w2'][layer]), "ident": ident,
            })
    return maps


def gather_B(results, T, key):
    L = T - CTX
    LB = L // 4
    out = []
    for b in range(NB):
        M = np.empty((D, T), np.float32)
        for j in range(4):
            r = results[b * 4 + j][key]
            M[:, j * CB:(j + 1) * CB] = r[:, 0:CB]
            M[:, CTX + j * LB:CTX + (j + 1) * LB] = r[:, CB:]
        out.append(M)
    return out


def build_fused(L, nst):
    C = CTX
    T = C + L
    LB = L // 4
    nc = bass.Bass("TRN2", target_bir_lowering=False)

    def din(name, shape, dt=F32):
        return nc.dram_tensor(name, list(shape), dt, kind="ExternalInput").ap()

    def dint(name, shape, dt=F32):
        return nc.dram_tensor(name, list(shape), dt, kind="Internal").ap()

    xT = din("xT", [D, T])
    cc = din("cc", [128, 8, 2])
    wmod = din("wmod", [2, D, 6144])
    bmod = din("bmod", [2, 128, 48])
    nrm1 = din("nrm1", [2, 128, 8])
    nrm2 = din("nrm2", [2, 128, 8])
    fnorm = din("fnorm", [128, 8])
    w_h = din("w_h", [2, 4, D, 960])
    wg = din("wg", [2, 4, 33, 64])
    gnorm = din("gnorm", [2, 4, 64, 1])
    dnorm = din("dnorm", [2, 4, 128, 1])
    poolw = din("poolw", [2, 4, 64, 64])
    pscale = din("pscale", [2, 4, 64, 1])
    bandm = din("bandm", [4, 5, 128, 128])
    cosT = din("cosT", [128, T])
    sinT = din("sinT", [128, T])
    lamv = din("lamv", [2, 128, 4, 64])
    lamc = din("lamc", [2, 128, 2])
    trif = din("trif", [128, 128])
    trib = din("trib", [128, 128])
    ident = din("ident", [128, 128])
    w_out = din("w_out", [2, D, D])
    wr = din("wr", [2, D, 20])
    br = din("br", [2, 1, 20])
    w1 = din("w1", [2, NEXP, D, DEXP])
    w3 = din("w3", [2, NEXP, D, DEXP])
    w2 = din("w2", [2, NEXP, DEXP, D])
    xfT = nc.dram_tensor("xfT", [D, T], F32, kind="ExternalOutput").ap()
    MIX = dint("MIX", [D, T])
    XN = [dint("XN0", [D, T]), dint("XN1", [D, T])]
    ofT = dint("ofT", [64, T])

    es = contextlib.ExitStack()
    with es:
        tk = TK(nc, es)
        ps = [es.enter_context(nc.psum_tensor("ps%d" % i, [128, 512], F32)) for i in range(8)]
        for l in range(2):
            xsrc = xT if l == 0 else XN[0]
            for h in range(4):
                dr = {"xT": xsrc, "cc": cc, "wmod": wmod[l][:, 0:2048], "bmod": bmod[l][:, 0:16], "nrm1": nrm1[l],
                      "w_h": w_h[l, h], "wg": wg[l, h], "gnorm": gnorm[l, h], "dnorm": dnorm[l, h],
                      "poolw": poolw[l, h], "pscale": pscale[l, h], "bandm": bandm[h], "cosT": cosT, "sinT": sinT,
                      "lamv": lamv[l], "lamc": lamc[l], "trif": trif, "trib": trib, "ident": ident, "ofT": ofT,
                      "mix_gla": MIX[h * 64:(h + 1) * 64, :], "mix_diff": MIX[256 + h * 128:256 + (h + 1) * 128, :],
                      "mix_pool": MIX[768 + h * 64:768 + (h + 1) * 64, :]}
                emit_A(nc, tk, ps, L, dr, uid="_a%d%d" % (l, h))
            for j in range(4):
                dr = {"xT": xsrc, "mixT": MIX, "w_out": w_out[l], "cc": cc, "wmod": wmod[l][:, 2048:6144],
                      "bmod": bmod[l][:, 16:48], "nrm2": nrm2[l], "fnorm": fnorm, "wr": wr[l], "br": br[l],
                      "w1": w1[l], "w3": w3[l], "w2": w2[l], "ident": ident, "xoT": XN[l], "xfT": xfT}
                gm = (lambda jj: (lambda c: jj * CB + c if c < CB else CTX + jj * LB + (c - CB)))(j)
                emit_B(nc, tk, ps, LB, nst, dr, gm, uid="_b%d%d" % (l, j), final=(l == 1))
        tk.finish()
    return nc


def prep_fused(p):
    L = p['x'].shape[1]
    cosT, sinT = _rope_tables(L)
    perm = _rope_perm()
    perm2 = np.concatenate([perm, 64 + perm])
    o_qg, o_kg, o_vg, o_og, o_af, o_ab, o_qd, o_kd, o_vd, o_pl = [int(v) for v in _IN_OFF[:10]]
    f32 = np.float32
    w_h = np.empty((2, 4, D, 960), f32)
    wg = np.zeros((2, 4, 33, 64), f32)
    gnorm = np.empty((2, 4, 64, 1), f32)
    dnorm = np.empty((2, 4, 128, 1), f32)
    pscale = np.empty((2, 4, 64, 1), f32)
    lamv = np.empty((2, 128, 4, 64), f32)
    lamc = np.empty((2, 128, 2), f32)
    for l in range(2):
        w_in = np.asarray(p['w_in'][l], f32)
        lam_init = 0.8 - 0.6 * math.exp(-0.3 * l)
        lamv[l] = np.stack([p['lam_q1'][l], p['lam_k1'][l], p['lam_q2'][l], p['lam_k2'][l]], 0)[None]
        lamc[l] = np.array([lam_init, 1.0 - lam_init], f32)[None]
        for h in range(4):
            qd = w_in[:, o_qd + h * 128:o_qd + (h + 1) * 128]
            kd = w_in[:, o_kd + h * 128:o_kd + (h + 1) * 128]
            qg = w_in[:, o_qg + h * 32:o_qg + (h + 1) * 32]
            kg = w_in[:, o_kg + h * 32:o_kg + (h + 1) * 32]
            vg = w_in[:, o_vg + h * 64:o_vg + (h + 1) * 64]
            og = w_in[:, o_og + h * 64:o_og + (h + 1) * 64]
            af = w_in[:, o_af:o_af + 16]
            ab = w_in[:, o_ab:o_ab + 16]
            vd = w_in[:, o_vd + h * 128:o_vd + (h + 1) * 128]
            pl = w_in[:, o_pl + h * 64:o_pl + (h + 1) * 64]
            w_h[l, h] = np.concatenate([qd, qd[:, perm2], kd, kd[:, perm2], qg, kg, og, af, ab, kg, vg, vd, pl], axis=1)
            wg[l, h, 0:16, 0:32] = p['gla_wa2_f'][l][:, h * 32:(h + 1) * 32]
            wg[l, h, 16:32, 32:64] = p['gla_wa2_b'][l][:, h * 32:(h + 1) * 32]
            wg[l, h, 32, 0:32] = p['gla_ba_f'][l][h * 32:(h + 1) * 32]
            wg[l, h, 32, 32:64] = p['gla_ba_b'][l][h * 32:(h + 1) * 32]
            gnorm[l, h, :, 0] = p['gla_norm'][l][h * 64:(h + 1) * 64]
            dnorm[l, h, :, 0] = p['diff_norm'][l][h * 128:(h + 1) * 128]
            pscale[l, h, :, 0] = p['pool_scale'][l][h * 64:(h + 1) * 64]
    shared = {
        "wmod": np.ascontiguousarray(p['w_mod'], f32),
        "bmod": np.ascontiguousarray(np.stack([np.asarray(p['b_mod'][l], f32).reshape(48, 128).T for l in range(2)])),
        "nrm1": np.stack([_chunks(p['norm1'][l]) for l in range(2)]),
        "nrm2": np.stack([_chunks(p['norm2'][l]) for l in range(2)]),
        "fnorm": _chunks(p['final_norm']),
        "w_h": w_h, "wg": wg, "gnorm": gnorm, "dnorm": dnorm,
        "poolw": np.ascontiguousarray(p['pool_w'], f32), "pscale": pscale,
        "bandm": np.stack([_band_mats(wn) for wn in POOL_WINDOWS]),
        "cosT": cosT, "sinT": sinT, "lamv": lamv, "lamc": lamc,
        "trif": np.triu(np.ones((128, 128), f32)), "trib": np.tril(np.ones((128, 128), f32)),
        "ident": np.eye(128, dtype=f32),
        "w_out": np.ascontiguousarray(p['w_out'], f32),
        "wr": np.ascontiguousarray(np.concatenate([p['router_wg'], p['router_we']], axis=2), f32),
        "br": np.ascontiguousarray(np.concatenate([p['router_bg'], p['router_be']], axis=1)[:, None, :], f32),
        "w1": np.ascontiguousarray(p['exp_w1'], f32), "w3": np.ascontiguousarray(p['exp_w3'], f32),
        "w2": np.ascontiguousarray(p['exp_w2'], f32),
    }
    maps = []
    for core in range(8):
        b = core // 4
        m = dict(shared)
        m["xT"] = np.ascontiguousarray(np.concatenate([p['ctx'][b].T, p['x'][b].T], axis=1).astype(f32))
        m["cc"] = np.ascontiguousarray(np.stack([_chunks(p['c'][b]), _chunks(p['c_ctx'])], axis=2))
        maps.append(m)
    return maps


def kernel_fused(**inputs):
    p = {k: np.asarray(v) for k, v in inputs.items()}
    L = p['x'].shape[1]
    nst = 3 if L >= 8192 else 2
    nc = build_fused(L, nst)
    res = run_bass_kernel_spmd(nc, prep_fused(p), core_ids=list(range(8)))
    out = np.stack([np.ascontiguousarray(res.results[b * 4]["xfT"][:, CTX:].T) for b in range(NB)], axis=0)
    return out.astype(np.float32)


def kernel_unfused(**inputs):
    p = {k: np.asarray(v) for k, v in inputs.items()}
    L = p['x'].shape[1]
    T = CTX + L
    nst = 3 if L >= 8192 else 2
    XT = [np.ascontiguousarray(np.concatenate([p['ctx'][b].T, p['x'][b].T], axis=1).astype(np.float32))
          for b in range(NB)]
    XF = None
    for layer in range(2):
        ncA = build_A(L)
        resA = run_bass_kernel_spmd(ncA, prep_A(p, layer, XT), core_ids=list(range(8)))
        MIX = gather_A(resA.results, T)
        del resA
        ncB = build_B(L // 4, nst)
        resB = run_bass_kernel_spmd(ncB, prep_B(p, layer, XT, MIX), core_ids=list(range(8)))
        XT = gather_B(resB.results, T, "xoT")
        if layer == 1:
            XF = gather_B(resB.results, T, "xfT")
        del resB, MIX
    out = np.stack([np.ascontiguousarray(XF[b][:, CTX:].T) for b in range(NB)], axis=0)
    return out.astype(np.float32)


def kernel(**inputs):
    return kernel_fused(**inputs)
```

```python
import contextlib
import math
import numpy as np
import concourse.bass as bass
import concourse.mybir as mybir
from concourse.bass_utils import run_bass_kernel_spmd

F32 = mybir.dt.float32
BF16 = mybir.dt.bfloat16
ALU = mybir.AluOpType
AF = mybir.ActivationFunctionType
AX = mybir.AxisListType

D = 1024
NB = 2
CTX = 256
GRID_W = 64
EPS = 1e-6
POOL_WINDOWS = (2, 4, 8, 16)
NEXP = 16
DEXP = 512


class TK:
    def __init__(self, nc, es, sync_same=True):
        self.nc = nc
        self.sync_same = sync_same
        self.E = {'pe': nc.tensor, 'dve': nc.vector, 'act': nc.scalar, 'pool': nc.gpsimd, 'sp': nc.sync}
        self.sem = {k: es.enter_context(nc.semaphore("s_" + k)) for k in ('pe', 'dve', 'act', 'pool')}
        self.cnt = {k: 0 for k in self.sem}
        self.waited = {}
        self.reg = {}
        self.NDS = 8
        self.dsem = {q: [es.enter_context(nc.semaphore("d_%s%d" % (q, i))) for i in range(self.NDS)]
                     for q in ('sp', 'pool')}
        self.dcnt = {q: [0] * self.NDS for q in self.dsem}
        self.dnext = {q: 0 for q in self.dsem}
        self.nops = 0

    def _semof(self, src):
        if isinstance(src, tuple):
            return self.dsem[src[1]][src[2]]
        return self.sem[src]

    def _wait(self, eng, src, val, raw):
        if src == eng:
            if eng == 'pe' or not raw or not self.sync_same:
                return
        key = (eng, src)
        if self.waited.get(key, 0) >= val:
            return
        self.waited[key] = val
        self.E[eng].wait_ge(self._semof(src), val)

    def _deps(self, eng, reads, writes):
        for k in reads:
            r = self.reg.get(k)
            if r is not None and r[0] is not None:
                self._wait(eng, r[0][0], r[0][1], True)
        for k in writes:
            r = self.reg.get(k)
            if r is not None:
                if r[0] is not None:
                    self._wait(eng, r[0][0], r[0][1], False)
                for s, v in r[1].items():
                    self._wait(eng, s, v, False)

    def _commit(self, src, val, reads, writes):
        for k in reads:
            r = self.reg.get(k)
            if r is None:
                r = [None, {}]
                self.reg[k] = r
            if r[1].get(src, 0) < val:
                r[1][src] = val
        for k in writes:
            self.reg[k] = [(src, val), {}]

    def op(self, eng, fn, reads=(), writes=()):
        pr = tuple(k for k in reads if isinstance(k, tuple) and k[0] == 'ps')
        if pr:
            writes = tuple(writes) + pr
        self._deps(eng, reads, writes)
        ins = fn(self.E[eng])
        self.cnt[eng] += 1
        ins.then_inc(self.sem[eng], 1)
        self._commit(eng, self.cnt[eng], reads, writes)
        self.nops += 1

    def dma(self, q, out, in_, reads=(), writes=()):
        i = self.dnext[q]
        self.dnext[q] = (i + 1) % self.NDS
        src = ('d', q, i)
        if self.dcnt[q][i] > 0:
            self._wait(q, src, self.dcnt[q][i], True)
        self._deps(q, reads, writes)
        ins = self.E[q].dma_start(out=out, in_=in_)
        self.dcnt[q][i] += 16
        ins.then_inc(self.dsem[q][i], 16)
        self._commit(src, self.dcnt[q][i], reads, writes)
        self.nops += 1

    def coll(self, kind, op, groups, in_ap, out_ap, reads=(), writes=()):
        q = 'pool'
        i = self.dnext[q]
        self.dnext[q] = (i + 1) % self.NDS
        src = ('d', q, i)
        if self.dcnt[q][i] > 0:
            self._wait(q, src, self.dcnt[q][i], True)
        self._deps(q, reads, writes)
        ins = self.nc.gpsimd.collective_compute(kind, op, replica_groups=groups, ins=[in_ap], outs=[out_ap])
        self.dcnt[q][i] += 16
        ins.then_inc(self.dsem[q][i], 16)
        self._commit(src, self.dcnt[q][i], reads, writes)

    def barrier(self):
        for e in ('pe', 'dve', 'act', 'pool', 'sp'):
            for s_ in self.sem:
                if s_ != e and self.cnt[s_] > 0:
                    self._wait(e, s_, self.cnt[s_], True)
            for q in self.dsem:
                for i in range(self.NDS):
                    if self.dcnt[q][i] > 0:
                        self._wait(e, ('d', q, i), self.dcnt[q][i], True)
        self.reg = {}

    def finish(self):
        for q in self.dsem:
            for i in range(self.NDS):
                if self.dcnt[q][i] > 0:
                    self._wait('sp', ('d', q, i), self.dcnt[q][i], True)
        for e in self.sem:
            if self.cnt[e] > 0:
                self._wait('sp', e, self.cnt[e], True)

    def mm(self, out, lhsT, rhs, start, stop, reads, writes):
        self.op('pe', lambda e: e.matmul(out, lhsT, rhs, start=start, stop=stop,
                                         skip_group_check=True), reads, writes)

    def act(self, out, in_, func, reads, writes, bias=None, scale=None, eng='act'):
        kw = {}
        if bias is not None:
            kw['bias'] = bias
        if scale is not None:
            kw['scale'] = scale
        self.op('act', lambda e: e.activation(out=out, in_=in_, func=func, **kw), reads, writes)

    def tt(self, eng, out, in0, in1, op, reads, writes):
        self.op(eng, lambda e: e.tensor_tensor(out=out, in0=in0, in1=in1, op=op), reads, writes)

    def ts(self, eng, out, in0, s1, op0, reads, writes, s2=None, op1=None):
        if op1 is None:
            self.op(eng, lambda e: e.tensor_scalar(out=out, in0=in0, scalar1=s1, scalar2=None, op0=op0),
                    reads, writes)
        else:
            self.op(eng, lambda e: e.tensor_scalar(out=out, in0=in0, scalar1=s1, scalar2=s2, op0=op0, op1=op1),
                    reads, writes)

    def stt(self, out, in0, scalar, in1, op0, op1, reads, writes):
        self.op('dve', lambda e: e.scalar_tensor_tensor(out=out, in0=in0, scalar=scalar, in1=in1,
                                                        op0=op0, op1=op1), reads, writes)

    def copy(self, eng, out, in_, reads, writes):
        if eng == 'act':
            self.op('act', lambda e: e.copy(out=out, in_=in_), reads, writes)
        else:
            self.op(eng, lambda e: e.tensor_copy(out=out, in_=in_), reads, writes)

    def memset(self, eng, ap, val, writes):
        self.op(eng, lambda e: e.memset(ap, val), (), writes)


class _PS(list):
    pass


def _alloc_psum(nc, es):
    big = [es.enter_context(nc.psum_tensor("psb%d" % i, [128, 1024], F32)) for i in range(4)]
    ps = _PS([big[i // 2][:, (i % 2) * 512:(i % 2 + 1) * 512] for i in range(8)])
    ps.big = big
    return ps


def _sb(nc, es, name, shape, dt):
    return es.enter_context(nc.sbuf_tensor(name, list(shape), dt))


def emit_A(nc, tk, ps, L, dr, uid=""):
    C = CTX
    T = C + L
    NKT = T // 128
    ntl = L // 512
    tiles = [(0, C)] + [(C + 512 * i, 512) for i in range(ntl)]
    stop = None
    xT, cc, wmod, bmod, nrm1, w_h, wg = (dr[k] for k in ("xT", "cc", "wmod", "bmod", "nrm1", "w_h", "wg"))
    gnorm, dnorm, poolw, pscale, bandm = (dr[k] for k in ("gnorm", "dnorm", "poolw", "pscale", "bandm"))
    cosT, sinT, lamv, lamc, trif, trib, ident, ofT = (dr[k] for k in ("cosT", "sinT", "lamv", "lamc", "trif",
                                                                      "trib", "ident", "ofT"))
    mix_gla, mix_diff, mix_pool = dr["mix_gla"], dr["mix_diff"], dr["mix_pool"]

    es = contextlib.ExitStack()
    with es:
        sb = lambda name, shape, dt=F32: _sb(nc, es, name + uid, shape, dt)
        PS = lambda i: ("ps", i)

        KT = sb("KT", [128, T], BF16)
        V = sb("V", [128, NKT, 130], BF16)
        PL = sb("PL", [128, NKT, 64], BF16)
        wfm = sb("wfm", [128, 8, 960], BF16)
        xt = [sb("xt%d" % i, [128, 8, 512]) for i in range(2)]
        hT = sb("hT", [128, 8, 512], BF16)
        sq = [sb("sq%d" % i, [128, 512], BF16) for i in range(2)]
        rstd = sb("rstd", [128, 512])
        tmpf = [sb("tmpf%d" % i, [128, 512]) for i in range(2)]
        cst = sb("cst", [128, 512])
        snt = sb("snt", [128, 512])
        QT = sb("QT", [128, 512], BF16)
        PT = [sb("PT%d" % i, [128, 512], BF16) for i in range(4)]
        onesb = sb("onesb", [128, 128], BF16)
        ones64 = sb("ones64", [64, 64], BF16)
        identS = sb("identS", [128, 128])
        trifS = sb("trifS", [128, 128])
        tribS = sb("tribS", [128, 128])
        trifN = sb("trifN", [128, 128])
        tribN = sb("tribN", [128, 128])
        band = sb("band", [128, 5, 128], BF16)
        bandf = sb("bandf", [128, 5, 128])
        wgS = sb("wgS", [33, 64])
        gnS = sb("gnS", [64, 1])
        dnS = sb("dnS", [128, 1])
        dnS2 = sb("dnS2", [128, 1])
        pwf = sb("pwf", [64, 64])
        pwS = sb("pwS", [64, 64], BF16)
        pscS = sb("pscS", [64, 1])
        lamS = sb("lamS", [128, 4, 64])
        lamcS = sb("lamcS", [128, 2])
        lamt = sb("lamt", [128, 2, 64])
        lamr = sb("lamr", [128, 4])
        nlam = sb("nlam", [128, 1])
        ccS = sb("ccS", [128, 16])
        scT = sb("scT", [128, 16])
        bmS = sb("bmS", [128, 16])
        n1S = sb("n1S", [128, 8])
        modS = sb("modS", [128, 32])
        Amod = sb("Amod", [128, 2, 8])
        Smod = sb("Smod", [128, 2, 8])
        G2 = sb("G2", [33, 512])
        qgT = sb("qgT", [32, 512])
        kgT = sb("kgT", [32, 512])
        ogT = sb("ogT", [64, 512])
        ktm = sb("ktm", [128, 128])
        vtm = sb("vtm", [128, 256], BF16)
        gz = sb("gz", [128, 256])
        gg = sb("gg", [128, 256])
        ebT = sb("ebT", [32, 512])
        enbT = sb("enbT", [32, 512])
        enb = sb("enb", [128, 128])
        qtl = sb("qtl", [32, 512], BF16)
        ktl = sb("ktl", [32, 512], BF16)
        ktlm = sb("ktlm", [128, 128], BF16)
        attm = sb("attm", [128, 512], BF16)
        Sst = sb("Sst", [32, 64])
        Sbf = sb("Sbf", [32, 64], BF16)
        Stmp = sb("Stmp", [32, 64])
        Ust = sb("Ust", [32, 256])
        oT = sb("oT", [64, 512])
        ofl = sb("ofl", [64, 512])
        osq = sb("osq", [64, 512], BF16)
        pdif = sb("pdif", [64, 128], BF16)
        o1 = sb("o1", [128, 128])
        o2 = sb("o2", [128, 128])
        rc = sb("rc", [128, 2])
        osq2 = sb("osq2", [128, 128])
        ss = sb("ss", [128, 1])
        dout = sb("dout", [128, 512])

        grs, gsg, gout, pout = rstd, cst, snt, tmpf[0]
        xT3 = xT.rearrange("(k p) t -> p k t", p=128)

        def ld(q, dst, src, key):
            tk.dma(q, dst, src, (), (key,))
        ld('sp', identS[:], ident, "identS")
        ld('sp', trifS[:], trif, "trifS")
        ld('sp', tribS[:], trib, "tribS")
        ld('sp', wgS[:], wg, "wgS")
        ld('sp', gnS[:], gnorm, "gnS")
        ld('sp', dnS[:], dnorm, "dnS")
        ld('sp', pwf[:], poolw, "pwf")
        ld('sp', pscS[:], pscale, "pscS")
        ld('sp', lamS[:], lamv, "lamS")
        ld('sp', lamcS[:], lamc, "lamcS")
        ld('sp', ccS[:], cc.rearrange("p k c -> p (k c)"), "ccS")
        ld('sp', bmS[:], bmod, "bmS")
        ld('sp', n1S[:], nrm1, "n1S")
        ld('sp', bandf[:], bandm.rearrange("b s t -> s b t"), "bandf")
        if stop == 0.1:
            tk.finish()
            return nc
        tk.memset('dve', onesb[:], 1.0 / D, ("onesb",))
        tk.memset('dve', ones64[:], 1.0 / 64, ("ones64",))
        tk.memset('dve', G2[:], 1.0, ("G2",))
        tk.memset('pool', V[:], 1.0, ("Vinit",))
        if stop == 0.2:
            tk.finish()
            return nc
        tk.ts('dve', trifN[:], trifS[:], -1.0 / 16, ALU.mult, ("trifS",), ("trifN",))
        tk.ts('dve', tribN[:], tribS[:], -1.0 / 16, ALU.mult, ("tribS",), ("tribN",))
        tk.copy('dve', band[:], bandf[:], ("bandf",), ("band",))
        tk.copy('dve', pwS[:], pwf[:], ("pwf",), ("pwS",))

        if stop == 0.3:
            tk.finish()
            return nc
        tk.tt('dve', lamt[:, 0, :], lamS[:, 0, :], lamS[:, 1, :], ALU.mult, ("lamS",), ("lamt",))
        tk.tt('dve', lamt[:, 1, :], lamS[:, 2, :], lamS[:, 3, :], ALU.mult, ("lamS", "lamt"), ("lamt",))
        if stop == 0.4:
            tk.finish()
            return nc
        tk.op('dve', lambda e: e.reduce_sum(out=lamr[:, 0:2], in_=lamt[:], axis=AX.X), ("lamt",), ("lamr",))
        if stop == 0.5:
            tk.finish()
            return nc
        tk.act(lamr[:, 2:4], lamr[:, 0:2], AF.Exp, ("lamr",), ("lamr2",))
        if stop == 0.6:
            tk.finish()
            return nc
        tk.tt('dve', nlam[:], lamr[:, 3:4], lamr[:, 2:3], ALU.subtract, ("lamr2",), ("nlam",))
        if stop == 0.7:
            tk.finish()
            return nc
        tk.tt('dve', nlam[:], nlam[:], lamcS[:, 0:1], ALU.subtract, ("nlam", "lamcS"), ("nlam",))
        if stop == 0.8:
            tk.finish()
            return nc
        tk.tt('dve', dnS2[:], dnS[:], lamcS[:, 1:2], ALU.mult, ("dnS", "lamcS"), ("dnS2",))

        if stop == 1:
            tk.finish()
            return nc
        tk.act(scT[:], ccS[:], AF.Exp, ("ccS",), ("scT",), scale=-1.0)
        tk.ts('dve', scT[:], scT[:], 1.0, ALU.add, ("scT",), ("scT",))
        tk.op('dve', lambda e: e.reciprocal(out=scT[:], in_=scT[:]), ("scT",), ("scT",))
        tk.tt('dve', scT[:], scT[:], ccS[:], ALU.mult, ("scT", "ccS"), ("scT",))
        wst = xt[0]
        wmod3 = wmod.rearrange("(k p) n -> p k n", p=128)
        for blk in range(4):
            tk.dma('sp', wst[:], wmod3[:, :, blk * 512:(blk + 1) * 512], (), ("xt0",))
            for jj in range(4):
                j = blk * 4 + jj
                for k in range(8):
                    tk.mm(ps[0][:, 2 * j:2 * j + 2], wst[:, k, jj * 128:(jj + 1) * 128],
                          scT[:, 2 * k:2 * k + 2], k == 0, k == 7, ("xt0", "scT"), (PS(0),))
        tk.copy('dve', modS[:], ps[0][:, 0:32], (PS(0),), ("modS",))
        for c in range(2):
            sh_v = modS[:, c:16:2]
            sc_v = modS[:, 16 + c:32:2]
            tk.tt('dve', Smod[:, c, :], sh_v, bmS[:, 0:8], ALU.add, ("modS", "bmS"), ("Smod",))
            tk.tt('dve', Amod[:, c, :], sc_v, bmS[:, 8:16], ALU.add, ("modS", "bmS"), ("Amod",))
            tk.ts('dve', Amod[:, c, :], Amod[:, c, :], 1.0, ALU.add, ("Amod",), ("Amod",))
            tk.tt('dve', Amod[:, c, :], Amod[:, c, :], n1S[:], ALU.mult, ("Amod", "n1S"), ("Amod",))

        if stop == 2:
            tk.finish()
            return nc
        w_h3 = w_h.rearrange("(k p) n -> p k n", p=128)
        for k in range(8):
            st = xt[1]
            tk.dma('sp', st[:, 0, :], w_h3[:, k, 0:512], (), ("xt1",))
            tk.dma('sp', st[:, 1, 0:448], w_h3[:, k, 512:960], (), ("xt1",))
            tk.copy('dve', wfm[:, k, 0:512], st[:, 0, :], ("xt1",), ("wfm",))
            tk.copy('pool', wfm[:, k, 512:960], st[:, 1, 0:448], ("xt1",), ("wfm",))

        if stop == 3:
            tk.finish()
            return nc
        def load_x(ti, buf):
            s, w = tiles[ti]
            tk.dma('sp', xt[buf][:, :, 0:w], xT3[:, :, s:s + w], (), ("xt%d" % buf,))

        def norm_tile(ti, buf):
            s, w = tiles[ti]
            c = 1 if ti == 0 else 0
            xk = "xt%d" % buf
            x_ = xt[buf]
            for k in range(8):
                tk.tt('pool', sq[k % 2][:, 0:w], x_[:, k, 0:w], x_[:, k, 0:w], ALU.mult, (xk,), ("sq%d" % (k % 2),))
                tk.mm(ps[7][:, 0:w], onesb[:], sq[k % 2][:, 0:w], k == 0, k == 7, ("onesb", "sq%d" % (k % 2)), (PS(7),))
            tk.ts('dve', rstd[:, 0:w], ps[7][:, 0:w], EPS, ALU.add, (PS(7),), ("rstd",))
            tk.act(rstd[:, 0:w], rstd[:, 0:w], AF.Ln, ("rstd",), ("rstd",))
            tk.act(rstd[:, 0:w], rstd[:, 0:w], AF.Exp, ("rstd",), ("rstd",), scale=-0.5)
            for k in range(8):
                tf = tmpf[k % 2]
                tfk = "tmpf%d" % (k % 2)
                tk.tt('dve', tf[:, 0:w], x_[:, k, 0:w], rstd[:, 0:w], ALU.mult, (xk, "rstd"), (tfk,))
                tk.act(hT[:, k, 0:w], tf[:, 0:w], AF.Identity, (tfk, "Amod", "Smod"), ("hT",),
                       bias=Smod[:, c, k:k + 1], scale=Amod[:, c, k:k + 1])

        def fm_proj(col0, M, w, bank):
            for k in range(8):
                tk.mm(ps[bank][0:M, 0:w], wfm[:, k, col0:col0 + M], hT[:, k, 0:w], k == 0, k == 7,
                      ("wfm", "hT"), (PS(bank),))

        def load_rope(ti):
            s, w = tiles[ti]
            tk.dma('sp', cst[:, 0:w], cosT[:, s:s + w], (), ("cst",))
            tk.dma('sp', snt[:, 0:w], sinT[:, s:s + w], (), ("snt",))

        def rope_from(bankA, bankB, w, out_ap, out_key):
            r1, r2 = tmpf[0], tmpf[1]
            tk.tt('dve', r1[:, 0:w], ps[bankA][:, 0:w], cst[:, 0:w], ALU.mult, (PS(bankA), "cst"), ("tmpf0",))
            tk.tt('dve', r2[:, 0:w], ps[bankB][:, 0:w], snt[:, 0:w], ALU.mult, (PS(bankB), "snt"), ("tmpf1",))
            tk.tt('pool', out_ap, r1[:, 0:w], r2[:, 0:w], ALU.add, ("tmpf0", "tmpf1"), (out_key,))

        def gla_proj(w):
            fm_proj(512, 32, w, 2)
            tk.copy('act', qgT[:, 0:w], ps[2][0:32, 0:w], (PS(2),), ("qgT",))
            fm_proj(544, 32, w, 3)
            tk.copy('act', kgT[:, 0:w], ps[3][0:32, 0:w], (PS(3),), ("kgT",))
            fm_proj(576, 64, w, 2)
            tk.copy('act', ogT[:, 0:w], ps[2][0:64, 0:w], (PS(2),), ("ogT",))
            fm_proj(640, 32, w, 3)
            tk.copy('act', G2[0:32, 0:w], ps[3][0:32, 0:w], (PS(3),), ("G2",))

        def tm_proj(j, ncol):
            for k in range(8):
                tk.mm(ps[4][:, 0:ncol], hT[:, k, j * 128:(j + 1) * 128], wfm[:, k, 672:672 + ncol],
                      k == 0, k == 7, ("hT", "wfm"), (PS(4),))

        gla_first = [True, True]

        def gla_tile(w, nsub, fwd):
            triN, triK = ("trifN", "trifS") if fwd else ("tribN", "tribS")
            triNt = trifN if fwd else tribN
            triM = trifS if fwd else tribS
            g0 = 0 if fwd else 32
            di = 0 if fwd else 1
            W2, W3 = nsub * 64, nsub * 32
            cs = [slice(j * 128, (j + 1) * 128) for j in range(nsub)]
            for j in range(nsub):
                tk.mm(ps[5][:, j * 64:(j + 1) * 64], G2[0:33, cs[j]], wgS[:], True, True, ("G2", "wgS"), (PS(5),))
            tk.act(gz[:, 0:W2], ps[5][:, 0:W2], AF.Exp, (PS(5),), ("gz",), scale=-1.0)
            tk.ts('dve', gz[:, 0:W2], gz[:, 0:W2], 1.0, ALU.add, ("gz",), ("gz",))
            tk.act(gg[:, 0:W2], gz[:, 0:W2], AF.Ln, ("gz",), ("gg",))
            for j in range(nsub):
                tk.mm(ps[5][:, 256 + j * 32:256 + (j + 1) * 32], triNt[:], gg[:, j * 64 + g0:j * 64 + g0 + 32],
                      True, True, (triN, "gg"), (PS(5),))
            for j in range(nsub):
                tk.mm(ps[6][0:32, cs[j]], gg[:, j * 64 + g0:j * 64 + g0 + 32], triNt[:], True, True,
                      (triN, "gg"), (PS(6),))
            tk.act(enb[:, 0:W3], ps[5][:, 256:256 + W3], AF.Exp, (PS(5),), ("enb",), scale=-1.0)
            tk.act(ebT[:, 0:w], ps[6][0:32, 0:w], AF.Exp, (PS(6),), ("ebT",))
            tk.act(enbT[:, 0:w], ps[6][0:32, 0:w], AF.Exp, (PS(6),), ("enbT",), scale=-1.0)
            tk.stt(qtl[:, 0:w], qgT[:, 0:w], 32 ** -0.5, ebT[:, 0:w], ALU.mult, ALU.mult, ("qgT", "ebT"), ("qtl",))
            tk.tt('dve', ktl[:, 0:w], kgT[:, 0:w], enbT[:, 0:w], ALU.mult, ("kgT", "enbT"), ("ktl",))
            tk.tt('dve', ktlm[:, 0:W3], ktm[:, 0:W3], enb[:, 0:W3], ALU.mult, ("ktm", "enb"), ("ktlm",))
            for j in range(nsub):
                tk.mm(ps[4][:, cs[j]], ktl[:, cs[j]], qtl[:, cs[j]], True, True, ("ktl", "qtl"), (PS(4),))
            for j in range(nsub):
                tk.tt('dve', attm[:, cs[j]], ps[4][:, cs[j]], triM[:], ALU.mult, (PS(4), triK), ("attm",))
            for j in range(nsub):
                tk.mm(ps[2][0:32, j * 64:(j + 1) * 64], ktlm[:, j * 32:(j + 1) * 32], vtm[:, j * 64:(j + 1) * 64],
                      True, True, ("ktlm", "vtm"), (PS(2),))
            tk.copy('act', Ust[:, 0:W2], ps[2][0:32, 0:W2], (PS(2),), ("Ust",))
            for j in (range(nsub) if fwd else range(nsub - 1, -1, -1)):
                first = gla_first[di]
                gla_first[di] = False
                tk.mm(ps[3][0:64, cs[j]], vtm[:, j * 64:(j + 1) * 64], attm[:, cs[j]], True, first,
                      ("vtm", "attm"), (PS(3),))
                if not first:
                    tk.mm(ps[3][0:64, cs[j]], Sbf[:], qtl[:, cs[j]], False, True, ("Sbf", "qtl"), (PS(3),))
                eend = ebT[:, j * 128 + 127:j * 128 + 128] if fwd else ebT[:, j * 128:j * 128 + 1]
                Uj = Ust[:, j * 64:(j + 1) * 64]
                if first:
                    tk.ts('dve', Sst[:], Uj, eend, ALU.mult, ("Ust", "ebT"), ("Sst",))
                else:
                    tk.tt('dve', Stmp[:], Uj, Sst[:], ALU.add, ("Ust", "Sst"), ("Stmp",))
                    tk.ts('dve', Sst[:], Stmp[:], eend, ALU.mult, ("Stmp", "ebT"), ("Sst",))
                tk.copy('dve', Sbf[:], Sst[:], ("Sst",), ("Sbf",))
            tk.copy('act', oT[:, 0:w], ps[3][0:64, 0:w], (PS(3),), ("oT",))

        def kt_of(ti):
            s, w = tiles[ti]
            return s // 128, w // 128

        first = True
        load_x(0, 0)
        for ti in range(len(tiles)):
            s, w = tiles[ti]
            buf = ti % 2
            if ti + 1 < len(tiles):
                load_x(ti + 1, 1 - buf)
            load_rope(ti)
            norm_tile(ti, buf)
            if stop == 4:
                tk.finish()
                return nc
            kt0, nsub = kt_of(ti)
            fm_proj(256, 128, w, 0)
            fm_proj(384, 128, w, 1)
            rope_from(0, 1, w, KT[:, s:s + w], ("KT", ti))
            if stop == 4.1:
                tk.finish()
                return nc
            gla_proj(w)
            if stop == 4.2:
                tk.finish()
                return nc
            for j in range(nsub):
                tm_proj(j, 288)
                if stop == 4.21:
                    tk.finish()
                    return nc
                tk.copy('act', ktm[:, j * 32:(j + 1) * 32], ps[4][:, 0:32], (PS(4),), ("ktm",))
                if stop == 4.22:
                    tk.finish()
                    return nc
                tk.copy('act', vtm[:, j * 64:(j + 1) * 64], ps[4][:, 32:96], (PS(4),), ("vtm",))
                if stop == 4.23:
                    tk.finish()
                    return nc
                tk.copy('dve', V[:, kt0 + j, 0:128], ps[4][:, 96:224], (PS(4), "Vinit"), (("V", ti),))
                if stop == 4.24:
                    tk.finish()
                    return nc
                tk.copy('act', PL[:, kt0 + j, :], ps[4][:, 224:288], (PS(4),), (("PL", kt0 + j),))
            if stop == 4.3:
                tk.finish()
                return nc
            gla_tile(w, nsub, True)
            if stop == 4.4:
                tk.finish()
                return nc
            tk.dma('pool', ofT[:, s:s + w], oT[:, 0:w], ("oT",), (("ofT", ti),))
            if stop == 5:
                tk.finish()
                return nc

        if stop == 6:
            tk.finish()
            return nc
        for ti in range(len(tiles)):
            s, w = tiles[ti]
            kt0, nsub = kt_of(ti)
            for j in range(nsub):
                kt = kt0 + j
                if kt < 2:
                    i, n, base = kt, 2, 0
                else:
                    i, n, base = kt - 2, NKT - 2, 2
                parts = []
                if i > 0:
                    parts.append((kt - 1, 0))
                parts.append((kt, 3 if i == 0 else (4 if i == n - 1 else 1)))
                if i < n - 1:
                    parts.append((kt + 1, 2))
                for pi, (skt, bi) in enumerate(parts):
                    tk.mm(ps[0][0:64, 0:128], PL[:, skt, :], band[:, bi, :], pi == 0, pi == len(parts) - 1,
                          (("PL", skt), "band"), (PS(0),))
                tk.copy('act', pdif[:], ps[0][0:64, 0:128], (PS(0),), ("pdif",))
                tk.mm(ps[1][0:64, 0:128], pwS[:], pdif[:], True, True, ("pwS", "pdif"), (PS(1),))
                tk.ts('dve', pout[0:64, j * 128:(j + 1) * 128], ps[1][0:64, 0:128], pscS[:], ALU.mult,
                      (PS(1), "pscS"), ("tmpf0",))
            tk.dma('pool', mix_pool[:, s:s + w], pout[0:64, 0:w], ("tmpf0",), ())

        if stop == 7:
            tk.finish()
            return nc
        order = [0] + list(range(len(tiles) - 1, 0, -1))
        first = True
        load_x(order[0], 0)
        for oi, ti in enumerate(order):
            s, w = tiles[ti]
            buf = oi % 2
            if oi + 1 < len(order):
                load_x(order[oi + 1], 1 - buf)
            load_rope(ti)
            tk.dma('sp', ofl[:, 0:w], ofT[:, s:s + w], (("ofT", ti),), ("ofl",))
            norm_tile(ti, buf)
            kt0, nsub = kt_of(ti)
            fm_proj(0, 128, w, 0)
            fm_proj(128, 128, w, 1)
            rope_from(0, 1, w, QT[:, 0:w], "QT")
            gla_proj(w)
            for j in range(nsub):
                tm_proj(j, 96)
                tk.copy('act', ktm[:, j * 32:(j + 1) * 32], ps[4][:, 0:32], (PS(4),), ("ktm",))
                tk.copy('act', vtm[:, j * 64:(j + 1) * 64], ps[4][:, 32:96], (PS(4),), ("vtm",))
            gla_tile(w, nsub, False)
            tk.tt('dve', oT[:, 0:w], oT[:, 0:w], ofl[:, 0:w], ALU.add, ("oT", "ofl"), ("oT",))
            tk.tt('pool', osq[:, 0:w], oT[:, 0:w], oT[:, 0:w], ALU.mult, ("oT",), ("osq",))
            tk.mm(ps[5][0:64, 0:w], ones64[:], osq[:, 0:w], True, True, ("ones64", "osq"), (PS(5),))
            tk.ts('dve', grs[0:64, 0:w], ps[5][0:64, 0:w], EPS, ALU.add, (PS(5),), ("rstd",))
            tk.act(grs[0:64, 0:w], grs[0:64, 0:w], AF.Ln, ("rstd",), ("rstd",))
            tk.act(grs[0:64, 0:w], grs[0:64, 0:w], AF.Exp, ("rstd",), ("rstd",), scale=-0.5)
            tk.act(gsg[0:64, 0:w], ogT[:, 0:w], AF.Exp, ("ogT",), ("cst",), scale=-1.0)
            tk.ts('dve', gsg[0:64, 0:w], gsg[0:64, 0:w], 1.0, ALU.add, ("cst",), ("cst",))
            tk.op('dve', lambda e: e.reciprocal(out=gsg[0:64, 0:w], in_=gsg[0:64, 0:w]), ("cst",), ("cst",))
            tk.tt('dve', gsg[0:64, 0:w], gsg[0:64, 0:w], ogT[:, 0:w], ALU.mult, ("cst", "ogT"), ("cst",))
            tk.stt(gout[0:64, 0:w], oT[:, 0:w], gnS[:], grs[0:64, 0:w], ALU.mult, ALU.mult,
                   ("oT", "gnS", "rstd"), ("snt",))
            tk.tt('dve', gout[0:64, 0:w], gout[0:64, 0:w], gsg[0:64, 0:w], ALU.mult, ("snt", "cst"), ("snt",))
            tk.dma('pool', mix_gla[:, s:s + w], gout[0:64, 0:w], ("snt",), ())

            if stop == 8:
                tk.finish()
                return nc
            nkt = 2 if ti == 0 else NKT
            accb = [1, 2, 3]
            sb_rot = [0, 6, 7]
            touched = set()
            sbk = [0, 5, 6, 7]
            LA = 1

            def emit_qk(kt):
                ktile = 0 if kt < 2 else 1 + (kt - 2) // 4
                for m in range(2):
                    bk = sbk[2 * (kt % 2) + m]
                    tk.mm(ps[bk][:, 0:w], KT[64 * m:64 * m + 64, kt * 128:(kt + 1) * 128],
                          QT[64 * m:64 * m + 64, 0:w], True, True, (("KT", ktile), "QT"), (PS(bk),))

            def emit_exp_pv(kt):
                ktile = 0 if kt < 2 else 1 + (kt - 2) // 4
                for m in range(2):
                    bk = sbk[2 * (kt % 2) + m]
                    pi = 2 * (kt % 2) + m
                    tk.act(PT[pi][:, 0:w], ps[bk][:, 0:w], AF.Exp, (PS(bk),), ("PT%d" % pi,), scale=0.125)
                for m in range(2):
                    pi = 2 * (kt % 2) + m
                    for j in range(nsub):
                        a = m * 4 + j
                        bank = accb[a // 3]
                        c0 = (a % 3) * 130
                        st = bank not in touched
                        touched.add(bank)
                        tk.mm(ps[bank][:, c0:c0 + 129], PT[pi][:, j * 128:(j + 1) * 128], V[:, kt, 0:129],
                              st, kt == nkt - 1, ("PT%d" % pi, ("V", ktile), "Vinit"), (PS(bank),))

            for i in range(nkt + LA):
                if i < nkt:
                    emit_qk(i)
                if i >= LA:
                    emit_exp_pv(i - LA)
            for j in range(nsub):
                a1, a2 = j, 4 + j
                b1, c1 = accb[a1 // 3], (a1 % 3) * 130
                b2, c2 = accb[a2 // 3], (a2 % 3) * 130
                tk.op('dve', lambda e: e.reciprocal(out=rc[:, 0:1], in_=ps[b1][:, c1 + 128:c1 + 129]),
                      (PS(b1),), ("rc",))
                tk.op('dve', lambda e: e.reciprocal(out=rc[:, 1:2], in_=ps[b2][:, c2 + 128:c2 + 129]),
                      (PS(b2),), ("rc",))
                tk.tt('dve', rc[:, 1:2], rc[:, 1:2], nlam[:], ALU.mult, ("rc", "nlam"), ("rc",))
                tk.ts('dve', o1[:], ps[b1][:, c1:c1 + 128], rc[:, 0:1], ALU.mult, (PS(b1), "rc"), ("o1",))
                tk.stt(o2[:], ps[b2][:, c2:c2 + 128], rc[:, 1:2], o1[:], ALU.mult, ALU.add,
                       (PS(b2), "rc", "o1"), ("o2",))
                tk.tt('pool', osq2[:], o2[:], o2[:], ALU.mult, ("o2",), ("osq2",))
                tk.op('dve', lambda e: e.reduce_sum(out=ss[:], in_=osq2[:], axis=AX.X), ("osq2",), ("ss",))
                tk.ts('dve', ss[:], ss[:], 1.0 / 128, ALU.mult, ("ss",), ("ss",), s2=EPS, op1=ALU.add)
                tk.act(ss[:], ss[:], AF.Ln, ("ss",), ("ss",))
                tk.act(ss[:], ss[:], AF.Exp, ("ss",), ("ss",), scale=-0.5)
                tk.ts('dve', o1[:], o2[:], ss[:], ALU.mult, ("o2", "ss"), ("o1",))
                tk.op('pe', lambda e: e.transpose(ps[4][:, 0:128], o1[:], identS[:]), ("o1", "identS"), (PS(4),))
                tk.ts('dve', dout[:, j * 128:(j + 1) * 128], ps[4][:, 0:128], dnS2[:], ALU.mult,
                      (PS(4), "dnS2"), ("dout",))
            tk.dma('pool', mix_diff[:, s:s + w], dout[:, 0:w], ("dout",), ())
            if stop == 9:
                tk.finish()
                return nc
        tk.barrier()


def build_A(L):
    C = CTX
    T = C + L
    nc = bass.Bass("TRN2", target_bir_lowering=False)

    def din(name, shape, dt=F32):
        return nc.dram_tensor(name, list(shape), dt, kind="ExternalInput").ap()

    dr = {"xT": din("xT", [D, T]), "cc": din("cc", [128, 8, 2]), "wmod": din("wmod", [D, 2048]),
          "bmod": din("bmod", [128, 16]), "nrm1": din("nrm1", [128, 8]), "w_h": din("w_h", [D, 960]),
          "wg": din("wg", [33, 64]), "gnorm": din("gnorm", [64, 1]), "dnorm": din("dnorm", [128, 1]),
          "poolw": din("poolw", [64, 64]), "pscale": din("pscale", [64, 1]), "bandm": din("bandm", [5, 128, 128]),
          "cosT": din("cosT", [128, T]), "sinT": din("sinT", [128, T]), "lamv": din("lamv", [128, 4, 64]),
          "lamc": din("lamc", [128, 2]), "trif": din("trif", [128, 128]), "trib": din("trib", [128, 128]),
          "ident": din("ident", [128, 128])}
    mixT = nc.dram_tensor("mixT", [256, T], F32, kind="ExternalOutput").ap()
    dr["ofT"] = nc.dram_tensor("ofT", [64, T], F32, kind="Internal").ap()
    dr["mix_gla"], dr["mix_diff"], dr["mix_pool"] = mixT[0:64, :], mixT[64:192, :], mixT[192:256, :]
    es = contextlib.ExitStack()
    with es:
        tk = TK(nc, es)
        ps = _alloc_psum(nc, es)
        emit_A(nc, tk, ps, L, dr)
        tk.finish()
    return nc


def _rope_tables(L):
    ax = 32
    t = np.arange(L)
    row = (t // GRID_W).astype(np.float32)
    col = (t % GRID_W).astype(np.float32)
    inv = (1.0 / (10000.0 ** (np.arange(0, ax, 2, dtype=np.float32) / ax))).astype(np.float32)
    ang_r = row[:, None] * inv[None, :]
    ang_c = col[:, None] * inv[None, :]
    cos64 = np.zeros((64, L), np.float32)
    sin64 = np.zeros((64, L), np.float32)
    for seg, ang in enumerate((ang_r, ang_c)):
        c = np.cos(ang).astype(np.float32).T
        s = np.sin(ang).astype(np.float32).T
        cos64[seg * 32:seg * 32 + 16] = c
        cos64[seg * 32 + 16:seg * 32 + 32] = c
        sin64[seg * 32:seg * 32 + 16] = -s
        sin64[seg * 32 + 16:seg * 32 + 32] = s
    cosT = np.concatenate([np.ones((64, CTX), np.float32), cos64], axis=1)
    sinT = np.concatenate([np.zeros((64, CTX), np.float32), sin64], axis=1)
    return (np.ascontiguousarray(np.concatenate([cosT, cosT], 0)),
            np.ascontiguousarray(np.concatenate([sinT, sinT], 0)))


def _rope_perm():
    p = np.zeros(64, np.int64)
    for seg in range(2):
        for i in range(32):
            p[seg * 32 + i] = seg * 32 + (i + 16) % 32
    return p


def _band_mats(win):
    n = 128 * 4
    seq = n
    A = np.zeros((seq, seq), np.float64)
    for t in range(seq):
        lo = max(t - win // 2, 0)
        hi = min(t + win - win // 2, seq)
        A[t, lo:hi] = 1.0 / (hi - lo)
    M = (A - np.eye(seq)).T
    out = np.zeros((5, 128, 128), np.float32)
    out[0] = M[128:256, 256:384]
    out[1] = M[256:384, 256:384]
    out[2] = M[384:512, 256:384]
    out[3] = M[0:128, 0:128]
    out[4] = M[384:512, 384:512]
    return out


def _chunks(v):
    return np.ascontiguousarray(np.asarray(v, np.float32).reshape(-1, 128).T)


_IN_OFF = np.cumsum([0, 128, 128, 256, 256, 16, 16, 512, 512, 512, 256])


def prep_A(p, layer, XT):
    T = XT[0].shape[1]
    L = T - CTX
    cosT, sinT = _rope_tables(L)
    perm = _rope_perm()
    perm2 = np.concatenate([perm, 64 + perm])
    lam_init = 0.8 - 0.6 * math.exp(-0.3 * layer)
    w_in = np.asarray(p['w_in'][layer], np.float32)
    o_qg, o_kg, o_vg, o_og, o_af, o_ab, o_qd, o_kd, o_vd, o_pl = [int(v) for v in _IN_OFF[:10]]
    trif = np.triu(np.ones((128, 128), np.float32))
    trib = np.tril(np.ones((128, 128), np.float32))
    ident = np.eye(128, dtype=np.float32)
    lamv = np.stack([p['lam_q1'][layer], p['lam_k1'][layer], p['lam_q2'][layer], p['lam_k2'][layer]], 0)
    lamv = np.ascontiguousarray(np.broadcast_to(lamv[None], (128, 4, 64)).astype(np.float32))
    lamc = np.ascontiguousarray(np.broadcast_to(np.array([[lam_init, 1.0 - lam_init]], np.float32), (128, 2)))
    wmod = np.ascontiguousarray(p['w_mod'][layer][:, 0:2048])
    bmod = np.ascontiguousarray(np.asarray(p['b_mod'][layer][:2048], np.float32).reshape(16, 128).T)
    nrm1 = _chunks(p['norm1'][layer])
    maps = []
    for b in range(NB):
        cc = np.ascontiguousarray(np.stack([_chunks(p['c'][b]), _chunks(p['c_ctx'])], axis=2))
        for h in range(4):
            qd = w_in[:, o_qd + h * 128:o_qd + (h + 1) * 128]
            kd = w_in[:, o_kd + h * 128:o_kd + (h + 1) * 128]
            qg = w_in[:, o_qg + h * 32:o_qg + (h + 1) * 32]
            kg = w_in[:, o_kg + h * 32:o_kg + (h + 1) * 32]
            vg = w_in[:, o_vg + h * 64:o_vg + (h + 1) * 64]
            og = w_in[:, o_og + h * 64:o_og + (h + 1) * 64]
            af = w_in[:, o_af:o_af + 16]
            ab = w_in[:, o_ab:o_ab + 16]
            vd = w_in[:, o_vd + h * 128:o_vd + (h + 1) * 128]
            pl = w_in[:, o_pl + h * 64:o_pl + (h + 1) * 64]
            w_h = np.ascontiguousarray(np.concatenate(
                [qd, qd[:, perm2], kd, kd[:, perm2], qg, kg, og, af, ab, kg, vg, vd, pl], axis=1))
            wg = np.zeros((33, 64), np.float32)
            wg[0:16, 0:32] = p['gla_wa2_f'][layer][:, h * 32:(h + 1) * 32]
            wg[16:32, 32:64] = p['gla_wa2_b'][layer][:, h * 32:(h + 1) * 32]
            wg[32, 0:32] = p['gla_ba_f'][layer][h * 32:(h + 1) * 32]
            wg[32, 32:64] = p['gla_ba_b'][layer][h * 32:(h + 1) * 32]
            maps.append({
                "xT": XT[b], "cc": cc, "wmod": wmod, "bmod": bmod, "nrm1": nrm1, "w_h": w_h, "wg": wg,
                "gnorm": np.ascontiguousarray(p['gla_norm'][layer][h * 64:(h + 1) * 64, None]),
                "dnorm": np.ascontiguousarray(p['diff_norm'][layer][h * 128:(h + 1) * 128, None]),
                "poolw": np.ascontiguousarray(p['pool_w'][layer][h]),
                "pscale": np.ascontiguousarray(p['pool_scale'][layer][h * 64:(h + 1) * 64, None]),
                "bandm": _band_mats(POOL_WINDOWS[h]),
                "cosT": cosT, "sinT": sinT, "lamv": lamv, "lamc": lamc,
                "trif": trif, "trib": trib, "ident": ident,
            })
    return maps


def gather_A(results, T):
    out = []
    for b in range(NB):
        M = np.empty((D, T), np.float32)
        for h in range(4):
            r = results[b * 4 + h]["mixT"]
            M[h * 64:(h + 1) * 64] = r[0:64]
            M[256 + h * 128:256 + (h + 1) * 128] = r[64:192]
            M[768 + h * 64:768 + (h + 1) * 64] = r[192:256]
        out.append(M)
    return out


CB = CTX // 4


def _make_sts(TB, nst):
    n64 = TB // 64
    base = n64 // nst
    sts, s = [], 0
    for i in range(nst):
        wd = (base + (1 if i < n64 % nst else 0)) * 64
        sts.append((s, wd))
        s += wd
    assert s == TB
    return sts


def emit_B(nc, tk, ps, LB, nst, dr, gmap, uid="", final=True):
    TB = CB + LB
    sts = _make_sts(TB, nst)
    STW = max(w for _, w in sts)
    NSUB = (STW + 127) // 128
    stop = None
    xT, mixT, w_out, cc, wmod, bmod, nrm2, fnorm = (dr[k] for k in ("xT", "mixT", "w_out", "cc", "wmod", "bmod",
                                                                    "nrm2", "fnorm"))
    wr, br, w1, w3, w2, ident, xoT, xfT = (dr[k] for k in ("wr", "br", "w1", "w3", "w2", "ident", "xoT", "xfT"))

    def pieces(s, w):
        out = []
        if s < CB:
            out.append((gmap(s), 0, min(CB, s + w) - s))
        if s + w > CB:
            a = max(CB, s) - s
            out.append((gmap(s + a), a, w))
        return out

    es = contextlib.ExitStack()
    with es:
        sb = lambda name, shape, dt=F32: _sb(nc, es, name + uid, shape, dt)
        PS = lambda i: ("ps", i)
        x3 = xT.rearrange("(k p) t -> p k t", p=128)
        m3 = mixT.rearrange("(k p) t -> p k t", p=128)
        xo3 = xoT.rearrange("(k p) t -> p k t", p=128)
        xf3 = xfT.rearrange("(k p) t -> p k t", p=128) if final else None

        acc = sb("acc", [128, 8, STW])
        h2T = sb("h2T", [128, 8, STW], BF16)
        wb = [sb("wb%d" % i, [128, 12288], BF16) for i in range(2)]
        stg = [sb("stg%d" % i, [128, 1024]) for i in range(2)]
        mst = sb("mst", [128, 8, 512])
        mbf = sb("mbf", [128, 8, 512], BF16)
        aT = sb("aT", [128, 4, 512], BF16)
        sA = [sb("sA%d" % i, [128, 512]) for i in range(2)]
        uB = [sb("uB%d" % i, [128, 512]) for i in range(2)]
        tmpf = [sb("tmpf%d" % i, [128, 512]) for i in range(2)]
        rstd = sb("rstd", [128, 512])
        sq = [sb("sq%d" % i, [128, 512], BF16) for i in range(2)]
        wgt = sb("wgt", [128, NSUB, 16])
        wrep = sb("wrep", [128, 128])
        wrS = sb("wrS", [128, 8, 20])
        brS = sb("brS", [1, 20])
        ones1 = sb("ones1", [1, 128])
        onesb = sb("onesb", [128, 128], BF16)
        identS = sb("identS", [128, 128])
        ccS = sb("ccS", [128, 16])
        scT = sb("scT", [128, 16])
        bmS = sb("bmS", [128, 32])
        n2S = sb("n2S", [128, 8])
        fnS = sb("fnS", [128, 8])
        modS = sb("modS", [128, 64])
        G1 = sb("G1", [128, 2, 8])
        S2 = sb("S2", [128, 2, 8])
        A2 = sb("A2", [128, 2, 8])
        G2m = sb("G2m", [128, 2, 8])
        lg = sb("lg", [128, 20])
        rt = sb("rt", [128, 40])

        def ld(q, dst, src, key):
            tk.dma(q, dst, src, (), (key,))
        ld('sp', identS[:], ident, "identS")
        ld('sp', ccS[:], cc.rearrange("p k c -> p (k c)"), "ccS")
        ld('sp', bmS[:], bmod, "bmS")
        ld('sp', n2S[:], nrm2, "n2S")
        ld('sp', fnS[:], fnorm, "fnS")
        ld('sp', wrS[:], wr.rearrange("(k p) n -> p k n", p=128), "wrS")
        ld('sp', brS[:], br, "brS")
        tk.memset('dve', onesb[:], 1.0 / D, ("onesb",))
        tk.memset('dve', ones1[:], 1.0, ("ones1",))

        tk.act(scT[:], ccS[:], AF.Exp, ("ccS",), ("scT",), scale=-1.0)
        tk.ts('dve', scT[:], scT[:], 1.0, ALU.add, ("scT",), ("scT",))
        tk.op('dve', lambda e: e.reciprocal(out=scT[:], in_=scT[:]), ("scT",), ("scT",))
        tk.tt('dve', scT[:], scT[:], ccS[:], ALU.mult, ("scT", "ccS"), ("scT",))
        wmod3 = wmod.rearrange("(k p) n -> p k n", p=128)
        for blk in range(8):
            tk.dma('sp', mst[:], wmod3[:, :, blk * 512:(blk + 1) * 512], (), ("mst",))
            for jj in range(4):
                j = blk * 4 + jj
                for k in range(8):
                    tk.mm(ps[7][:, 2 * j:2 * j + 2], mst[:, k, jj * 128:(jj + 1) * 128],
                          scT[:, 2 * k:2 * k + 2], k == 0, k == 7, ("mst", "scT"), (PS(7),))
        tk.copy('dve', modS[:], ps[7][:, 0:64], (PS(7),), ("modS",))
        for c in range(2):
            for dst, j0, key in ((G1, 0, "G1"), (S2, 8, "S2"), (A2, 16, "A2"), (G2m, 24, "G2m")):
                tk.tt('dve', dst[:, c, :], modS[:, 2 * j0 + c:2 * j0 + 16:2], bmS[:, j0:j0 + 8], ALU.add,
                      ("modS", "bmS"), (key,))
            tk.ts('dve', A2[:, c, :], A2[:, c, :], 1.0, ALU.add, ("A2",), ("A2",))
            tk.tt('dve', A2[:, c, :], A2[:, c, :], n2S[:], ALU.mult, ("A2", "n2S"), ("A2",))
        if stop == 1:
            tk.finish()
            return nc

        def segs(s, w):
            out = []
            if s < CB:
                out.append((0, min(CB, s + w) - s, 1))
            if s + w > CB:
                a = max(CB, s) - s
                out.append((a, w, 0))
            return out

        cast_i = [0]

        def load_cast(dst_ap, src_ap, dkey, n):
            tk.dma('pool', dst_ap, src_ap, (), (dkey,))

        def mean_rstd(src_fn, w, keys):
            for k in range(8):
                tk.tt('pool', sq[k % 2][:, 0:w], src_fn(k), src_fn(k), ALU.mult, keys, ("sq%d" % (k % 2),))
                tk.mm(ps[7][:, 0:w], onesb[:], sq[k % 2][:, 0:w], k == 0, k == 7,
                      ("onesb", "sq%d" % (k % 2)), (PS(7),))
            tk.ts('dve', rstd[:, 0:w], ps[7][:, 0:w], EPS, ALU.add, (PS(7),), ("rstd",))
            tk.act(rstd[:, 0:w], rstd[:, 0:w], AF.Ln, ("rstd",), ("rstd",))
            tk.act(rstd[:, 0:w], rstd[:, 0:w], AF.Exp, ("rstd",), ("rstd",), scale=-0.5)

        for (S0, SW) in sts:
            ttiles = [(ls, min(512, SW - ls)) for ls in range(0, SW, 512)]
            for k in range(8):
                load_cast(wb[0][:, k * 1024:(k + 1) * 1024], w_out[k * 128:(k + 1) * 128, :], "wb0", 1024)
            for (ls, w) in ttiles:
                s = S0 + ls
                sg = segs(s, w)
                for (g0, a, b) in pieces(s, w):
                    tk.dma('sp', acc[:, :, ls + a:ls + b], x3[:, :, g0:g0 + b - a], (), (("acc", ls),))
                    tk.dma('pool', mbf[:, :, a:b], m3[:, :, g0:g0 + b - a], (), ("mbf",))
                for k2 in range(8):
                    bk = 5 + k2 % 2
                    for k in range(8):
                        tk.mm(ps[bk][:, 0:w], wb[0][:, k * 1024 + k2 * 128:k * 1024 + (k2 + 1) * 128],
                              mbf[:, k, 0:w], k == 0, k == 7, ("wb0", "mbf"), (PS(bk),))
                    for (a, b, c) in sg:
                        tk.stt(acc[:, k2, ls + a:ls + b], ps[bk][:, a:b], G1[:, c, k2:k2 + 1],
                               acc[:, k2, ls + a:ls + b], ALU.mult, ALU.add,
                               (PS(bk), "G1", ("acc", ls)), (("acc", ls),))
                mean_rstd(lambda k: acc[:, k, ls:ls + w], w, (("acc", ls),))
                for k in range(8):
                    tf = tmpf[k % 2]
                    tfk = "tmpf%d" % (k % 2)
                    tk.tt('dve', tf[:, 0:w], acc[:, k, ls:ls + w], rstd[:, 0:w], ALU.mult,
                          (("acc", ls), "rstd"), (tfk,))
                    for (a, b, c) in sg:
                        tk.act(mst[:, k, a:b], tf[:, a:b], AF.Identity, (tfk, "A2", "S2"), ("mst",),
                               bias=S2[:, c, k:k + 1], scale=A2[:, c, k:k + 1])
                    tk.copy('act', h2T[:, k, ls:ls + w], mst[:, k, 0:w], ("mst",), (("h2T", ls),))
                for c0 in range(0, w, 128):
                    m = min(128, w - c0)
                    si = (ls + c0) // 128
                    for k in range(8):
                        tk.mm(ps[7][0:m, 0:20], mst[:, k, c0:c0 + m], wrS[:, k, :], k == 0, False,
                              ("mst", "wrS"), (PS(7),))
                    tk.mm(ps[7][0:m, 0:20], ones1[0:1, 0:m], brS[0:1, :], False, True, ("ones1", "brS"), (PS(7),))
                    R_ = ("rt",)
                    tk.copy('dve', lg[0:m, :], ps[7][0:m, 0:20], (PS(7),), ("lg",))
                    gmax, ngmax, gsum, gtop = rt[0:m, 0:1], rt[0:m, 1:2], rt[0:m, 2:3], rt[0:m, 3:4]
                    ge, oh, esel = rt[0:m, 4:8], rt[0:m, 8:12], rt[0:m, 12:16]
                    m1, nm1, m2, psm = rt[0:m, 16:17], rt[0:m, 17:18], rt[0:m, 18:19], rt[0:m, 19:20]
                    mk1, es2, mk2, pe_ = rt[0:m, 20:24], rt[0:m, 24:28], rt[0:m, 28:32], rt[0:m, 32:36]
                    wl = rt[0:m, 36:40]
                    tk.op('dve', lambda e: e.reduce_max(out=gmax, in_=lg[0:m, 0:4], axis=AX.X), ("lg",), R_)
                    tk.ts('dve', ngmax, gmax, -1.0, ALU.mult, R_, R_)
                    tk.act(ge, lg[0:m, 0:4], AF.Exp, ("lg", "rt"), R_, bias=ngmax)
                    tk.op('dve', lambda e: e.reduce_sum(out=gsum, in_=ge, axis=AX.X), R_, R_)
                    tk.op('dve', lambda e: e.reciprocal(out=gtop, in_=gsum), R_, R_)
                    tk.ts('dve', oh, lg[0:m, 0:4], gmax, ALU.is_equal, ("lg", "rt"), R_)
                    tk.ts('dve', esel, lg[0:m, 4:8], oh[:, 0:1], ALU.mult, ("lg", "rt"), R_)
                    for g in range(1, 4):
                        tk.stt(esel, lg[0:m, 4 + 4 * g:8 + 4 * g], oh[:, g:g + 1], esel, ALU.mult, ALU.add,
                               ("lg", "rt"), R_)
                    tk.op('dve', lambda e: e.reduce_max(out=m1, in_=esel, axis=AX.X), R_, R_)
                    tk.ts('dve', mk1, esel, m1, ALU.is_equal, R_, R_)
                    tk.stt(es2, mk1, -1.0e30, esel, ALU.mult, ALU.add, R_, R_)
                    tk.op('dve', lambda e: e.reduce_max(out=m2, in_=es2, axis=AX.X), R_, R_)
                    tk.ts('dve', mk2, es2, m2, ALU.is_equal, R_, R_)
                    tk.tt('dve', mk2, mk2, mk1, ALU.add, R_, R_)
                    tk.ts('dve', nm1, m1, -1.0, ALU.mult, R_, R_)
                    tk.act(pe_, esel, AF.Exp, R_, R_, bias=nm1)
                    tk.tt('dve', pe_, pe_, mk2, ALU.mult, R_, R_)
                    tk.op('dve', lambda e: e.reduce_sum(out=psm, in_=pe_, axis=AX.X), R_, R_)
                    tk.op('dve', lambda e: e.reciprocal(out=psm, in_=psm), R_, R_)
                    tk.tt('dve', psm, psm, gtop, ALU.mult, R_, R_)
                    tk.ts('dve', wl, pe_, psm, ALU.mult, R_, R_)
                    for g in range(4):
                        tk.ts('dve', wgt[0:m, si, 4 * g:4 * g + 4], wl, oh[:, g:g + 1], ALU.mult,
                              R_, (("wgt", si),))
            if stop == 2:
                tk.finish()
                return nc
            for e in range(NEXP):
                wbe = wb[e % 2]
                wk = "wb%d" % (e % 2)
                for k in range(0, 8, 2):
                    load_cast(wbe[:, k * 512:(k + 2) * 512],
                              w1[e].rearrange("(k p) n -> p k n", p=128)[:, k:k + 2, :], wk, 1024)
                for k in range(0, 8, 2):
                    load_cast(wbe[:, 4096 + k * 512:4096 + (k + 2) * 512],
                              w3[e].rearrange("(k p) n -> p k n", p=128)[:, k:k + 2, :], wk, 1024)
                for dc in range(4):
                    load_cast(wbe[:, 8192 + dc * 1024:8192 + (dc + 1) * 1024],
                              w2[e][dc * 128:(dc + 1) * 128, :], wk, 1024)
                for (ls, w) in ttiles:
                    s = S0 + ls
                    sg = segs(s, w)
                    for c0 in range(0, w, 128):
                        m = min(128, w - c0)
                        si = (ls + c0) // 128
                        tk.copy('dve', wrep[0:m, :], wgt[0:m, si, e:e + 1].to_broadcast([m, 128]),
                                (("wgt", si),), ("wrep",))
                        tk.mm(ps[0][:, c0:c0 + m], wrep[0:m, :], identS[0:m, 0:m], True, True,
                              ("wrep", "identS"), (PS(0),))
                    for dc in range(4):
                        bA, bB = 1 + dc % 2, 3 + dc % 2
                        for k in range(8):
                            tk.mm(ps[bA][:, 0:w], wbe[:, k * 512 + dc * 128:k * 512 + (dc + 1) * 128],
                                  h2T[:, k, ls:ls + w], k == 0, k == 7, (wk, ("h2T", ls)), (PS(bA),))
                        for k in range(8):
                            tk.mm(ps[bB][:, 0:w],
                                  wbe[:, 4096 + k * 512 + dc * 128:4096 + k * 512 + (dc + 1) * 128],
                                  h2T[:, k, ls:ls + w], k == 0, k == 7, (wk, ("h2T", ls)), (PS(bB),))
                        sa, ub = sA[dc % 2], uB[dc % 2]
                        sak, ubk = "sA%d" % (dc % 2), "uB%d" % (dc % 2)
                        tk.act(sa[:, 0:w], ps[bA][:, 0:w], AF.Silu, (PS(bA),), (sak,))
                        tk.tt('dve', ub[:, 0:w], sa[:, 0:w], ps[bB][:, 0:w], ALU.mult, (sak, PS(bB)), (ubk,))
                        tk.tt('dve', aT[:, dc, 0:w], ub[:, 0:w], ps[0][:, 0:w], ALU.mult, (ubk, PS(0)), ("aT",))
                    for k2 in range(8):
                        bk = 5 + k2 % 2
                        for dc in range(4):
                            tk.mm(ps[bk][:, 0:w],
                                  wbe[:, 8192 + dc * 1024 + k2 * 128:8192 + dc * 1024 + (k2 + 1) * 128],
                                  aT[:, dc, 0:w], dc == 0, dc == 3, (wk, "aT"), (PS(bk),))
                        for (a, b, c) in sg:
                            tk.stt(acc[:, k2, ls + a:ls + b], ps[bk][:, a:b], G2m[:, c, k2:k2 + 1],
                                   acc[:, k2, ls + a:ls + b], ALU.mult, ALU.add,
                                   (PS(bk), "G2m", ("acc", ls)), (("acc", ls),))
            for (ls, w) in ttiles:
                s = S0 + ls
                for (g0, a, b) in pieces(s, w):
                    tk.dma('pool', xo3[:, :, g0:g0 + b - a], acc[:, :, ls + a:ls + b], (("acc", ls),), ())
                if not final:
                    continue
                mean_rstd(lambda k: acc[:, k, ls:ls + w], w, (("acc", ls),))
                for k in range(8):
                    tk.stt(mst[:, k, 0:w], acc[:, k, ls:ls + w], fnS[:, k:k + 1], rstd[:, 0:w],
                           ALU.mult, ALU.mult, (("acc", ls), "fnS", "rstd"), ("mst",))
                for (g0, a, b) in pieces(s, w):
                    tk.dma('pool', xf3[:, :, g0:g0 + b - a], mst[:, :, a:b], ("mst",), ())
        tk.barrier()


def build_B(LB, nst):
    TB = CB + LB
    nc = bass.Bass("TRN2", target_bir_lowering=False)

    def din(name, shape, dt=F32):
        return nc.dram_tensor(name, list(shape), dt, kind="ExternalInput").ap()

    dr = {"xT": din("xT", [D, TB]), "mixT": din("mixT", [D, TB]), "w_out": din("w_out", [D, D]),
          "cc": din("cc", [128, 8, 2]), "wmod": din("wmod", [D, 4096]), "bmod": din("bmod", [128, 32]),
          "nrm2": din("nrm2", [128, 8]), "fnorm": din("fnorm", [128, 8]), "wr": din("wr", [D, 20]),
          "br": din("br", [1, 20]), "w1": din("w1", [NEXP, D, DEXP]), "w3": din("w3", [NEXP, D, DEXP]),
          "w2": din("w2", [NEXP, DEXP, D]), "ident": din("ident", [128, 128])}
    dr["xoT"] = nc.dram_tensor("xoT", [D, TB], F32, kind="ExternalOutput").ap()
    dr["xfT"] = nc.dram_tensor("xfT", [D, TB], F32, kind="ExternalOutput").ap()
    es = contextlib.ExitStack()
    with es:
        tk = TK(nc, es)
        ps = _alloc_psum(nc, es)
        emit_B(nc, tk, ps, LB, nst, dr, lambda c: c)
        tk.finish()
    return nc


def prep_B(p, layer, XT, MIX):
    T = XT[0].shape[1]
    L = T - CTX
    LB = L // 4
    wmod = np.ascontiguousarray(p['w_mod'][layer][:, 2048:6144])
    bmod = np.ascontiguousarray(np.asarray(p['b_mod'][layer][2048:6144], np.float32).reshape(32, 128).T)
    bsel = np.concatenate([np.arange(0, 8), np.arange(8, 32)])
    wr = np.ascontiguousarray(np.concatenate([p['router_wg'][layer], p['router_we'][layer]], axis=1))
    br = np.ascontiguousarray(np.concatenate([p['router_bg'][layer], p['router_be'][layer]])[None, :])
    ident = np.eye(128, dtype=np.float32)
    maps = []
    for b in range(NB):
        cc = np.ascontiguousarray(np.stack([_chunks(p['c'][b]), _chunks(p['c_ctx'])], axis=2))
        for j in range(4):
            cols = np.concatenate([np.arange(j * CB, (j + 1) * CB), CTX + np.arange(j * LB, (j + 1) * LB)])
            maps.append({
                "xT": np.ascontiguousarray(XT[b][:, cols]), "mixT": np.ascontiguousarray(MIX[b][:, cols]),
                "w_out": np.ascontiguousarray(p['w_out'][layer]), "cc": cc, "wmod": wmod, "bmod": bmod,
                "nrm2": _chunks(p['norm2'][layer]), "fnorm": _chunks(p['final_norm']),
                "wr": wr, "br": br,
                "w1": np.ascontiguousarray(p['exp_w1'][layer]), "w3": np.ascontiguousarray(p['exp_w3'][layer]),
                "w2": np.ascontiguousarray(p['exp_w2'][layer]), "ident": ident,
            })
    return maps


def gather_B(results, T, key):
    L = T - CTX
    LB = L // 4
    out = []
    for b in range(NB):
        M = np.empty((D, T), np.float32)
        for j in range(4):
            r = results[b * 4 + j][key]
            M[:, j * CB:(j + 1) * CB] = r[:, 0:CB]
            M[:, CTX + j * LB:CTX + (j + 1) * LB] = r[:, CB:]
        out.append(M)
    return out


def build_fused(L, nst):
    C = CTX
    T = C + L
    LB = L // 4
    nc = bass.Bass("TRN2", target_bir_lowering=False)

    def din(name, shape, dt=F32):
        return nc.dram_tensor(name, list(shape), dt, kind="ExternalInput").ap()

    def dint(name, shape, dt=F32):
        return nc.dram_tensor(name, list(shape), dt, kind="Internal").ap()

    xT = din("xT", [D, T])
    cc = din("cc", [128, 8, 2])
    wmod = din("wmod", [2, D, 6144])
    bmod = din("bmod", [2, 128, 48])
    nrm1 = din("nrm1", [2, 128, 8])
    nrm2 = din("nrm2", [2, 128, 8])
    fnorm = din("fnorm", [128, 8])
    w_h = din("w_h", [2, 4, D, 960])
    wg = din("wg", [2, 4, 33, 64])
    gnorm = din("gnorm", [2, 4, 64, 1])
    dnorm = din("dnorm", [2, 4, 128, 1])
    poolw = din("poolw", [2, 4, 64, 64])
    pscale = din("pscale", [2, 4, 64, 1])
    bandm = din("bandm", [4, 5, 128, 128])
    cosT = din("cosT", [128, T])
    sinT = din("sinT", [128, T])
    lamv = din("lamv", [2, 128, 4, 64])
    lamc = din("lamc", [2, 128, 2])
    trif = din("trif", [128, 128])
    trib = din("trib", [128, 128])
    ident = din("ident", [128, 128])
    w_out = din("w_out", [2, D, D])
    wr = din("wr", [2, D, 20])
    br = din("br", [2, 1, 20])
    w1 = din("w1", [2, NEXP, D, DEXP])
    w3 = din("w3", [2, NEXP, D, DEXP])
    w2 = din("w2", [2, NEXP, DEXP, D])
    xfT = nc.dram_tensor("xfT", [D, T], F32, kind="ExternalOutput").ap()
    MIX = dint("MIX", [D, T])
    XN = [dint("XN0", [D, T]), dint("XN1", [D, T])]
    ofT = dint("ofT", [64, T])

    es = contextlib.ExitStack()
    with es:
        tk = TK(nc, es)
        ps = _alloc_psum(nc, es)
        for l in range(2):
            xsrc = xT if l == 0 else XN[0]
            for h in range(4):
                dr = {"xT": xsrc, "cc": cc, "wmod": wmod[l][:, 0:2048], "bmod": bmod[l][:, 0:16], "nrm1": nrm1[l],
                      "w_h": w_h[l, h], "wg": wg[l, h], "gnorm": gnorm[l, h], "dnorm": dnorm[l, h],
                      "poolw": poolw[l, h], "pscale": pscale[l, h], "bandm": bandm[h], "cosT": cosT, "sinT": sinT,
                      "lamv": lamv[l], "lamc": lamc[l], "trif": trif, "trib": trib, "ident": ident, "ofT": ofT,
                      "mix_gla": MIX[h * 64:(h + 1) * 64, :], "mix_diff": MIX[256 + h * 128:256 + (h + 1) * 128, :],
                      "mix_pool": MIX[768 + h * 64:768 + (h + 1) * 64, :]}
                emit_A(nc, tk, ps, L, dr, uid="_a%d%d" % (l, h))
            for j in range(4):
                dr = {"xT": xsrc, "mixT": MIX, "w_out": w_out[l], "cc": cc, "wmod": wmod[l][:, 2048:6144],
                      "bmod": bmod[l][:, 16:48], "nrm2": nrm2[l], "fnorm": fnorm, "wr": wr[l], "br": br[l],
                      "w1": w1[l], "w3": w3[l], "w2": w2[l], "ident": ident, "xoT": XN[l], "xfT": xfT}
                gm = (lambda jj: (lambda c: jj * CB + c if c < CB else CTX + jj * LB + (c - CB)))(j)
                emit_B(nc, tk, ps, LB, nst, dr, gm, uid="_b%d%d" % (l, j), final=(l == 1))
        tk.finish()
    return nc


def prep_fused(p):
    L = p['x'].shape[1]
    cosT, sinT = _rope_tables(L)
    perm = _rope_perm()
    perm2 = np.concatenate([perm, 64 + perm])
    o_qg, o_kg, o_vg, o_og, o_af, o_ab, o_qd, o_kd, o_vd, o_pl = [int(v) for v in _IN_OFF[:10]]
    f32 = np.float32
    w_h = np.empty((2, 4, D, 960), f32)
    wg = np.zeros((2, 4, 33, 64), f32)
    gnorm = np.empty((2, 4, 64, 1), f32)
    dnorm = np.empty((2, 4, 128, 1), f32)
    pscale = np.empty((2, 4, 64, 1), f32)
    lamv = np.empty((2, 128, 4, 64), f32)
    lamc = np.empty((2, 128, 2), f32)
    for l in range(2):
        w_in = np.asarray(p['w_in'][l], f32)
        lam_init = 0.8 - 0.6 * math.exp(-0.3 * l)
        lamv[l] = np.stack([p['lam_q1'][l], p['lam_k1'][l], p['lam_q2'][l], p['lam_k2'][l]], 0)[None]
        lamc[l] = np.array([lam_init, 1.0 - lam_init], f32)[None]
        for h in range(4):
            qd = w_in[:, o_qd + h * 128:o_qd + (h + 1) * 128]
            kd = w_in[:, o_kd + h * 128:o_kd + (h + 1) * 128]
            qg = w_in[:, o_qg + h * 32:o_qg + (h + 1) * 32]
            kg = w_in[:, o_kg + h * 32:o_kg + (h + 1) * 32]
            vg = w_in[:, o_vg + h * 64:o_vg + (h + 1) * 64]
            og = w_in[:, o_og + h * 64:o_og + (h + 1) * 64]
            af = w_in[:, o_af:o_af + 16]
            ab = w_in[:, o_ab:o_ab + 16]
            vd = w_in[:, o_vd + h * 128:o_vd + (h + 1) * 128]
            pl = w_in[:, o_pl + h * 64:o_pl + (h + 1) * 64]
            w_h[l, h] = np.concatenate([qd, qd[:, perm2], kd, kd[:, perm2], qg, kg, og, af, ab, kg, vg, vd, pl], axis=1)
            wg[l, h, 0:16, 0:32] = p['gla_wa2_f'][l][:, h * 32:(h + 1) * 32]
            wg[l, h, 16:32, 32:64] = p['gla_wa2_b'][l][:, h * 32:(h + 1) * 32]
            wg[l, h, 32, 0:32] = p['gla_ba_f'][l][h * 32:(h + 1) * 32]
            wg[l, h, 32, 32:64] = p['gla_ba_b'][l][h * 32:(h + 1) * 32]
            gnorm[l, h, :, 0] = p['gla_norm'][l][h * 64:(h + 1) * 64]
            dnorm[l, h, :, 0] = p['diff_norm'][l][h * 128:(h + 1) * 128]
            pscale[l, h, :, 0] = p['pool_scale'][l][h * 64:(h + 1) * 64]
    shared = {
        "wmod": np.ascontiguousarray(p['w_mod'], f32),
        "bmod": np.ascontiguousarray(np.stack([np.asarray(p['b_mod'][l], f32).reshape(48, 128).T for l in range(2)])),
        "nrm1": np.stack([_chunks(p['norm1'][l]) for l in range(2)]),
        "nrm2": np.stack([_chunks(p['norm2'][l]) for l in range(2)]),
        "fnorm": _chunks(p['final_norm']),
        "w_h": w_h, "wg": wg, "gnorm": gnorm, "dnorm": dnorm,
        "poolw": np.ascontiguousarray(p['pool_w'], f32), "pscale": pscale,
        "bandm": np.stack([_band_mats(wn) for wn in POOL_WINDOWS]),
        "cosT": cosT, "sinT": sinT, "lamv": lamv, "lamc": lamc,
        "trif": np.triu(np.ones((128, 128), f32)), "trib": np.tril(np.ones((128, 128), f32)),
        "ident": np.eye(128, dtype=f32),
        "w_out": np.ascontiguousarray(p['w_out'], f32),
        "wr": np.ascontiguousarray(np.concatenate([p['router_wg'], p['router_we']], axis=2), f32),
        "br": np.ascontiguousarray(np.concatenate([p['router_bg'], p['router_be']], axis=1)[:, None, :], f32),
        "w1": np.ascontiguousarray(p['exp_w1'], f32), "w3": np.ascontiguousarray(p['exp_w3'], f32),
        "w2": np.ascontiguousarray(p['exp_w2'], f32),
    }
    maps = []
    for core in range(8):
        b = core // 4
        m = dict(shared)
        m["xT"] = np.ascontiguousarray(np.concatenate([p['ctx'][b].T, p['x'][b].T], axis=1).astype(f32))
        m["cc"] = np.ascontiguousarray(np.stack([_chunks(p['c'][b]), _chunks(p['c_ctx'])], axis=2))
        maps.append(m)
    return maps


def kernel_fused(**inputs):
    p = {k: np.asarray(v) for k, v in inputs.items()}
    L = p['x'].shape[1]
    nst = 3 if L >= 8192 else 2
    nc = build_fused(L, nst)
    res = run_bass_kernel_spmd(nc, prep_fused(p), core_ids=list(range(8)))
    out = np.stack([np.ascontiguousarray(res.results[b * 4]["xfT"][:, CTX:].T) for b in range(NB)], axis=0)
    return out.astype(np.float32)


def kernel_unfused(**inputs):
    p = {k: np.asarray(v) for k, v in inputs.items()}
    L = p['x'].shape[1]
    T = CTX + L
    nst = 3 if L >= 8192 else 2
    XT = [np.ascontiguousarray(np.concatenate([p['ctx'][b].T, p['x'][b].T], axis=1).astype(np.float32))
          for b in range(NB)]
    XF = None
    for layer in range(2):
        ncA = build_A(L)
        resA = run_bass_kernel_spmd(ncA, prep_A(p, layer, XT), core_ids=list(range(8)))
        MIX = gather_A(resA.results, T)
        del resA
        ncB = build_B(L // 4, nst)
        resB = run_bass_kernel_spmd(ncB, prep_B(p, layer, XT, MIX), core_ids=list(range(8)))
        XT = gather_B(resB.results, T, "xoT")
        if layer == 1:
            XF = gather_B(resB.results, T, "xfT")
        del resB, MIX
    out = np.stack([np.ascontiguousarray(XF[b][:, CTX:].T) for b in range(NB)], axis=0)
    return out.astype(np.float32)


def kernel(**inputs):
    return kernel_fused(**inputs)
```

```python
import contextlib
import math
import numpy as np
import concourse.bass as bass
import concourse.mybir as mybir
from concourse.bass_utils import run_bass_kernel_spmd

F32 = mybir.dt.float32
BF16 = mybir.dt.bfloat16
ALU = mybir.AluOpType
AF = mybir.ActivationFunctionType
AX = mybir.AxisListType

D = 1024
NB = 2
CTX = 256
GRID_W = 64
EPS = 1e-6
POOL_WINDOWS = (2, 4, 8, 16)
NEXP = 16
DEXP = 512


class TK:
    def __init__(self, nc, es, sync_same=True):
        self.nc = nc
        self.sync_same = sync_same
        self.E = {'pe': nc.tensor, 'dve': nc.vector, 'act': nc.scalar, 'pool': nc.gpsimd, 'sp': nc.sync}
        self.sem = {k: es.enter_context(nc.semaphore("s_" + k)) for k in ('pe', 'dve', 'act', 'pool')}
        self.cnt = {k: 0 for k in self.sem}
        self.waited = {}
        self.reg = {}
        self.NDS = 8
        self.dsem = {q: [es.enter_context(nc.semaphore("d_%s%d" % (q, i))) for i in range(self.NDS)]
                     for q in ('sp', 'pool')}
        self.dcnt = {q: [0] * self.NDS for q in self.dsem}
        self.dnext = {q: 0 for q in self.dsem}
        self.nops = 0

    def _semof(self, src):
        if isinstance(src, tuple):
            return self.dsem[src[1]][src[2]]
        return self.sem[src]

    def _wait(self, eng, src, val, raw):
        if src == eng:
            if eng == 'pe' or not raw or not self.sync_same:
                return
        key = (eng, src)
        if self.waited.get(key, 0) >= val:
            return
        self.waited[key] = val
        self.E[eng].wait_ge(self._semof(src), val)

    def _deps(self, eng, reads, writes):
        for k in reads:
            r = self.reg.get(k)
            if r is not None and r[0] is not None:
                self._wait(eng, r[0][0], r[0][1], True)
        for k in writes:
            r = self.reg.get(k)
            if r is not None:
                if r[0] is not None:
                    self._wait(eng, r[0][0], r[0][1], False)
                for s, v in r[1].items():
                    self._wait(eng, s, v, False)

    def _commit(self, src, val, reads, writes):
        for k in reads:
            r = self.reg.get(k)
            if r is None:
                r = [None, {}]
                self.reg[k] = r
            if r[1].get(src, 0) < val:
                r[1][src] = val
        for k in writes:
            self.reg[k] = [(src, val), {}]

    def op(self, eng, fn, reads=(), writes=()):
        pr = tuple(k for k in reads if isinstance(k, tuple) and k[0] == 'ps')
        if pr:
            writes = tuple(writes) + pr
        self._deps(eng, reads, writes)
        ins = fn(self.E[eng])
        self.cnt[eng] += 1
        ins.then_inc(self.sem[eng], 1)
        self._commit(eng, self.cnt[eng], reads, writes)
        self.nops += 1

    def dma(self, q, out, in_, reads=(), writes=()):
        i = self.dnext[q]
        self.dnext[q] = (i + 1) % self.NDS
        src = ('d', q, i)
        if self.dcnt[q][i] > 0:
            self._wait(q, src, self.dcnt[q][i], True)
        self._deps(q, reads, writes)
        ins = self.E[q].dma_start(out=out, in_=in_)
        self.dcnt[q][i] += 16
        ins.then_inc(self.dsem[q][i], 16)
        self._commit(src, self.dcnt[q][i], reads, writes)
        self.nops += 1

    def coll(self, kind, op, groups, in_ap, out_ap, reads=(), writes=()):
        q = 'pool'
        i = self.dnext[q]
        self.dnext[q] = (i + 1) % self.NDS
        src = ('d', q, i)
        if self.dcnt[q][i] > 0:
            self._wait(q, src, self.dcnt[q][i], True)
        self._deps(q, reads, writes)
        ins = self.nc.gpsimd.collective_compute(kind, op, replica_groups=groups, ins=[in_ap], outs=[out_ap])
        self.dcnt[q][i] += 16
        ins.then_inc(self.dsem[q][i], 16)
        self._commit(src, self.dcnt[q][i], reads, writes)

    def barrier(self):
        for e in ('pe', 'dve', 'act', 'pool', 'sp'):
            for s_ in self.sem:
                if s_ != e and self.cnt[s_] > 0:
                    self._wait(e, s_, self.cnt[s_], True)
            for q in self.dsem:
                for i in range(self.NDS):
                    if self.dcnt[q][i] > 0:
                        self._wait(e, ('d', q, i), self.dcnt[q][i], True)
        self.reg = {}

    def finish(self):
        for q in self.dsem:
            for i in range(self.NDS):
                if self.dcnt[q][i] > 0:
                    self._wait('sp', ('d', q, i), self.dcnt[q][i], True)
        for e in self.sem:
            if self.cnt[e] > 0:
                self._wait('sp', e, self.cnt[e], True)

    def mm(self, out, lhsT, rhs, start, stop, reads, writes):
        self.op('pe', lambda e: e.matmul(out, lhsT, rhs, start=start, stop=stop,
                                         skip_group_check=True), reads, writes)

    def act(self, out, in_, func, reads, writes, bias=None, scale=None, eng='act'):
        kw = {}
        if bias is not None:
            kw['bias'] = bias
        if scale is not None:
            kw['scale'] = scale
        self.op('act', lambda e: e.activation(out=out, in_=in_, func=func, **kw), reads, writes)

    def tt(self, eng, out, in0, in1, op, reads, writes):
        self.op(eng, lambda e: e.tensor_tensor(out=out, in0=in0, in1=in1, op=op), reads, writes)

    def ts(self, eng, out, in0, s1, op0, reads, writes, s2=None, op1=None):
        if op1 is None:
            self.op(eng, lambda e: e.tensor_scalar(out=out, in0=in0, scalar1=s1, scalar2=None, op0=op0),
                    reads, writes)
        else:
            self.op(eng, lambda e: e.tensor_scalar(out=out, in0=in0, scalar1=s1, scalar2=s2, op0=op0, op1=op1),
                    reads, writes)

    def stt(self, out, in0, scalar, in1, op0, op1, reads, writes):
        self.op('dve', lambda e: e.scalar_tensor_tensor(out=out, in0=in0, scalar=scalar, in1=in1,
                                                        op0=op0, op1=op1), reads, writes)

    def copy(self, eng, out, in_, reads, writes):
        if eng == 'act':
            self.op('act', lambda e: e.copy(out=out, in_=in_), reads, writes)
        else:
            self.op(eng, lambda e: e.tensor_copy(out=out, in_=in_), reads, writes)

    def memset(self, eng, ap, val, writes):
        self.op(eng, lambda e: e.memset(ap, val), (), writes)


class _PS(list):
    pass


def _alloc_psum(nc, es):
    big = [es.enter_context(nc.psum_tensor("psb%d" % i, [128, 1024], F32)) for i in range(4)]
    ps = _PS([big[i // 2][:, (i % 2) * 512:(i % 2 + 1) * 512] for i in range(8)])
    ps.big = big
    return ps


def _sb(nc, es, name, shape, dt):
    return es.enter_context(nc.sbuf_tensor(name, list(shape), dt))


def emit_A(nc, tk, ps, L, dr, uid=""):
    C = CTX
    T = C + L
    NKT = T // 128
    ntl = L // 512
    tiles = [(0, C)] + [(C + 512 * i, 512) for i in range(ntl)]
    stop = None
    xT, cc, wmod, bmod, nrm1, w_h, wg = (dr[k] for k in ("xT", "cc", "wmod", "bmod", "nrm1", "w_h", "wg"))
    gnorm, dnorm, poolw, pscale, bandm = (dr[k] for k in ("gnorm", "dnorm", "poolw", "pscale", "bandm"))
    cosT, sinT, lamv, lamc, trif, trib, ident, ofT = (dr[k] for k in ("cosT", "sinT", "lamv", "lamc", "trif",
                                                                      "trib", "ident", "ofT"))
    mix_gla, mix_diff, mix_pool = dr["mix_gla"], dr["mix_diff"], dr["mix_pool"]

    es = contextlib.ExitStack()
    with es:
        sb = lambda name, shape, dt=F32: _sb(nc, es, name + uid, shape, dt)
        PS = lambda i: ("ps", i)

        KT = sb("KT", [128, T], BF16)
        V = sb("V", [128, NKT, 130], BF16)
        PL = sb("PL", [128, NKT, 64], BF16)
        wfm = sb("wfm", [128, 8, 960], BF16)
        xt = [sb("xt%d" % i, [128, 8, 512]) for i in range(2)]
        hT = sb("hT", [128, 8, 512], BF16)
        sq = [sb("sq%d" % i, [128, 512], BF16) for i in range(2)]
        rstd = sb("rstd", [128, 512])
        tmpf = [sb("tmpf%d" % i, [128, 512]) for i in range(2)]
        cst = sb("cst", [128, 512])
        snt = sb("snt", [128, 512])
        QT = sb("QT", [128, 512], BF16)
        PT = [sb("PT%d" % i, [128, 512], BF16) for i in range(4)]
        onesb = sb("onesb", [128, 128], BF16)
        ones64 = sb("ones64", [64, 64], BF16)
        identS = sb("identS", [128, 128])
        trifS = sb("trifS", [128, 128])
        tribS = sb("tribS", [128, 128])
        trifN = sb("trifN", [128, 128])
        tribN = sb("tribN", [128, 128])
        band = sb("band", [128, 5, 128], BF16)
        bandf = sb("bandf", [128, 5, 128])
        wgS = sb("wgS", [33, 64])
        gnS = sb("gnS", [64, 1])
        dnS = sb("dnS", [128, 1])
        dnS2 = sb("dnS2", [128, 1])
        pwf = sb("pwf", [64, 64])
        pwS = sb("pwS", [64, 64], BF16)
        pscS = sb("pscS", [64, 1])
        lamS = sb("lamS", [128, 4, 64])
        lamcS = sb("lamcS", [128, 2])
        lamt = sb("lamt", [128, 2, 64])
        lamr = sb("lamr", [128, 4])
        nlam = sb("nlam", [128, 1])
        ccS = sb("ccS", [128, 16])
        scT = sb("scT", [128, 16])
        bmS = sb("bmS", [128, 16])
        n1S = sb("n1S", [128, 8])
        modS = sb("modS", [128, 32])
        Amod = sb("Amod", [128, 2, 8])
        Smod = sb("Smod", [128, 2, 8])
        G2 = sb("G2", [33, 512])
        qgT = sb("qgT", [32, 512])
        kgT = sb("kgT", [32, 512])
        ogT = sb("ogT", [64, 512])
        ktm = sb("ktm", [128, 128])
        vtm = sb("vtm", [128, 256], BF16)
        gz = sb("gz", [128, 256])
        gg = sb("gg", [128, 256])
        ebT = sb("ebT", [32, 512])
        enbT = sb("enbT", [32, 512])
        enb = sb("enb", [128, 128])
        qtl = sb("qtl", [32, 512], BF16)
        ktl = sb("ktl", [32, 512], BF16)
        ktlm = sb("ktlm", [128, 128], BF16)
        attm = sb("attm", [128, 512], BF16)
        Sst = sb("Sst", [32, 64])
        Sbf = sb("Sbf", [32, 64], BF16)
        Stmp = sb("Stmp", [32, 64])
        Ust = sb("Ust", [32, 256])
        oT = sb("oT", [64, 512])
        ofl = sb("ofl", [64, 512])
        osq = sb("osq", [64, 512], BF16)
        pdif = sb("pdif", [64, 128], BF16)
        o1 = sb("o1", [128, 128])
        o2 = sb("o2", [128, 128])
        rc = sb("rc", [128, 2])
        osq2 = sb("osq2", [128, 128])
        ss = sb("ss", [128, 1])
        dout = sb("dout", [128, 512])

        grs, gsg, gout, pout = rstd, cst, snt, tmpf[0]
        xT3 = xT.rearrange("(k p) t -> p k t", p=128)

        def ld(q, dst, src, key):
            tk.dma(q, dst, src, (), (key,))
        ld('sp', identS[:], ident, "identS")
        ld('sp', trifS[:], trif, "trifS")
        ld('sp', tribS[:], trib, "tribS")
        ld('sp', wgS[:], wg, "wgS")
        ld('sp', gnS[:], gnorm, "gnS")
        ld('sp', dnS[:], dnorm, "dnS")
        ld('sp', pwf[:], poolw, "pwf")
        ld('sp', pscS[:], pscale, "pscS")
        ld('sp', lamS[:], lamv, "lamS")
        ld('sp', lamcS[:], lamc, "lamcS")
        ld('sp', ccS[:], cc.rearrange("p k c -> p (k c)"), "ccS")
        ld('sp', bmS[:], bmod, "bmS")
        ld('sp', n1S[:], nrm1, "n1S")
        ld('sp', bandf[:], bandm.rearrange("b s t -> s b t"), "bandf")
        if stop == 0.1:
            tk.finish()
            return nc
        tk.memset('dve', onesb[:], 1.0 / D, ("onesb",))
        tk.memset('dve', ones64[:], 1.0 / 64, ("ones64",))
        tk.memset('dve', G2[:], 1.0, ("G2",))
        tk.memset('pool', V[:], 1.0, ("Vinit",))
        if stop == 0.2:
            tk.finish()
            return nc
        tk.ts('dve', trifN[:], trifS[:], -1.0 / 16, ALU.mult, ("trifS",), ("trifN",))
        tk.ts('dve', tribN[:], tribS[:], -1.0 / 16, ALU.mult, ("tribS",), ("tribN",))
        tk.copy('dve', band[:], bandf[:], ("bandf",), ("band",))
        tk.copy('dve', pwS[:], pwf[:], ("pwf",), ("pwS",))

        if stop == 0.3:
            tk.finish()
            return nc
        tk.tt('dve', lamt[:, 0, :], lamS[:, 0, :], lamS[:, 1, :], ALU.mult, ("lamS",), ("lamt",))
        tk.tt('dve', lamt[:, 1, :], lamS[:, 2, :], lamS[:, 3, :], ALU.mult, ("lamS", "lamt"), ("lamt",))
        if stop == 0.4:
            tk.finish()
            return nc
        tk.op('dve', lambda e: e.reduce_sum(out=lamr[:, 0:2], in_=lamt[:], axis=AX.X), ("lamt",), ("lamr",))
        if stop == 0.5:
            tk.finish()
            return nc
        tk.act(lamr[:, 2:4], lamr[:, 0:2], AF.Exp, ("lamr",), ("lamr2",))
        if stop == 0.6:
            tk.finish()
            return nc
        tk.tt('dve', nlam[:], lamr[:, 3:4], lamr[:, 2:3], ALU.subtract, ("lamr2",), ("nlam",))
        if stop == 0.7:
            tk.finish()
            return nc
        tk.tt('dve', nlam[:], nlam[:], lamcS[:, 0:1], ALU.subtract, ("nlam", "lamcS"), ("nlam",))
        if stop == 0.8:
            tk.finish()
            return nc
        tk.tt('dve', dnS2[:], dnS[:], lamcS[:, 1:2], ALU.mult, ("dnS", "lamcS"), ("dnS2",))

        if stop == 1:
            tk.finish()
            return nc
        tk.act(scT[:], ccS[:], AF.Exp, ("ccS",), ("scT",), scale=-1.0)
        tk.ts('dve', scT[:], scT[:], 1.0, ALU.add, ("scT",), ("scT",))
        tk.op('dve', lambda e: e.reciprocal(out=scT[:], in_=scT[:]), ("scT",), ("scT",))
        tk.tt('dve', scT[:], scT[:], ccS[:], ALU.mult, ("scT", "ccS"), ("scT",))
        wst = xt[0]
        wmod3 = wmod.rearrange("(k p) n -> p k n", p=128)
        for blk in range(4):
            tk.dma('sp', wst[:], wmod3[:, :, blk * 512:(blk + 1) * 512], (), ("xt0",))
            for jj in range(4):
                j = blk * 4 + jj
                for k in range(8):
                    tk.mm(ps[0][:, 2 * j:2 * j + 2], wst[:, k, jj * 128:(jj + 1) * 128],
                          scT[:, 2 * k:2 * k + 2], k == 0, k == 7, ("xt0", "scT"), (PS(0),))
        tk.copy('dve', modS[:], ps[0][:, 0:32], (PS(0),), ("modS",))
        for c in range(2):
            sh_v = modS[:, c:16:2]
            sc_v = modS[:, 16 + c:32:2]
            tk.tt('dve', Smod[:, c, :], sh_v, bmS[:, 0:8], ALU.add, ("modS", "bmS"), ("Smod",))
            tk.tt('dve', Amod[:, c, :], sc_v, bmS[:, 8:16], ALU.add, ("modS", "bmS"), ("Amod",))
            tk.ts('dve', Amod[:, c, :], Amod[:, c, :], 1.0, ALU.add, ("Amod",), ("Amod",))
            tk.tt('dve', Amod[:, c, :], Amod[:, c, :], n1S[:], ALU.mult, ("Amod", "n1S"), ("Amod",))

        if stop == 2:
            tk.finish()
            return nc
        w_h3 = w_h.rearrange("(k p) n -> p k n", p=128)
        for k in range(8):
            st = xt[1]
            tk.dma('sp', st[:, 0, :], w_h3[:, k, 0:512], (), ("xt1",))
            tk.dma('sp', st[:, 1, 0:448], w_h3[:, k, 512:960], (), ("xt1",))
            tk.copy('dve', wfm[:, k, 0:512], st[:, 0, :], ("xt1",), ("wfm",))
            tk.copy('pool', wfm[:, k, 512:960], st[:, 1, 0:448], ("xt1",), ("wfm",))

        if stop == 3:
            tk.finish()
            return nc
        def load_x(ti, buf):
            s, w = tiles[ti]
            tk.dma('sp', xt[buf][:, :, 0:w], xT3[:, :, s:s + w], (), ("xt%d" % buf,))

        def norm_tile(ti, buf):
            s, w = tiles[ti]
            c = 1 if ti == 0 else 0
            xk = "xt%d" % buf
            x_ = xt[buf]
            for k in range(8):
                tk.tt('pool', sq[k % 2][:, 0:w], x_[:, k, 0:w], x_[:, k, 0:w], ALU.mult, (xk,), ("sq%d" % (k % 2),))
                tk.mm(ps[7][:, 0:w], onesb[:], sq[k % 2][:, 0:w], k == 0, k == 7, ("onesb", "sq%d" % (k % 2)), (PS(7),))
            tk.ts('dve', rstd[:, 0:w], ps[7][:, 0:w], EPS, ALU.add, (PS(7),), ("rstd",))
            tk.act(rstd[:, 0:w], rstd[:, 0:w], AF.Ln, ("rstd",), ("rstd",))
            tk.act(rstd[:, 0:w], rstd[:, 0:w], AF.Exp, ("rstd",), ("rstd",), scale=-0.5)
            for k in range(8):
                tf = tmpf[k % 2]
                tfk = "tmpf%d" % (k % 2)
                tk.tt('dve', tf[:, 0:w], x_[:, k, 0:w], rstd[:, 0:w], ALU.mult, (xk, "rstd"), (tfk,))
                tk.act(hT[:, k, 0:w], tf[:, 0:w], AF.Identity, (tfk, "Amod", "Smod"), ("hT",),
                       bias=Smod[:, c, k:k + 1], scale=Amod[:, c, k:k + 1])

        def fm_proj(col0, M, w, bank):
            for k in range(8):
                tk.mm(ps[bank][0:M, 0:w], wfm[:, k, col0:col0 + M], hT[:, k, 0:w], k == 0, k == 7,
                      ("wfm", "hT"), (PS(bank),))

        def load_rope(ti):
            s, w = tiles[ti]
            tk.dma('sp', cst[:, 0:w], cosT[:, s:s + w], (), ("cst",))
            tk.dma('sp', snt[:, 0:w], sinT[:, s:s + w], (), ("snt",))

        def rope_from(bankA, bankB, w, out_ap, out_key):
            r1, r2 = tmpf[0], tmpf[1]
            tk.tt('dve', r1[:, 0:w], ps[bankA][:, 0:w], cst[:, 0:w], ALU.mult, (PS(bankA), "cst"), ("tmpf0",))
            tk.tt('dve', r2[:, 0:w], ps[bankB][:, 0:w], snt[:, 0:w], ALU.mult, (PS(bankB), "snt"), ("tmpf1",))
            tk.tt('pool', out_ap, r1[:, 0:w], r2[:, 0:w], ALU.add, ("tmpf0", "tmpf1"), (out_key,))

        def gla_proj(w):
            fm_proj(512, 32, w, 2)
            tk.copy('act', qgT[:, 0:w], ps[2][0:32, 0:w], (PS(2),), ("qgT",))
            fm_proj(544, 32, w, 3)
            tk.copy('act', kgT[:, 0:w], ps[3][0:32, 0:w], (PS(3),), ("kgT",))
            fm_proj(576, 64, w, 2)
            tk.copy('act', ogT[:, 0:w], ps[2][0:64, 0:w], (PS(2),), ("ogT",))
            fm_proj(640, 32, w, 3)
            tk.copy('act', G2[0:32, 0:w], ps[3][0:32, 0:w], (PS(3),), ("G2",))

        def tm_proj(j, ncol):
            for k in range(8):
                tk.mm(ps[4][:, 0:ncol], hT[:, k, j * 128:(j + 1) * 128], wfm[:, k, 672:672 + ncol],
                      k == 0, k == 7, ("hT", "wfm"), (PS(4),))

        gla_first = [True, True]

        def gla_tile(w, nsub, fwd):
            triN, triK = ("trifN", "trifS") if fwd else ("tribN", "tribS")
            triNt = trifN if fwd else tribN
            triM = trifS if fwd else tribS
            g0 = 0 if fwd else 32
            di = 0 if fwd else 1
            W2, W3 = nsub * 64, nsub * 32
            cs = [slice(j * 128, (j + 1) * 128) for j in range(nsub)]
            for j in range(nsub):
                tk.mm(ps[5][:, j * 64:(j + 1) * 64], G2[0:33, cs[j]], wgS[:], True, True, ("G2", "wgS"), (PS(5),))
            tk.act(gz[:, 0:W2], ps[5][:, 0:W2], AF.Exp, (PS(5),), ("gz",), scale=-1.0)
            tk.ts('dve', gz[:, 0:W2], gz[:, 0:W2], 1.0, ALU.add, ("gz",), ("gz",))
            tk.act(gg[:, 0:W2], gz[:, 0:W2], AF.Ln, ("gz",), ("gg",))
            for j in range(nsub):
                tk.mm(ps[5][:, 256 + j * 32:256 + (j + 1) * 32], triNt[:], gg[:, j * 64 + g0:j * 64 + g0 + 32],
                      True, True, (triN, "gg"), (PS(5),))
            for j in range(nsub):
                tk.mm(ps[6][0:32, cs[j]], gg[:, j * 64 + g0:j * 64 + g0 + 32], triNt[:], True, True,
                      (triN, "gg"), (PS(6),))
            tk.act(enb[:, 0:W3], ps[5][:, 256:256 + W3], AF.Exp, (PS(5),), ("enb",), scale=-1.0)
            tk.act(ebT[:, 0:w], ps[6][0:32, 0:w], AF.Exp, (PS(6),), ("ebT",))
            tk.act(enbT[:, 0:w], ps[6][0:32, 0:w], AF.Exp, (PS(6),), ("enbT",), scale=-1.0)
            tk.stt(qtl[:, 0:w], qgT[:, 0:w], 32 ** -0.5, ebT[:, 0:w], ALU.mult, ALU.mult, ("qgT", "ebT"), ("qtl",))
            tk.tt('dve', ktl[:, 0:w], kgT[:, 0:w], enbT[:, 0:w], ALU.mult, ("kgT", "enbT"), ("ktl",))
            tk.tt('dve', ktlm[:, 0:W3], ktm[:, 0:W3], enb[:, 0:W3], ALU.mult, ("ktm", "enb"), ("ktlm",))
            for j in range(nsub):
                tk.mm(ps[4][:, cs[j]], ktl[:, cs[j]], qtl[:, cs[j]], True, True, ("ktl", "qtl"), (PS(4),))
            for j in range(nsub):
                tk.tt('dve', attm[:, cs[j]], ps[4][:, cs[j]], triM[:], ALU.mult, (PS(4), triK), ("attm",))
            for j in range(nsub):
                tk.mm(ps[2][0:32, j * 64:(j + 1) * 64], ktlm[:, j * 32:(j + 1) * 32], vtm[:, j * 64:(j + 1) * 64],
                      True, True, ("ktlm", "vtm"), (PS(2),))
            tk.copy('act', Ust[:, 0:W2], ps[2][0:32, 0:W2], (PS(2),), ("Ust",))
            for j in (range(nsub) if fwd else range(nsub - 1, -1, -1)):
                first = gla_first[di]
                gla_first[di] = False
                tk.mm(ps[3][0:64, cs[j]], vtm[:, j * 64:(j + 1) * 64], attm[:, cs[j]], True, first,
                      ("vtm", "attm"), (PS(3),))
                if not first:
                    tk.mm(ps[3][0:64, cs[j]], Sbf[:], qtl[:, cs[j]], False, True, ("Sbf", "qtl"), (PS(3),))
                eend = ebT[:, j * 128 + 127:j * 128 + 128] if fwd else ebT[:, j * 128:j * 128 + 1]
                Uj = Ust[:, j * 64:(j + 1) * 64]
                if first:
                    tk.ts('dve', Sst[:], Uj, eend, ALU.mult, ("Ust", "ebT"), ("Sst",))
                else:
                    tk.tt('dve', Stmp[:], Uj, Sst[:], ALU.add, ("Ust", "Sst"), ("Stmp",))
                    tk.ts('dve', Sst[:], Stmp[:], eend, ALU.mult, ("Stmp", "ebT"), ("Sst",))
                tk.copy('dve', Sbf[:], Sst[:], ("Sst",), ("Sbf",))
            tk.copy('act', oT[:, 0:w], ps[3][0:64, 0:w], (PS(3),), ("oT",))

        def kt_of(ti):
            s, w = tiles[ti]
            return s // 128, w // 128

        first = True
        load_x(0, 0)
        for ti in range(len(tiles)):
            s, w = tiles[ti]
            buf = ti % 2
            if ti + 1 < len(tiles):
                load_x(ti + 1, 1 - buf)
            load_rope(ti)
            norm_tile(ti, buf)
            if stop == 4:
                tk.finish()
                return nc
            kt0, nsub = kt_of(ti)
            fm_proj(256, 128, w, 0)
            fm_proj(384, 128, w, 1)
            rope_from(0, 1, w, KT[:, s:s + w], ("KT", ti))
            if stop == 4.1:
                tk.finish()
                return nc
            gla_proj(w)
            if stop == 4.2:
                tk.finish()
                return nc
            for j in range(nsub):
                tm_proj(j, 288)
                if stop == 4.21:
                    tk.finish()
                    return nc
                tk.copy('act', ktm[:, j * 32:(j + 1) * 32], ps[4][:, 0:32], (PS(4),), ("ktm",))
                if stop == 4.22:
                    tk.finish()
                    return nc
                tk.copy('act', vtm[:, j * 64:(j + 1) * 64], ps[4][:, 32:96], (PS(4),), ("vtm",))
                if stop == 4.23:
                    tk.finish()
                    return nc
                tk.copy('dve', V[:, kt0 + j, 0:128], ps[4][:, 96:224], (PS(4), "Vinit"), (("V", ti),))
                if stop == 4.24:
                    tk.finish()
                    return nc
                tk.copy('act', PL[:, kt0 + j, :], ps[4][:, 224:288], (PS(4),), (("PL", kt0 + j),))
            if stop == 4.3:
                tk.finish()
                return nc
            gla_tile(w, nsub, True)
            if stop == 4.4:
                tk.finish()
                return nc
            tk.dma('pool', ofT[:, s:s + w], oT[:, 0:w], ("oT",), (("ofT", ti),))
            if stop == 5:
                tk.finish()
                return nc

        if stop == 6:
            tk.finish()
            return nc
        for ti in range(len(tiles)):
            s, w = tiles[ti]
            kt0, nsub = kt_of(ti)
            for j in range(nsub):
                kt = kt0 + j
                if kt < 2:
                    i, n, base = kt, 2, 0
                else:
                    i, n, base = kt - 2, NKT - 2, 2
                parts = []
                if i > 0:
                    parts.append((kt - 1, 0))
                parts.append((kt, 3 if i == 0 else (4 if i == n - 1 else 1)))
                if i < n - 1:
                    parts.append((kt + 1, 2))
                for pi, (skt, bi) in enumerate(parts):
                    tk.mm(ps[0][0:64, 0:128], PL[:, skt, :], band[:, bi, :], pi == 0, pi == len(parts) - 1,
                          (("PL", skt), "band"), (PS(0),))
                tk.copy('act', pdif[:], ps[0][0:64, 0:128], (PS(0),), ("pdif",))
                tk.mm(ps[1][0:64, 0:128], pwS[:], pdif[:], True, True, ("pwS", "pdif"), (PS(1),))
                tk.ts('dve', pout[0:64, j * 128:(j + 1) * 128], ps[1][0:64, 0:128], pscS[:], ALU.mult,
                      (PS(1), "pscS"), ("tmpf0",))
            tk.dma('pool', mix_pool[:, s:s + w], pout[0:64, 0:w], ("tmpf0",), ())

        if stop == 7:
            tk.finish()
            return nc
        order = [0] + list(range(len(tiles) - 1, 0, -1))
        first = True
        load_x(order[0], 0)
        for oi, ti in enumerate(order):
            s, w = tiles[ti]
            buf = oi % 2
            if oi + 1 < len(order):
                load_x(order[oi + 1], 1 - buf)
            load_rope(ti)
            tk.dma('sp', ofl[:, 0:w], ofT[:, s:s + w], (("ofT", ti),), ("ofl",))
            norm_tile(ti, buf)
            kt0, nsub = kt_of(ti)
            fm_proj(0, 128, w, 0)
            fm_proj(128, 128, w, 1)
            rope_from(0, 1, w, QT[:, 0:w], "QT")
            gla_proj(w)
            for j in range(nsub):
                tm_proj(j, 96)
                tk.copy('act', ktm[:, j * 32:(j + 1) * 32], ps[4][:, 0:32], (PS(4),), ("ktm",))
                tk.copy('act', vtm[:, j * 64:(j + 1) * 64], ps[4][:, 32:96], (PS(4),), ("vtm",))
            gla_tile(w, nsub, False)
            tk.tt('dve', oT[:, 0:w], oT[:, 0:w], ofl[:, 0:w], ALU.add, ("oT", "ofl"), ("oT",))
            tk.tt('pool', osq[:, 0:w], oT[:, 0:w], oT[:, 0:w], ALU.mult, ("oT",), ("osq",))
            tk.mm(ps[5][0:64, 0:w], ones64[:], osq[:, 0:w], True, True, ("ones64", "osq"), (PS(5),))
            tk.ts('dve', grs[0:64, 0:w], ps[5][0:64, 0:w], EPS, ALU.add, (PS(5),), ("rstd",))
            tk.act(grs[0:64, 0:w], grs[0:64, 0:w], AF.Ln, ("rstd",), ("rstd",))
            tk.act(grs[0:64, 0:w], grs[0:64, 0:w], AF.Exp, ("rstd",), ("rstd",), scale=-0.5)
            tk.act(gsg[0:64, 0:w], ogT[:, 0:w], AF.Exp, ("ogT",), ("cst",), scale=-1.0)
            tk.ts('dve', gsg[0:64, 0:w], gsg[0:64, 0:w], 1.0, ALU.add, ("cst",), ("cst",))
            tk.op('dve', lambda e: e.reciprocal(out=gsg[0:64, 0:w], in_=gsg[0:64, 0:w]), ("cst",), ("cst",))
            tk.tt('dve', gsg[0:64, 0:w], gsg[0:64, 0:w], ogT[:, 0:w], ALU.mult, ("cst", "ogT"), ("cst",))
            tk.stt(gout[0:64, 0:w], oT[:, 0:w], gnS[:], grs[0:64, 0:w], ALU.mult, ALU.mult,
                   ("oT", "gnS", "rstd"), ("snt",))
            tk.tt('dve', gout[0:64, 0:w], gout[0:64, 0:w], gsg[0:64, 0:w], ALU.mult, ("snt", "cst"), ("snt",))
            tk.dma('pool', mix_gla[:, s:s + w], gout[0:64, 0:w], ("snt",), ())

            if stop == 8:
                tk.finish()
                return nc
            nkt = 2 if ti == 0 else NKT
            accb = [1, 2, 3]
            sb_rot = [0, 6, 7]
            touched = set()
            sbk = [0, 5, 6, 7]
            LA = 1

            def emit_qk(kt):
                ktile = 0 if kt < 2 else 1 + (kt - 2) // 4
                for m in range(2):
                    bk = sbk[2 * (kt % 2) + m]
                    tk.mm(ps[bk][:, 0:w], KT[64 * m:64 * m + 64, kt * 128:(kt + 1) * 128],
                          QT[64 * m:64 * m + 64, 0:w], True, True, (("KT", ktile), "QT"), (PS(bk),))

            def emit_exp_pv(kt):
                ktile = 0 if kt < 2 else 1 + (kt - 2) // 4
                for m in range(2):
                    bk = sbk[2 * (kt % 2) + m]
                    pi = 2 * (kt % 2) + m
                    tk.act(PT[pi][:, 0:w], ps[bk][:, 0:w], AF.Exp, (PS(bk),), ("PT%d" % pi,), scale=0.125)
                for m in range(2):
                    pi = 2 * (kt % 2) + m
                    for j in range(nsub):
                        a = m * 4 + j
                        bank = accb[a // 3]
                        c0 = (a % 3) * 130
                        st = bank not in touched
                        touched.add(bank)
                        tk.mm(ps[bank][:, c0:c0 + 129], PT[pi][:, j * 128:(j + 1) * 128], V[:, kt, 0:129],
                              st, kt == nkt - 1, ("PT%d" % pi, ("V", ktile), "Vinit"), (PS(bank),))

            for i in range(nkt + LA):
                if i < nkt:
                    emit_qk(i)
                if i >= LA:
                    emit_exp_pv(i - LA)
            for j in range(nsub):
                a1, a2 = j, 4 + j
                b1, c1 = accb[a1 // 3], (a1 % 3) * 130
                b2, c2 = accb[a2 // 3], (a2 % 3) * 130
                tk.op('dve', lambda e: e.reciprocal(out=rc[:, 0:1], in_=ps[b1][:, c1 + 128:c1 + 129]),
                      (PS(b1),), ("rc",))
                tk.op('dve', lambda e: e.reciprocal(out=rc[:, 1:2], in_=ps[b2][:, c2 + 128:c2 + 129]),
                      (PS(b2),), ("rc",))
                tk.tt('dve', rc[:, 1:2], rc[:, 1:2], nlam[:], ALU.mult, ("rc", "nlam"), ("rc",))
                tk.ts('dve', o1[:], ps[b1][:, c1:c1 + 128], rc[:, 0:1], ALU.mult, (PS(b1), "rc"), ("o1",))
                tk.stt(o2[:], ps[b2][:, c2:c2 + 128], rc[:, 1:2], o1[:], ALU.mult, ALU.add,
                       (PS(b2), "rc", "o1"), ("o2",))
                tk.tt('pool', osq2[:], o2[:], o2[:], ALU.mult, ("o2",), ("osq2",))
                tk.op('dve', lambda e: e.reduce_sum(out=ss[:], in_=osq2[:], axis=AX.X), ("osq2",), ("ss",))
                tk.ts('dve', ss[:], ss[:], 1.0 / 128, ALU.mult, ("ss",), ("ss",), s2=EPS, op1=ALU.add)
                tk.act(ss[:], ss[:], AF.Ln, ("ss",), ("ss",))
                tk.act(ss[:], ss[:], AF.Exp, ("ss",), ("ss",), scale=-0.5)
                tk.ts('dve', o1[:], o2[:], ss[:], ALU.mult, ("o2", "ss"), ("o1",))
                tk.op('pe', lambda e: e.transpose(ps[4][:, 0:128], o1[:], identS[:]), ("o1", "identS"), (PS(4),))
                tk.ts('dve', dout[:, j * 128:(j + 1) * 128], ps[4][:, 0:128], dnS2[:], ALU.mult,
                      (PS(4), "dnS2"), ("dout",))
            tk.dma('pool', mix_diff[:, s:s + w], dout[:, 0:w], ("dout",), ())
            if stop == 9:
                tk.finish()
                return nc
        tk.barrier()


def build_A(L):
    C = CTX
    T = C + L
    nc = bass.Bass("TRN2", target_bir_lowering=False)

    def din(name, shape, dt=F32):
        return nc.dram_tensor(name, list(shape), dt, kind="ExternalInput").ap()

    dr = {"xT": din("xT", [D, T]), "cc": din("cc", [128, 8, 2]), "wmod": din("wmod", [D, 2048]),
          "bmod": din("bmod", [128, 16]), "nrm1": din("nrm1", [128, 8]), "w_h": din("w_h", [D, 960]),
          "wg": din("wg", [33, 64]), "gnorm": din("gnorm", [64, 1]), "dnorm": din("dnorm", [128, 1]),
          "poolw": din("poolw", [64, 64]), "pscale": din("pscale", [64, 1]), "bandm": din("bandm", [5, 128, 128]),
          "cosT": din("cosT", [128, T]), "sinT": din("sinT", [128, T]), "lamv": din("lamv", [128, 4, 64]),
          "lamc": din("lamc", [128, 2]), "trif": din("trif", [128, 128]), "trib": din("trib", [128, 128]),
          "ident": din("ident", [128, 128])}
    mixT = nc.dram_tensor("mixT", [256, T], F32, kind="ExternalOutput").ap()
    dr["ofT"] = nc.dram_tensor("ofT", [64, T], F32, kind="Internal").ap()
    dr["mix_gla"], dr["mix_diff"], dr["mix_pool"] = mixT[0:64, :], mixT[64:192, :], mixT[192:256, :]
    es = contextlib.ExitStack()
    with es:
        tk = TK(nc, es)
        ps = _alloc_psum(nc, es)
        emit_A(nc, tk, ps, L, dr)
        tk.finish()
    return nc


def _rope_tables(L):
    ax = 32
    t = np.arange(L)
    row = (t // GRID_W).astype(np.float32)
    col = (t % GRID_W).astype(np.float32)
    inv = (1.0 / (10000.0 ** (np.arange(0, ax, 2, dtype=np.float32) / ax))).astype(np.float32)
    ang_r = row[:, None] * inv[None, :]
    ang_c = col[:, None] * inv[None, :]
    cos64 = np.zeros((64, L), np.float32)
    sin64 = np.zeros((64, L), np.float32)
    for seg, ang in enumerate((ang_r, ang_c)):
        c = np.cos(ang).astype(np.float32).T
        s = np.sin(ang).astype(np.float32).T
        cos64[seg * 32:seg * 32 + 16] = c
        cos64[seg * 32 + 16:seg * 32 + 32] = c
        sin64[seg * 32:seg * 32 + 16] = -s
        sin64[seg * 32 + 16:seg * 32 + 32] = s
    cosT = np.concatenate([np.ones((64, CTX), np.float32), cos64], axis=1)
    sinT = np.concatenate([np.zeros((64, CTX), np.float32), sin64], axis=1)
    return (np.ascontiguousarray(np.concatenate([cosT, cosT], 0)),
            np.ascontiguousarray(np.concatenate([sinT, sinT], 0)))


def _rope_perm():
    p = np.zeros(64, np.int64)
    for seg in range(2):
        for i in range(32):
            p[seg * 32 + i] = seg * 32 + (i + 16) % 32
    return p


def _band_mats(win):
    n = 128 * 4
    seq = n
    A = np.zeros((seq, seq), np.float64)
    for t in range(seq):
        lo = max(t - win // 2, 0)
        hi = min(t + win - win // 2, seq)
        A[t, lo:hi] = 1.0 / (hi - lo)
    M = (A - np.eye(seq)).T
    out = np.zeros((5, 128, 128), np.float32)
    out[0] = M[128:256, 256:384]
    out[1] = M[256:384, 256:384]
    out[2] = M[384:512, 256:384]
    out[3] = M[0:128, 0:128]
    out[4] = M[384:512, 384:512]
    return out


def _chunks(v):
    return np.ascontiguousarray(np.asarray(v, np.float32).reshape(-1, 128).T)


_IN_OFF = np.cumsum([0, 128, 128, 256, 256, 16, 16, 512, 512, 512, 256])


def prep_A(p, layer, XT):
    T = XT[0].shape[1]
    L = T - CTX
    cosT, sinT = _rope_tables(L)
    perm = _rope_perm()
    perm2 = np.concatenate([perm, 64 + perm])
    lam_init = 0.8 - 0.6 * math.exp(-0.3 * layer)
    w_in = np.asarray(p['w_in'][layer], np.float32)
    o_qg, o_kg, o_vg, o_og, o_af, o_ab, o_qd, o_kd, o_vd, o_pl = [int(v) for v in _IN_OFF[:10]]
    trif = np.triu(np.ones((128, 128), np.float32))
    trib = np.tril(np.ones((128, 128), np.float32))
    ident = np.eye(128, dtype=np.float32)
    lamv = np.stack([p['lam_q1'][layer], p['lam_k1'][layer], p['lam_q2'][layer], p['lam_k2'][layer]], 0)
    lamv = np.ascontiguousarray(np.broadcast_to(lamv[None], (128, 4, 64)).astype(np.float32))
    lamc = np.ascontiguousarray(np.broadcast_to(np.array([[lam_init, 1.0 - lam_init]], np.float32), (128, 2)))
    wmod = np.ascontiguousarray(p['w_mod'][layer][:, 0:2048])
    bmod = np.ascontiguousarray(np.asarray(p['b_mod'][layer][:2048], np.float32).reshape(16, 128).T)
    nrm1 = _chunks(p['norm1'][layer])
    maps = []
    for b in range(NB):
        cc = np.ascontiguousarray(np.stack([_chunks(p['c'][b]), _chunks(p['c_ctx'])], axis=2))
        for h in range(4):
            qd = w_in[:, o_qd + h * 128:o_qd + (h + 1) * 128]
            kd = w_in[:, o_kd + h * 128:o_kd + (h + 1) * 128]
            qg = w_in[:, o_qg + h * 32:o_qg + (h + 1) * 32]
            kg = w_in[:, o_kg + h * 32:o_kg + (h + 1) * 32]
            vg = w_in[:, o_vg + h * 64:o_vg + (h + 1) * 64]
            og = w_in[:, o_og + h * 64:o_og + (h + 1) * 64]
            af = w_in[:, o_af:o_af + 16]
            ab = w_in[:, o_ab:o_ab + 16]
            vd = w_in[:, o_vd + h * 128:o_vd + (h + 1) * 128]
            pl = w_in[:, o_pl + h * 64:o_pl + (h + 1) * 64]
            w_h = np.ascontiguousarray(np.concatenate(
                [qd, qd[:, perm2], kd, kd[:, perm2], qg, kg, og, af, ab, kg, vg, vd, pl], axis=1))
            wg = np.zeros((33, 64), np.float32)
            wg[0:16, 0:32] = p['gla_wa2_f'][layer][:, h * 32:(h + 1) * 32]
            wg[16:32, 32:64] = p['gla_wa2_b'][layer][:, h * 32:(h + 1) * 32]
            wg[32, 0:32] = p['gla_ba_f'][layer][h * 32:(h + 1) * 32]
            wg[32, 32:64] = p['gla_ba_b'][layer][h * 32:(h + 1) * 32]
            maps.append({
                "xT": XT[b], "cc": cc, "wmod": wmod, "bmod": bmod, "nrm1": nrm1, "w_h": w_h, "wg": wg,
                "gnorm": np.ascontiguousarray(p['gla_norm'][layer][h * 64:(h + 1) * 64, None]),
                "dnorm": np.ascontiguousarray(p['diff_norm'][layer][h * 128:(h + 1) * 128, None]),
                "poolw": np.ascontiguousarray(p['pool_w'][layer][h]),
                "pscale": np.ascontiguousarray(p['pool_scale'][layer][h * 64:(h + 1) * 64, None]),
                "bandm": _band_mats(POOL_WINDOWS[h]),
                "cosT": cosT, "sinT": sinT, "lamv": lamv, "lamc": lamc,
                "trif": trif, "trib": trib, "ident": ident,
            })
    return maps


def gather_A(results, T):
    out = []
    for b in range(NB):
        M = np.empty((D, T), np.float32)
        for h in range(4):
            r = results[b * 4 + h]["mixT"]
            M[h * 64:(h + 1) * 64] = r[0:64]
            M[256 + h * 128:256 + (h + 1) * 128] = r[64:192]
            M[768 + h * 64:768 + (h + 1) * 64] = r[192:256]
        out.append(M)
    return out


CB = CTX // 4


def _make_sts(TB, nst):
    n64 = TB // 64
    base = n64 // nst
    sts, s = [], 0
    for i in range(nst):
        wd = (base + (1 if i < n64 % nst else 0)) * 64
        sts.append((s, wd))
        s += wd
    assert s == TB
    return sts


def emit_B(nc, tk, ps, LB, nst, dr, gmap, uid="", final=True):
    TB = CB + LB
    sts = _make_sts(TB, nst)
    STW = max(w for _, w in sts)
    NSUB = (STW + 127) // 128
    stop = None
    xT, mixT, w_out, cc, wmod, bmod, nrm2, fnorm = (dr[k] for k in ("xT", "mixT", "w_out", "cc", "wmod", "bmod",
                                                                    "nrm2", "fnorm"))
    wr, br, w1, w3, w2, ident, xoT, xfT = (dr[k] for k in ("wr", "br", "w1", "w3", "w2", "ident", "xoT", "xfT"))

    def pieces(s, w):
        out = []
        if s < CB:
            out.append((gmap(s), 0, min(CB, s + w) - s))
        if s + w > CB:
            a = max(CB, s) - s
            out.append((gmap(s + a), a, w))
        return out

    es = contextlib.ExitStack()
    with es:
        sb = lambda name, shape, dt=F32: _sb(nc, es, name + uid, shape, dt)
        PS = lambda i: ("ps", i)
        x3 = xT.rearrange("(k p) t -> p k t", p=128)
        m3 = mixT.rearrange("(k p) t -> p k t", p=128)
        xo3 = xoT.rearrange("(k p) t -> p k t", p=128)
        xf3 = xfT.rearrange("(k p) t -> p k t", p=128) if final else None

        acc = sb("acc", [128, 8, STW])
        h2T = sb("h2T", [128, 8, STW], BF16)
        wb = [sb("wb%d" % i, [128, 12288], BF16) for i in range(2)]
        stg = [sb("stg%d" % i, [128, 1024]) for i in range(2)]
        mst = sb("mst", [128, 8, 512])
        mbf = sb("mbf", [128, 8, 512], BF16)
        aT = sb("aT", [128, 4, 512], BF16)
        sA = [sb("sA%d" % i, [128, 512]) for i in range(2)]
        uB = [sb("uB%d" % i, [128, 512]) for i in range(2)]
        tmpf = [sb("tmpf%d" % i, [128, 512]) for i in range(2)]
        rstd = sb("rstd", [128, 512])
        sq = [sb("sq%d" % i, [128, 512], BF16) for i in range(2)]
        wgt = sb("wgt", [128, NSUB, 16])
        wrep = sb("wrep", [128, 128])
        wrS = sb("wrS", [128, 8, 20])
        brS = sb("brS", [1, 20])
        ones1 = sb("ones1", [1, 128])
        onesb = sb("onesb", [128, 128], BF16)
        identS = sb("identS", [128, 128])
        ccS = sb("ccS", [128, 16])
        scT = sb("scT", [128, 16])
        bmS = sb("bmS", [128, 32])
        n2S = sb("n2S", [128, 8])
        fnS = sb("fnS", [128, 8])
        modS = sb("modS", [128, 64])
        G1 = sb("G1", [128, 2, 8])
        S2 = sb("S2", [128, 2, 8])
        A2 = sb("A2", [128, 2, 8])
        G2m = sb("G2m", [128, 2, 8])
        lg = sb("lg", [128, 20])
        rt = sb("rt", [128, 40])

        def ld(q, dst, src, key):
            tk.dma(q, dst, src, (), (key,))
        ld('sp', identS[:], ident, "identS")
        ld('sp', ccS[:], cc.rearrange("p k c -> p (k c)"), "ccS")
        ld('sp', bmS[:], bmod, "bmS")
        ld('sp', n2S[:], nrm2, "n2S")
        ld('sp', fnS[:], fnorm, "fnS")
        ld('sp', wrS[:], wr.rearrange("(k p) n -> p k n", p=128), "wrS")
        ld('sp', brS[:], br, "brS")
        tk.memset('dve', onesb[:], 1.0 / D, ("onesb",))
        tk.memset('dve', ones1[:], 1.0, ("ones1",))

        tk.act(scT[:], ccS[:], AF.Exp, ("ccS",), ("scT",), scale=-1.0)
        tk.ts('dve', scT[:], scT[:], 1.0, ALU.add, ("scT",), ("scT",))
        tk.op('dve', lambda e: e.reciprocal(out=scT[:], in_=scT[:]), ("scT",), ("scT",))
        tk.tt('dve', scT[:], scT[:], ccS[:], ALU.mult, ("scT", "ccS"), ("scT",))
        wmod3 = wmod.rearrange("(k p) n -> p k n", p=128)
        for blk in range(8):
            tk.dma('sp', mst[:], wmod3[:, :, blk * 512:(blk + 1) * 512], (), ("mst",))
            for jj in range(4):
                j = blk * 4 + jj
                for k in range(8):
                    tk.mm(ps[7][:, 2 * j:2 * j + 2], mst[:, k, jj * 128:(jj + 1) * 128],
                          scT[:, 2 * k:2 * k + 2], k == 0, k == 7, ("mst", "scT"), (PS(7),))
        tk.copy('dve', modS[:], ps[7][:, 0:64], (PS(7),), ("modS",))
        for c in range(2):
            for dst, j0, key in ((G1, 0, "G1"), (S2, 8, "S2"), (A2, 16, "A2"), (G2m, 24, "G2m")):
                tk.tt('dve', dst[:, c, :], modS[:, 2 * j0 + c:2 * j0 + 16:2], bmS[:, j0:j0 + 8], ALU.add,
                      ("modS", "bmS"), (key,))
            tk.ts('dve', A2[:, c, :], A2[:, c, :], 1.0, ALU.add, ("A2",), ("A2",))
            tk.tt('dve', A2[:, c, :], A2[:, c, :], n2S[:], ALU.mult, ("A2", "n2S"), ("A2",))
        if stop == 1:
            tk.finish()
            return nc

        def segs(s, w):
            out = []
            if s < CB:
                out.append((0, min(CB, s + w) - s, 1))
            if s + w > CB:
                a = max(CB, s) - s
                out.append((a, w, 0))
            return out

        cast_i = [0]

        def load_cast(dst_ap, src_ap, dkey, n):
            tk.dma('pool', dst_ap, src_ap, (), (dkey,))

        def mean_rstd(src_fn, w, keys):
            for k in range(8):
                tk.tt('pool', sq[k % 2][:, 0:w], src_fn(k), src_fn(k), ALU.mult, keys, ("sq%d" % (k % 2),))
                tk.mm(ps[7][:, 0:w], onesb[:], sq[k % 2][:, 0:w], k == 0, k == 7,
                      ("onesb", "sq%d" % (k % 2)), (PS(7),))
            tk.ts('dve', rstd[:, 0:w], ps[7][:, 0:w], EPS, ALU.add, (PS(7),), ("rstd",))
            tk.act(rstd[:, 0:w], rstd[:, 0:w], AF.Ln, ("rstd",), ("rstd",))
            tk.act(rstd[:, 0:w], rstd[:, 0:w], AF.Exp, ("rstd",), ("rstd",), scale=-0.5)

        for (S0, SW) in sts:
            ttiles = [(ls, min(512, SW - ls)) for ls in range(0, SW, 512)]
            for k in range(8):
                load_cast(wb[0][:, k * 1024:(k + 1) * 1024], w_out[k * 128:(k + 1) * 128, :], "wb0", 1024)
            for (ls, w) in ttiles:
                s = S0 + ls
                sg = segs(s, w)
                for (g0, a, b) in pieces(s, w):
                    tk.dma('sp', acc[:, :, ls + a:ls + b], x3[:, :, g0:g0 + b - a], (), (("acc", ls),))
                    tk.dma('pool', mbf[:, :, a:b], m3[:, :, g0:g0 + b - a], (), ("mbf",))
                for k2 in range(8):
                    bk = 5 + k2 % 2
                    for k in range(8):
                        tk.mm(ps[bk][:, 0:w], wb[0][:, k * 1024 + k2 * 128:k * 1024 + (k2 + 1) * 128],
                              mbf[:, k, 0:w], k == 0, k == 7, ("wb0", "mbf"), (PS(bk),))
                    for (a, b, c) in sg:
                        tk.stt(acc[:, k2, ls + a:ls + b], ps[bk][:, a:b], G1[:, c, k2:k2 + 1],
                               acc[:, k2, ls + a:ls + b], ALU.mult, ALU.add,
                               (PS(bk), "G1", ("acc", ls)), (("acc", ls),))
                mean_rstd(lambda k: acc[:, k, ls:ls + w], w, (("acc", ls),))
                for k in range(8):
                    tf = tmpf[k % 2]
                    tfk = "tmpf%d" % (k % 2)
                    tk.tt('dve', tf[:, 0:w], acc[:, k, ls:ls + w], rstd[:, 0:w], ALU.mult,
                          (("acc", ls), "rstd"), (tfk,))
                    for (a, b, c) in sg:
                        tk.act(mst[:, k, a:b], tf[:, a:b], AF.Identity, (tfk, "A2", "S2"), ("mst",),
                               bias=S2[:, c, k:k + 1], scale=A2[:, c, k:k + 1])
                    tk.copy('act', h2T[:, k, ls:ls + w], mst[:, k, 0:w], ("mst",), (("h2T", ls),))
                for c0 in range(0, w, 128):
                    m = min(128, w - c0)
                    si = (ls + c0) // 128
                    for k in range(8):
                        tk.mm(ps[7][0:m, 0:20], mst[:, k, c0:c0 + m], wrS[:, k, :], k == 0, False,
                              ("mst", "wrS"), (PS(7),))
                    tk.mm(ps[7][0:m, 0:20], ones1[0:1, 0:m], brS[0:1, :], False, True, ("ones1", "brS"), (PS(7),))
                    R_ = ("rt",)
                    tk.copy('dve', lg[0:m, :], ps[7][0:m, 0:20], (PS(7),), ("lg",))
                    gmax, ngmax, gsum, gtop = rt[0:m, 0:1], rt[0:m, 1:2], rt[0:m, 2:3], rt[0:m, 3:4]
                    ge, oh, esel = rt[0:m, 4:8], rt[0:m, 8:12], rt[0:m, 12:16]
                    m1, nm1, m2, psm = rt[0:m, 16:17], rt[0:m, 17:18], rt[0:m, 18:19], rt[0:m, 19:20]
                    mk1, es2, mk2, pe_ = rt[0:m, 20:24], rt[0:m, 24:28], rt[0:m, 28:32], rt[0:m, 32:36]
                    wl = rt[0:m, 36:40]
                    tk.op('dve', lambda e: e.reduce_max(out=gmax, in_=lg[0:m, 0:4], axis=AX.X), ("lg",), R_)
                    tk.ts('dve', ngmax, gmax, -1.0, ALU.mult, R_, R_)
                    tk.act(ge, lg[0:m, 0:4], AF.Exp, ("lg", "rt"), R_, bias=ngmax)
                    tk.op('dve', lambda e: e.reduce_sum(out=gsum, in_=ge, axis=AX.X), R_, R_)
                    tk.op('dve', lambda e: e.reciprocal(out=gtop, in_=gsum), R_, R_)
                    tk.ts('dve', oh, lg[0:m, 0:4], gmax, ALU.is_equal, ("lg", "rt"), R_)
                    tk.ts('dve', esel, lg[0:m, 4:8], oh[:, 0:1], ALU.mult, ("lg", "rt"), R_)
                    for g in range(1, 4):
                        tk.stt(esel, lg[0:m, 4 + 4 * g:8 + 4 * g], oh[:, g:g + 1], esel, ALU.mult, ALU.add,
                               ("lg", "rt"), R_)
                    tk.op('dve', lambda e: e.reduce_max(out=m1, in_=esel, axis=AX.X), R_, R_)
                    tk.ts('dve', mk1, esel, m1, ALU.is_equal, R_, R_)
                    tk.stt(es2, mk1, -1.0e30, esel, ALU.mult, ALU.add, R_, R_)
                    tk.op('dve', lambda e: e.reduce_max(out=m2, in_=es2, axis=AX.X), R_, R_)
                    tk.ts('dve', mk2, es2, m2, ALU.is_equal, R_, R_)
                    tk.tt('dve', mk2, mk2, mk1, ALU.add, R_, R_)
                    tk.ts('dve', nm1, m1, -1.0, ALU.mult, R_, R_)
                    tk.act(pe_, esel, AF.Exp, R_, R_, bias=nm1)
                    tk.tt('dve', pe_, pe_, mk2, ALU.mult, R_, R_)
                    tk.op('dve', lambda e: e.reduce_sum(out=psm, in_=pe_, axis=AX.X), R_, R_)
                    tk.op('dve', lambda e: e.reciprocal(out=psm, in_=psm), R_, R_)
                    tk.tt('dve', psm, psm, gtop, ALU.mult, R_, R_)
                    tk.ts('dve', wl, pe_, psm, ALU.mult, R_, R_)
                    for g in range(4):
                        tk.ts('dve', wgt[0:m, si, 4 * g:4 * g + 4], wl, oh[:, g:g + 1], ALU.mult,
                              R_, (("wgt", si),))
            if stop == 2:
                tk.finish()
                return nc
            for e in range(NEXP):
                wbe = wb[e % 2]
                wk = "wb%d" % (e % 2)
                for k in range(0, 8, 2):
                    load_cast(wbe[:, k * 512:(k + 2) * 512],
                              w1[e].rearrange("(k p) n -> p k n", p=128)[:, k:k + 2, :], wk, 1024)
                for k in range(0, 8, 2):
                    load_cast(wbe[:, 4096 + k * 512:4096 + (k + 2) * 512],
                              w3[e].rearrange("(k p) n -> p k n", p=128)[:, k:k + 2, :], wk, 1024)
                for dc in range(4):
                    load_cast(wbe[:, 8192 + dc * 1024:8192 + (dc + 1) * 1024],
                              w2[e][dc * 128:(dc + 1) * 128, :], wk, 1024)
                for (ls, w) in ttiles:
                    s = S0 + ls
                    sg = segs(s, w)
                    for c0 in range(0, w, 128):
                        m = min(128, w - c0)
                        si = (ls + c0) // 128
                        tk.copy('dve', wrep[0:m, :], wgt[0:m, si, e:e + 1].to_broadcast([m, 128]),
                                (("wgt", si),), ("wrep",))
                        tk.mm(ps[0][:, c0:c0 + m], wrep[0:m, :], identS[0:m, 0:m], True, True,
                              ("wrep", "identS"), (PS(0),))
                    for dc in range(4):
                        bA, bB = 1 + dc % 2, 3 + dc % 2
                        for k in range(8):
                            tk.mm(ps[bA][:, 0:w], wbe[:, k * 512 + dc * 128:k * 512 + (dc + 1) * 128],
                                  h2T[:, k, ls:ls + w], k == 0, k == 7, (wk, ("h2T", ls)), (PS(bA),))
                        for k in range(8):
                            tk.mm(ps[bB][:, 0:w],
                                  wbe[:, 4096 + k * 512 + dc * 128:4096 + k * 512 + (dc + 1) * 128],
                                  h2T[:, k, ls:ls + w], k == 0, k == 7, (wk, ("h2T", ls)), (PS(bB),))
                        sa, ub = sA[dc % 2], uB[dc % 2]
                        sak, ubk = "sA%d" % (dc % 2), "uB%d" % (dc % 2)
                        tk.act(sa[:, 0:w], ps[bA][:, 0:w], AF.Silu, (PS(bA),), (sak,))
                        tk.tt('dve', ub[:, 0:w], sa[:, 0:w], ps[bB][:, 0:w], ALU.mult, (sak, PS(bB)), (ubk,))
                        tk.tt('dve', aT[:, dc, 0:w], ub[:, 0:w], ps[0][:, 0:w], ALU.mult, (ubk, PS(0)), ("aT",))
                    for k2 in range(8):
                        bk = 5 + k2 % 2
                        for dc in range(4):
                            tk.mm(ps[bk][:, 0:w],
                                  wbe[:, 8192 + dc * 1024 + k2 * 128:8192 + dc * 1024 + (k2 + 1) * 128],
                                  aT[:, dc, 0:w], dc == 0, dc == 3, (wk, "aT"), (PS(bk),))
                        for (a, b, c) in sg:
                            tk.stt(acc[:, k2, ls + a:ls + b], ps[bk][:, a:b], G2m[:, c, k2:k2 + 1],
                                   acc[:, k2, ls + a:ls + b], ALU.mult, ALU.add,
                                   (PS(bk), "G2m", ("acc", ls)), (("acc", ls),))
            for (ls, w) in ttiles:
                s = S0 + ls
                for (g0, a, b) in pieces(s, w):
                    tk.dma('pool', xo3[:, :, g0:g0 + b - a], acc[:, :, ls + a:ls + b], (("acc", ls),), ())
                if not final:
                    continue
                mean_rstd(lambda k: acc[:, k, ls:ls + w], w, (("acc", ls),))
                for k in range(8):
                    tk.stt(mst[:, k, 0:w], acc[:, k, ls:ls + w], fnS[:, k:k + 1], rstd[:, 0:w],
                           ALU.mult, ALU.mult, (("acc", ls), "fnS", "rstd"), ("mst",))
                for (g0, a, b) in pieces(s, w):
                    tk.dma('pool', xf3[:, :, g0:g0 + b - a], mst[:, :, a:b], ("mst",), ())
        tk.barrier()


def build_B(LB, nst):
    TB = CB + LB
    nc = bass.Bass("TRN2", target_bir_lowering=False)

    def din(name, shape, dt=F32):
        return nc.dram_tensor(name, list(shape), dt, kind="ExternalInput").ap()

    dr = {"xT": din("xT", [D, TB]), "mixT": din("mixT", [D, TB]), "w_out": din("w_out", [D, D]),
          "cc": din("cc", [128, 8, 2]), "wmod": din("wmod", [D, 4096]), "bmod": din("bmod", [128, 32]),
          "nrm2": din("nrm2", [128, 8]), "fnorm": din("fnorm", [128, 8]), "wr": din("wr", [D, 20]),
          "br": din("br", [1, 20]), "w1": din("w1", [NEXP, D, DEXP]), "w3": din("w3", [NEXP, D, DEXP]),
          "w2": din("w2", [NEXP, DEXP, D]), "ident": din("ident", [128, 128])}
    dr["xoT"] = nc.dram_tensor("xoT", [D, TB], F32, kind="ExternalOutput").ap()
    dr["xfT"] = nc.dram_tensor("xfT", [D, TB], F32, kind="ExternalOutput").ap()
    es = contextlib.ExitStack()
    with es:
        tk = TK(nc, es)
        ps = _alloc_psum(nc, es)
        emit_B(nc, tk, ps, LB, nst, dr, lambda c: c)
        tk.finish()
    return nc


def prep_B(p, layer, XT, MIX):
    T = XT[0].shape[1]
    L = T - CTX
    LB = L // 4
    wmod = np.ascontiguousarray(p['w_mod'][layer][:, 2048:6144])
    bmod = np.ascontiguousarray(np.asarray(p['b_mod'][layer][2048:6144], np.float32).reshape(32, 128).T)
    bsel = np.concatenate([np.arange(0, 8), np.arange(8, 32)])
    wr = np.ascontiguousarray(np.concatenate([p['router_wg'][layer], p['router_we'][layer]], axis=1))
    br = np.ascontiguousarray(np.concatenate([p['router_bg'][layer], p['router_be'][layer]])[None, :])
    ident = np.eye(128, dtype=np.float32)
    maps = []
    for b in range(NB):
        cc = np.ascontiguousarray(np.stack([_chunks(p['c'][b]), _chunks(p['c_ctx'])], axis=2))
        for j in range(4):
            cols = np.concatenate([np.arange(j * CB, (j + 1) * CB), CTX + np.arange(j * LB, (j + 1) * LB)])
            maps.append({
                "xT": np.ascontiguousarray(XT[b][:, cols]), "mixT": np.ascontiguousarray(MIX[b][:, cols]),
                "w_out": np.ascontiguousarray(p['w_out'][layer]), "cc": cc, "wmod": wmod, "bmod": bmod,
                "nrm2": _chunks(p['norm2'][layer]), "fnorm": _chunks(p['final_norm']),
                "wr": wr, "br": br,
                "w1": np.ascontiguousarray(p['exp_w1'][layer]), "w3": np.ascontiguousarray(p['exp_w3'][layer]),
                "w2": np.ascontiguousarray(p['exp_w2'][layer]), "ident": ident,
            })
    return maps


def gather_B(results, T, key):
    L = T - CTX
    LB = L // 4
    out = []
    for b in range(NB):
        M = np.empty((D, T), np.float32)
        for j in range(4):
            r = results[b * 4 + j][key]
            M[:, j * CB:(j + 1) * CB] = r[:, 0:CB]
            M[:, CTX + j * LB:CTX + (j + 1) * LB] = r[:, CB:]
        out.append(M)
    return out


def build_fused(L, nst):
    C = CTX
    T = C + L
    LB = L // 4
    nc = bass.Bass("TRN2", target_bir_lowering=False)

    def din(name, shape, dt=F32):
        return nc.dram_tensor(name, list(shape), dt, kind="ExternalInput").ap()

    def dint(name, shape, dt=F32):
        return nc.dram_tensor(name, list(shape), dt, kind="Internal").ap()

    xT = din("xT", [D, T])
    cc = din("cc", [128, 8, 2])
    wmod = din("wmod", [2, D, 6144])
    bmod = din("bmod", [2, 128, 48])
    nrm1 = din("nrm1", [2, 128, 8])
    nrm2 = din("nrm2", [2, 128, 8])
    fnorm = din("fnorm", [128, 8])
    w_h = din("w_h", [2, 4, D, 960])
    wg = din("wg", [2, 4, 33, 64])
    gnorm = din("gnorm", [2, 4, 64, 1])
    dnorm = din("dnorm", [2, 4, 128, 1])
    poolw = din("poolw", [2, 4, 64, 64])
    pscale = din("pscale", [2, 4, 64, 1])
    bandm = din("bandm", [4, 5, 128, 128])
    cosT = din("cosT", [128, T])
    sinT = din("sinT", [128, T])
    lamv = din("lamv", [2, 128, 4, 64])
    lamc = din("lamc", [2, 128, 2])
    trif = din("trif", [128, 128])
    trib = din("trib", [128, 128])
    ident = din("ident", [128, 128])
    w_out = din("w_out", [2, D, D])
    wr = din("wr", [2, D, 20])
    br = din("br", [2, 1, 20])
    w1 = din("w1", [2, NEXP, D, DEXP])
    w3 = din("w3", [2, NEXP, D, DEXP])
    w2 = din("w2", [2, NEXP, DEXP, D])
    xfT = nc.dram_tensor("xfT", [D, T], F32, kind="ExternalOutput").ap()
    MIX = dint("MIX", [D, T])
    XN = [dint("XN0", [D, T]), dint("XN1", [D, T])]
    ofT = dint("ofT", [64, T])

    es = contextlib.ExitStack()
    with es:
        tk = TK(nc, es)
        ps = _alloc_psum(nc, es)
        for l in range(2):
            xsrc = xT if l == 0 else XN[0]
            for h in range(4):
                dr = {"xT": xsrc, "cc": cc, "wmod": wmod[l][:, 0:2048], "bmod": bmod[l][:, 0:16], "nrm1": nrm1[l],
                      "w_h": w_h[l, h], "wg": wg[l, h], "gnorm": gnorm[l, h], "dnorm": dnorm[l, h],
                      "poolw": poolw[l, h], "pscale": pscale[l, h], "bandm": bandm[h], "cosT": cosT, "sinT": sinT,
                      "lamv": lamv[l], "lamc": lamc[l], "trif": trif, "trib": trib, "ident": ident, "ofT": ofT,
                      "mix_gla": MIX[h * 64:(h + 1) * 64, :], "mix_diff": MIX[256 + h * 128:256 + (h + 1) * 128, :],
                      "mix_pool": MIX[768 + h * 64:768 + (h + 1) * 64, :]}
                emit_A(nc, tk, ps, L, dr, uid="_a%d%d" % (l, h))
            for j in range(4):
                dr = {"xT": xsrc, "mixT": MIX, "w_out": w_out[l], "cc": cc, "wmod": wmod[l][:, 2048:6144],
                      "bmod": bmod[l][:, 16:48], "nrm2": nrm2[l], "fnorm": fnorm, "wr": wr[l], "br": br[l],
                      "w1": w1[l], "w3": w3[l], "w2": w2[l], "ident": ident, "xoT": XN[l], "xfT": xfT}
                gm = (lambda jj: (lambda c: jj * CB + c if c < CB else CTX + jj * LB + (c - CB)))(j)
                emit_B(nc, tk, ps, LB, nst, dr, gm, uid="_b%d%d" % (l, j), final=(l == 1))
        tk.finish()
    return nc


def prep_fused(p):
    L = p['x'].shape[1]
    cosT, sinT = _rope_tables(L)
    perm = _rope_perm()
    perm2 = np.concatenate([perm, 64 + perm])
    o_qg, o_kg, o_vg, o_og, o_af, o_ab, o_qd, o_kd, o_vd, o_pl = [int(v) for v in _IN_OFF[:10]]
    f32 = np.float32
    w_h = np.empty((2, 4, D, 960), f32)
    wg = np.zeros((2, 4, 33, 64), f32)
    gnorm = np.empty((2, 4, 64, 1), f32)
    dnorm = np.empty((2, 4, 128, 1), f32)
    pscale = np.empty((2, 4, 64, 1), f32)
    lamv = np.empty((2, 128, 4, 64), f32)
    lamc = np.empty((2, 128, 2), f32)
    for l in range(2):
        w_in = np.asarray(p['w_in'][l], f32)
        lam_init = 0.8 - 0.6 * math.exp(-0.3 * l)
        lamv[l] = np.stack([p['lam_q1'][l], p['lam_k1'][l], p['lam_q2'][l], p['lam_k2'][l]], 0)[None]
        lamc[l] = np.array([lam_init, 1.0 - lam_init], f32)[None]
        for h in range(4):
            qd = w_in[:, o_qd + h * 128:o_qd + (h + 1) * 128]
            kd = w_in[:, o_kd + h * 128:o_kd + (h + 1) * 128]
            qg = w_in[:, o_qg + h * 32:o_qg + (h + 1) * 32]
            kg = w_in[:, o_kg + h * 32:o_kg + (h + 1) * 32]
            vg = w_in[:, o_vg + h * 64:o_vg + (h + 1) * 64]
            og = w_in[:, o_og + h * 64:o_og + (h + 1) * 64]
            af = w_in[:, o_af:o_af + 16]
            ab = w_in[:, o_ab:o_ab + 16]
            vd = w_in[:, o_vd + h * 128:o_vd + (h + 1) * 128]
            pl = w_in[:, o_pl + h * 64:o_pl + (h + 1) * 64]
            w_h[l, h] = np.concatenate([qd, qd[:, perm2], kd, kd[:, perm2], qg, kg, og, af, ab, kg, vg, vd, pl], axis=1)
            wg[l, h, 0:16, 0:32] = p['gla_wa2_f'][l][:, h * 32:(h + 1) * 32]
            wg[l, h, 16:32, 32:64] = p['gla_wa2_b'][l][:, h * 32:(h + 1) * 32]
            wg[l, h, 32, 0:32] = p['gla_ba_f'][l][h * 32:(h + 1) * 32]
            wg[l, h, 32, 32:64] = p['gla_ba_b'][l][h * 32:(h + 1) * 32]
            gnorm[l, h, :, 0] = p['gla_norm'][l][h * 64:(h + 1) * 64]
            dnorm[l, h, :, 0] = p['diff_norm'][l][h * 128:(h + 1) * 128]
            pscale[l, h, :, 0] = p['pool_scale'][l][h * 64:(h + 1) * 64]
    shared = {
        "wmod": np.ascontiguousarray(p['w_mod'], f32),
        "bmod": np.ascontiguousarray(np.stack([np.asarray(p['b_mod'][l], f32).reshape(48, 128).T for l in range(2)])),
        "nrm1": np.stack([_chunks(p['norm1'][l]) for l in range(2)]),
        "nrm2": np.stack([_chunks(p['norm2'][l]) for l in range(2)]),
        "fnorm": _chunks(p['final_norm']),
        "w_h": w_h, "wg": wg, "gnorm": gnorm, "dnorm": dnorm,
        "poolw": np.ascontiguousarray(p['pool_w'], f32), "pscale": pscale,
        "bandm": np.stack([_band_mats(wn) for wn in POOL_WINDOWS]),
        "cosT": cosT, "sinT": sinT, "lamv": lamv, "lamc": lamc,
        "trif": np.triu(np.ones((128, 128), f32)), "trib": np.tril(np.ones((128, 128), f32)),
        "ident": np.eye(128, dtype=f32),
        "w_out": np.ascontiguousarray(p['w_out'], f32),
        "wr": np.ascontiguousarray(np.concatenate([p['router_wg'], p['router_we']], axis=2), f32),
        "br": np.ascontiguousarray(np.concatenate([p['router_bg'], p['router_be']], axis=1)[:, None, :], f32),
        "w1": np.ascontiguousarray(p['exp_w1'], f32), "w3": np.ascontiguousarray(p['exp_w3'], f32),
        "w2": np.ascontiguousarray(p['exp_w2'], f32),
    }
    maps = []
    for core in range(8):
        b = core // 4
        m = dict(shared)
        m["xT"] = np.ascontiguousarray(np.concatenate([p['ctx'][b].T, p['x'][b].T], axis=1).astype(f32))
        m["cc"] = np.ascontiguousarray(np.stack([_chunks(p['c'][b]), _chunks(p['c_ctx'])], axis=2))
        maps.append(m)
    return maps


def kernel_fused(**inputs):
    p = {k: np.asarray(v) for k, v in inputs.items()}
    L = p['x'].shape[1]
    nst = 3 if L >= 8192 else 2
    nc = build_fused(L, nst)
    res = run_bass_kernel_spmd(nc, prep_fused(p), core_ids=list(range(8)))
    out = np.stack([np.ascontiguousarray(res.results[b * 4]["xfT"][:, CTX:].T) for b in range(NB)], axis=0)
    return out.astype(np.float32)


def kernel_unfused(**inputs):
    p = {k: np.asarray(v) for k, v in inputs.items()}
    L = p['x'].shape[1]
    T = CTX + L
    nst = 3 if L >= 8192 else 2
    XT = [np.ascontiguousarray(np.concatenate([p['ctx'][b].T, p['x'][b].T], axis=1).astype(np.float32))
          for b in range(NB)]
    XF = None
    for layer in range(2):
        ncA = build_A(L)
        resA = run_bass_kernel_spmd(ncA, prep_A(p, layer, XT), core_ids=list(range(8)))
        MIX = gather_A(resA.results, T)
        del resA
        ncB = build_B(L // 4, nst)
        resB = run_bass_kernel_spmd(ncB, prep_B(p, layer, XT, MIX), core_ids=list(range(8)))
        XT = gather_B(resB.results, T, "xoT")
        if layer == 1:
            XF = gather_B(resB.results, T, "xfT")
        del resB, MIX
    out = np.stack([np.ascontiguousarray(XF[b][:, CTX:].T) for b in range(NB)], axis=0)
    return out.astype(np.float32)


def kernel(**inputs):
    return kernel_unfused(**inputs)
```
